# Optimizing a Trainium2 kernel written in Bass

```python
import jax
import jax.numpy as jnp
from jax import lax
import numpy as np

D_MODEL = 1024
BATCH = 8
SEQ = 4096
DEPTH = 4

CTX_LEN = 256
GRID_W = 64
HEAD_DIM = 64
ROPE_HALF = HEAD_DIM // 2
ROPE_THETA = 10000.0
ATTN_HEADS = 8
ATTN_KV_HEADS = 2
ATTN_GROUP = ATTN_HEADS // ATTN_KV_HEADS
ATTN_WIDTH = ATTN_HEADS * HEAD_DIM
KV_WIDTH = ATTN_KV_HEADS * HEAD_DIM
ATTN_IN = ATTN_WIDTH + 2 * KV_WIDTH
ATTN_SCALE = HEAD_DIM ** -0.5
Q_BLOCK = 128
RWKV_HEADS = 8
RWKV_WIDTH = RWKV_HEADS * HEAD_DIM
DECAY_LORA = 64
ICLR_LORA = 64
GATE_LORA = 128
SHIFT_WIDTH = 3 * RWKV_WIDTH + 2 * DECAY_LORA + 2 * ICLR_LORA + GATE_LORA
IN_WIDTH = ATTN_IN + SHIFT_WIDTH
MIX_WIDTH = ATTN_WIDTH + RWKV_WIDTH
D_FF_DENSE = 2816
D_FF_EXPERT = 3584
N_EXPERTS = 8
TOP_K = 2
MOE_BLOCK = 128
N_DENSE = (DEPTH + 1) // 2
N_MOE = DEPTH // 2
N_MOD = 6
EPS = 1e-6
GN_EPS = 64e-5
F32 = jnp.float32

kernel_name = 'hymba_gqa_rwkv7_moe_prefix_dit'


def rmsnorm(x, g):
    xf = x.astype(F32)
    y = xf * lax.rsqrt(jnp.mean(xf * xf, axis=-1, keepdims=True) + EPS)
    return (y * g.astype(F32)).astype(x.dtype)


def axial_rope(rows):
    row = jnp.repeat(jnp.arange(rows, dtype=F32), GRID_W)
    col = jnp.tile(jnp.arange(GRID_W, dtype=F32), rows)
    inv = ROPE_THETA ** (-jnp.arange(0, ROPE_HALF, 2, dtype=F32) / ROPE_HALF)
    ang = jnp.concatenate([row[:, None] * inv, col[:, None] * inv], axis=-1)
    return jnp.cos(ang), jnp.sin(ang)


def apply_rope(x, cos, sin):
    shape = (1, x.shape[1]) + (1,) * (x.ndim - 3) + (ROPE_HALF,)
    c, s = cos.reshape(shape), sin.reshape(shape)
    xf = x.astype(F32)
    x1, x2 = xf[..., :ROPE_HALF], xf[..., ROPE_HALF:]
    return jnp.concatenate([x1 * c - x2 * s, x1 * s + x2 * c], axis=-1).astype(x.dtype)


def attn_heads(p):
    B, L = p.shape[:2]
    q = p[..., :ATTN_WIDTH].reshape(B, L, ATTN_KV_HEADS, ATTN_GROUP, HEAD_DIM)
    k = p[..., ATTN_WIDTH:ATTN_WIDTH + KV_WIDTH].reshape(B, L, ATTN_KV_HEADS, HEAD_DIM)
    v = p[..., ATTN_WIDTH + KV_WIDTH:ATTN_IN].reshape(B, L, ATTN_KV_HEADS, HEAD_DIM)
    return q, k, v


def attend(q, k, v):
    s = jnp.einsum('bqkgd,bskd->bkgqs', q, k, preferred_element_type=F32) * ATTN_SCALE
    p = jax.nn.softmax(s, axis=-1).astype(v.dtype)
    return jnp.einsum('bkgqs,bskd->bqkgd', p, v)


def blocked_attention(q, k, v):
    B, L = q.shape[:2]
    qb = q.reshape((B, L // Q_BLOCK, Q_BLOCK) + q.shape[2:]).swapaxes(0, 1)
    ob = lax.map(lambda qq: attend(qq, k, v), qb)
    return ob.swapaxes(0, 1).reshape(q.shape)


def token_shift(p, mu):
    zero = jnp.zeros_like(p[:, :1])
    prev = jnp.concatenate([zero, p[:, :-1]], axis=1)
    nxt = jnp.concatenate([p[:, 1:], zero], axis=1)
    return p + mu * (0.5 * (prev + nxt) - p)


def rwkv_streams(p, lp):
    p = p.astype(F32)
    B, L = p.shape[:2]
    W = RWKV_WIDTH
    heads = lambda t: t.reshape(B, L, RWKV_HEADS, HEAD_DIM)
    r, k, v = p[..., :W], p[..., W:2 * W], p[..., 2 * W:3 * W]
    o = 3 * W
    wd = [p[..., o + d * DECAY_LORA:o + (d + 1) * DECAY_LORA] for d in range(2)]
    o += 2 * DECAY_LORA
    ad = [p[..., o + d * ICLR_LORA:o + (d + 1) * ICLR_LORA] for d in range(2)]
    gd = p[..., o + 2 * ICLR_LORA:]
    kk = heads(k * lp['kk'])
    kk = kk * lax.rsqrt(jnp.sum(kk * kk, axis=-1, keepdims=True) + 1e-12)
    dirs = []
    for d in range(2):
        w_log = -jax.nn.softplus(-(lp['w0'][d] + jnp.tanh(wd[d]) @ lp['w2'][d])) - 0.5
        a = jax.nn.sigmoid(lp['a0'][d] + ad[d] @ lp['a2'][d])
        k_d = k * (1.0 + (a - 1.0) * lp['ka'])
        dirs.append((heads(jnp.exp(-jnp.exp(w_log))), heads(k_d), heads(a)))
    return heads(r), heads(v), kk, gd, dirs


def rwkv_scan(S0, dirn, v, kk, r, reverse):
    decay, k, a = dirn
    xs = (decay, k, v, kk, kk * a) + (() if r is None else (r,))
    xs = tuple(t.swapaxes(0, 1) for t in xs)

    def step(S, inp):
        w_t, k_t, v_t, kk_t, b_t = inp[:5]
        sa = jnp.einsum('bhvk,bhk->bhv', S, kk_t)
        S = S * w_t[:, :, None, :] - sa[..., None] * b_t[:, :, None, :] + v_t[..., None] * k_t[:, :, None, :]
        out = None if r is None else jnp.einsum('bhvk,bhk->bhv', S, inp[5])
        return S, out

    S, o = lax.scan(step, S0, xs, reverse=reverse)
    return S, (None if r is None else o.swapaxes(0, 1))


def rwkv_readout(o_dirs, r, v, dirs, gd, lp):
    o = o_dirs[0] + o_dirs[1]
    B, L = o.shape[:2]
    mu = jnp.mean(o, axis=-1, keepdims=True)
    var = jnp.mean(jnp.square(o - mu), axis=-1, keepdims=True)
    y = (o - mu) * lax.rsqrt(var + GN_EPS)
    k_sum = dirs[0][1] + dirs[1][1]
    bonus = jnp.sum(r * k_sum * lp['rk'].reshape(RWKV_HEADS, HEAD_DIM), axis=-1, keepdims=True) * v
    y = y.reshape(B, L, RWKV_WIDTH) * lp['gn_w'] + lp['gn_b'] + bonus.reshape(B, L, RWKV_WIDTH)
    g = jax.nn.sigmoid(gd) @ lp['g2']
    return y * g


def mixer(h, hc, cos, sin, lp, ctx_out):
    B, L, _ = h.shape
    Lc = hc.shape[1]
    p = h @ lp['w_in']
    pc = hc @ lp['w_in']
    q, k, v = attn_heads(p)
    q = apply_rope(rmsnorm(q, lp['q_gain']), cos, sin)
    k = apply_rope(rmsnorm(k, lp['k_gain']), cos, sin)
    qc, kc, vc = attn_heads(pc)
    kc = rmsnorm(kc, lp['k_gain'])
    k_all = jnp.concatenate([k, kc], axis=1)
    v_all = jnp.concatenate([v, vc], axis=1)
    att = blocked_attention(q, k_all, v_all).reshape(B, L, ATTN_WIDTH)
    r_l, v_l, kk_l, gd_l, dirs_l = rwkv_streams(token_shift(p[..., ATTN_IN:], lp['mu']), lp)
    r_c, v_c, kk_c, gd_c, dirs_c = rwkv_streams(token_shift(pc[..., ATTN_IN:], lp['mu']), lp)
    S0 = jnp.zeros((B, RWKV_HEADS, HEAD_DIM, HEAD_DIM), F32)
    o_l, o_c = [], []
    for d, rev in enumerate((False, True)):
        S_c, oc = rwkv_scan(S0, dirs_c[d], v_c, kk_c, r_c if ctx_out else None, rev)
        _, ol = rwkv_scan(S_c, dirs_l[d], v_l, kk_l, r_l, rev)
        o_l.append(ol)
        o_c.append(oc)
    rw = rwkv_readout(o_l, r_l, v_l, dirs_l, gd_l, lp).astype(h.dtype)
    out = jnp.concatenate([att, rw], axis=-1) @ lp['w_out']
    if not ctx_out:
        return out, None
    att_c = attend(rmsnorm(qc, lp['q_gain']), kc, vc).reshape(B, Lc, ATTN_WIDTH)
    rw_c = rwkv_readout(o_c, r_c, v_c, dirs_c, gd_c, lp).astype(hc.dtype)
    out_c = jnp.concatenate([att_c, rw_c], axis=-1) @ lp['w_out']
    return out, out_c


def swiglu(h, wg, wu, wd):
    return (jax.nn.silu(h @ wg) * (h @ wu)) @ wd


def moe_ffn(h, w_router, wg, wu, wd):
    T, D = h.shape
    logits = (h @ w_router).astype(F32)
    top_val, top_idx = lax.top_k(logits, TOP_K)
    gates = jax.nn.softmax(top_val, axis=-1)
    n_assign = T * TOP_K
    expert = top_idx.reshape(-1)
    token = jnp.repeat(jnp.arange(T, dtype=jnp.int32), TOP_K)
    gate = gates.reshape(-1)
    order = jnp.argsort(expert)
    e_sorted = expert[order]
    counts = jnp.bincount(expert, length=N_EXPERTS)
    padded = (counts + MOE_BLOCK - 1) // MOE_BLOCK * MOE_BLOCK
    pad_end = jnp.cumsum(padded)
    pad_start = pad_end - padded
    grp_start = jnp.cumsum(counts) - counts
    slot = pad_start[e_sorted] + jnp.arange(n_assign) - grp_start[e_sorted]
    nb = (n_assign + N_EXPERTS * (MOE_BLOCK - 1) + MOE_BLOCK - 1) // MOE_BLOCK
    n_slots = nb * MOE_BLOCK
    slot_token = jnp.full((n_slots,), T, jnp.int32).at[slot].set(token[order])
    slot_gate = jnp.zeros((n_slots,), h.dtype).at[slot].set(gate[order].astype(h.dtype))
    block_expert = jnp.minimum(jnp.searchsorted(pad_end, jnp.arange(nb) * MOE_BLOCK, side='right'), N_EXPERTS - 1)
    h_pad = jnp.concatenate([h, jnp.zeros((1, D), h.dtype)], axis=0)
    xb = h_pad[slot_token].reshape(nb, MOE_BLOCK, D)

    def expert_block(args):
        xblk, e = args
        return swiglu(xblk, wg[e], wu[e], wd[e])

    yb = lax.map(expert_block, (xb, block_expert)).reshape(n_slots, D)
    out = jnp.zeros((T + 1, D), h.dtype).at[slot_token].add(yb * slot_gate[:, None])
    return out[:T]


def setup_inputs(seed: int = 0) -> dict:
    key = jax.random.key(seed)
    keys = jax.random.split(key, 40)
    counter = [0]

    def nxt():
        counter[0] += 1
        return keys[counter[0] - 1]

    def nrm(shape, scale):
        return jax.random.normal(nxt(), shape, F32) * scale

    def gain(shape):
        return 1.0 + nrm(shape, 0.02)

    def unif(shape, lo, hi):
        return jax.random.uniform(nxt(), shape, F32, lo, hi)

    D = D_MODEL
    return {
        'x': nrm((BATCH, SEQ, D), 1.0),
        'c': nrm((BATCH, D), 1.0),
        'ctx': nrm((BATCH, CTX_LEN, D), 1.0),
        'c_ctx': nrm((D,), 1.0),
        'ada_w': nrm((DEPTH, D, N_MOD * D), 0.5 * D ** -0.5),
        'ada_b': nrm((DEPTH, N_MOD * D), 0.02),
        'norm1_g': gain((DEPTH, D)),
        'norm2_g': gain((DEPTH, D)),
        'w_in': nrm((DEPTH, D, IN_WIDTH), D ** -0.5),
        'w_out': nrm((DEPTH, MIX_WIDTH, D), MIX_WIDTH ** -0.5),
        'q_gain': gain((DEPTH, HEAD_DIM)),
        'k_gain': gain((DEPTH, HEAD_DIM)),
        'shift_mu': unif((DEPTH, SHIFT_WIDTH), 0.0, 1.0),
        'rw_w0': unif((DEPTH, 2, RWKV_WIDTH), -4.0, 1.0),
        'rw_w2': nrm((DEPTH, 2, DECAY_LORA, RWKV_WIDTH), 0.1),
        'rw_a0': nrm((DEPTH, 2, RWKV_WIDTH), 0.5),
        'rw_a2': nrm((DEPTH, 2, ICLR_LORA, RWKV_WIDTH), ICLR_LORA ** -0.5),
        'rw_g2': nrm((DEPTH, GATE_LORA, RWKV_WIDTH), GATE_LORA ** -0.5),
        'rw_kk': 0.85 + nrm((DEPTH, RWKV_WIDTH), 0.02),
        'rw_ka': gain((DEPTH, RWKV_WIDTH)),
        'rw_rk': nrm((DEPTH, RWKV_WIDTH), 0.1),
        'rw_gn_w': gain((DEPTH, RWKV_WIDTH)),
        'rw_gn_b': nrm((DEPTH, RWKV_WIDTH), 0.02),
        'ffn_wg': nrm((N_DENSE, D, D_FF_DENSE), D ** -0.5),
        'ffn_wu': nrm((N_DENSE, D, D_FF_DENSE), D ** -0.5),
        'ffn_wd': nrm((N_DENSE, D_FF_DENSE, D), D_FF_DENSE ** -0.5),
        'moe_router': nrm((N_MOE, D, N_EXPERTS), D ** -0.5),
        'moe_wg': nrm((N_MOE, N_EXPERTS, D, D_FF_EXPERT), D ** -0.5),
        'moe_wu': nrm((N_MOE, N_EXPERTS, D, D_FF_EXPERT), D ** -0.5),
        'moe_wd': nrm((N_MOE, N_EXPERTS, D_FF_EXPERT, D), D_FF_EXPERT ** -0.5),
    }


def reference(x, c, ctx, c_ctx, ada_w, ada_b, norm1_g, norm2_g, w_in, w_out, q_gain, k_gain, shift_mu,
              rw_w0, rw_w2, rw_a0, rw_a2, rw_g2, rw_kk, rw_ka, rw_rk, rw_gn_w, rw_gn_b,
              ffn_wg, ffn_wu, ffn_wd, moe_router, moe_wg, moe_wu, moe_wd):
    B, L, D = x.shape
    rows = L // GRID_W
    cos, sin = axial_rope(rows)
    silu_c = jax.nn.silu(c)
    silu_cc = jax.nn.silu(c_ctx)
    xc = ctx
    for i in range(DEPTH):
        last = i == DEPTH - 1
        lp = dict(w_in=w_in[i], w_out=w_out[i], q_gain=q_gain[i], k_gain=k_gain[i], mu=shift_mu[i],
                  w0=rw_w0[i], w2=rw_w2[i], a0=rw_a0[i], a2=rw_a2[i], g2=rw_g2[i], kk=rw_kk[i],
                  ka=rw_ka[i], rk=rw_rk[i], gn_w=rw_gn_w[i], gn_b=rw_gn_b[i])
        m = (silu_c @ ada_w[i] + ada_b[i]).reshape(B, N_MOD, 1, D)
        mc = (silu_cc @ ada_w[i] + ada_b[i]).reshape(N_MOD, D)
        h = rmsnorm(x, norm1_g[i]) * (1.0 + m[:, 1]) + m[:, 0]
        hc = rmsnorm(xc, norm1_g[i]) * (1.0 + mc[1]) + mc[0]
        mix, mix_c = mixer(h, hc, cos, sin, lp, not last)
        x = x + m[:, 2] * mix
        h = rmsnorm(x, norm2_g[i]) * (1.0 + m[:, 4]) + m[:, 3]
        tokens = h.reshape(B * L, D)
        if not last:
            xc = xc + mc[2] * mix_c
            hc = rmsnorm(xc, norm2_g[i]) * (1.0 + mc[4]) + mc[3]
            tokens = jnp.concatenate([tokens, hc.reshape(-1, D)], axis=0)
        j = i // 2
        if i % 2 == 0:
            f = swiglu(tokens, ffn_wg[j], ffn_wu[j], ffn_wd[j])
        else:
            f = moe_ffn(tokens, moe_router[j], moe_wg[j], moe_wu[j], moe_wd[j])
        x = x + m[:, 5] * f[:B * L].reshape(B, L, D)
        if not last:
            xc = xc + mc[5] * f[B * L:].reshape(xc.shape)
    return x
```

```python
import contextlib
import numpy as np
import ml_dtypes
import concourse.bass as bass
import concourse.mybir as mybir
from concourse.bass_utils import run_bass_kernel_spmd

F32 = mybir.dt.float32
BF16 = mybir.dt.bfloat16
AF = mybir.ActivationFunctionType
ALU = mybir.AluOpType
AX = mybir.AxisListType

D = 1024
L = 4096
LC = 256
T = L + LC
NT = T // 128
NLT = L // 128
DEPTH = 4
HD = 64
ATT_IN = 768
SHIFT_W = 1920
IN_W = 2688
DFF = 2816
DFE = 3584
NE = 8
EPS = 1e-6
GN_EPS = 64e-5
CH = 64
NCH = T // CH
DECAY_C = float(np.exp(-0.5))


class Res:
    __slots__ = ("w", "r")

    def __init__(self):
        self.w = None
        self.r = {}


class Prog:
    LIMIT = 30000

    def __init__(self, nc, stack):
        self.nc = nc
        self.stack = stack
        self.eng = {"pe": nc.tensor, "act": nc.scalar, "dve": nc.vector, "pool": nc.gpsimd, "sp": nc.sync}
        self.sem = {}
        self.val = {}
        self.cur = {}
        self.nsem = 0
        self.waited = {e: {} for e in self.eng}
        for e in ("pe", "act", "dve", "pool"):
            self._fresh(e)
        self.slots = {}
        self.rr = {}
        for q, n in (("sp", 8), ("act", 4), ("pool", 6)):
            self.slots[q] = []
            self.rr[q] = 0
            for i in range(n):
                b = "d_%s%d" % (q, i)
                self._fresh(b)
                self.slots[q].append(b)
        self.dram_res = {}
        self.n_inst = 0

    def _fresh(self, base):
        ep = self.cur[base][1] + 1 if base in self.cur else 0
        k = (base, ep)
        self.nsem += 1
        self.sem[k] = self.stack.enter_context(self.nc.semaphore("s%d_%s_%d" % (self.nsem, base, ep)))
        self.val[k] = 0
        self.cur[base] = k
        return k

    def R(self, *key):
        r = self.dram_res.get(key)
        if r is None:
            r = self.dram_res[key] = Res()
        return r

    def _deps(self, eng, reads, writes, extra=()):
        need = {}
        for k, v in extra:
            if v > need.get(k, 0):
                need[k] = v
        for t in reads:
            if t.w is not None:
                k, v = t.w
                if v > need.get(k, 0):
                    need[k] = v
        for t in writes:
            if t.w is not None:
                k, v = t.w
                if k[0] != eng and v > need.get(k, 0):
                    need[k] = v
            for k, v in t.r.items():
                if k[0] != eng and v > need.get(k, 0):
                    need[k] = v
        wd = self.waited[eng]
        e = self.eng[eng]
        for k, v in need.items():
            if k[0] == eng and eng == "pe":
                continue
            if wd.get(k, 0) >= v:
                continue
            wd[k] = v
            e.wait_ge(self.sem[k], v)
            self.n_inst += 1

    def _mark(self, ev, reads, writes):
        k, v = ev
        for t in reads:
            if v > t.r.get(k, 0):
                t.r[k] = v
        for t in writes:
            t.w = ev
            t.r = {}

    def op(self, eng, fn, reads=(), writes=()):
        self._deps(eng, reads, writes)
        ins = fn(self.eng[eng])
        k = self.cur[eng]
        if self.val[k] >= self.LIMIT:
            k = self._fresh(eng)
        self.val[k] += 1
        ins.then_inc(self.sem[k], 1)
        self.n_inst += 1
        self._mark((k, self.val[k]), reads, writes)

    def dma(self, q, out, in_, reads=(), writes=(), **kw):
        sl = self.slots[q]
        b = sl[self.rr[q] % len(sl)]
        self.rr[q] += 1
        k = self.cur[b]
        self._deps(q, reads, writes, extra=((k, self.val[k]),))
        ins = self.eng[q].dma_start(out=out, in_=in_, **kw)
        if self.val[k] >= self.LIMIT:
            k = self._fresh(b)
        self.val[k] += 16
        ins.then_inc(self.sem[k], 16)
        self.n_inst += 1
        self._mark((k, self.val[k]), reads, writes)

    def barrier(self):
        for e in self.eng:
            wd = self.waited[e]
            for k, v in self.val.items():
                if v > 0 and wd.get(k, 0) < v and not (k[0] == e and e == "pe"):
                    wd[k] = v
                    self.eng[e].wait_ge(self.sem[k], v)

    def finish(self):
        wd = self.waited["sp"]
        for k, v in self.val.items():
            if v > 0 and wd.get(k, 0) < v:
                wd[k] = v
                self.eng["sp"].wait_ge(self.sem[k], v)


class Tile:
    def __init__(self, t):
        self.t = t
        self.res = Res()

    def __getitem__(self, key):
        return self.t[key]


def build(nc, n_layers=DEPTH, stop_after=None, dbg=()):
    stack = contextlib.ExitStack()
    with stack:
        _build(nc, stack, n_layers, stop_after, dbg)
    return nc


def _build(nc, stack, n_layers, stop_after, dbg):
    P = Prog(nc, stack)

    def din(name, shape, dt=F32):
        return nc.dram_tensor(name, list(shape), dt, kind="ExternalInput").ap()

    def dscr(name, shape, dt=F32):
        kind = "ExternalOutput" if name in dbg else "Internal"
        return nc.dram_tensor(name, list(shape), dt, kind=kind).ap()

    sb_n = [0]

    def sb(name, shape, dt=F32, st=None):
        sb_n[0] += 1
        return Tile((st or stack).enter_context(nc.sbuf_tensor("%s_%d" % (name, sb_n[0]), list(shape), dt)))

    x_in = din("x", [L, D])
    ctx_in = din("ctx", [LC, D])
    cvec = din("cvec", [128, 16])
    ada_w = din("ada_w", [DEPTH, D, 6 * D])
    ada_b = din("ada_b", [DEPTH, 6 * D])
    norm1_g = din("norm1_g", [DEPTH, D])
    norm2_g = din("norm2_g", [DEPTH, D])
    w_in = din("w_in", [DEPTH, D, IN_W])
    w_out = din("w_out", [DEPTH, D, D])
    qk_gain = din("qk_gain", [DEPTH, 2 * HD])
    rope = din("rope", [L, 64])
    ident_d = din("ident", [128, 128])
    rwv = din("rwv", [DEPTH, 128, 43])
    rw_w2 = din("rw_w2", [DEPTH, 128, 512])
    rw_a2 = din("rw_a2", [DEPTH, 128, 512])
    rw_g2 = din("rw_g2", [DEPTH, 128, 512])
    gn_wb = din("gn_wb", [DEPTH, 2, 512])
    ffn_wg = din("ffn_wg", [2, D, DFF])
    ffn_wu = din("ffn_wu", [2, D, DFF])
    ffn_wd = din("ffn_wd", [2, DFF, D])
    moe_router = din("moe_router", [2, D, NE])
    moe_wg = din("moe_wg", [2, NE, D, DFE])
    moe_wu = din("moe_wu", [2, NE, D, DFE])
    moe_wd = din("moe_wd", [2, NE, DFE, D])
    cst128 = din("cst128", [128, 386])
    cst64 = din("cst64", [64, 2048], BF16)
    cstbd = din("cstbd", [128, 3, 512], BF16)
    y_out = nc.dram_tensor("y", [L, D], F32, kind="ExternalOutput").ap()

    xs = dscr("xs", [T, D])
    pT = dscr("pT", [SHIFT_W, T])
    qs = dscr("qs", [T, 512], BF16)
    FM = [dscr("FM%d" % d, [NCH, 64, 2, 512], BF16) for d in range(2)]
    TM = [dscr("TM%d" % d, [NCH, 64, 1024], BF16) for d in range(2)]
    VM = dscr("VM", [NCH, 64, 512], BF16)
    MAT = [dscr("MAT%d" % d, [NCH, 64, 2560], BF16) for d in range(2)]
    Od = [dscr("Od%d" % d, [T, 512]) for d in range(2)]
    gtm = dscr("gtm", [T, 512])
    bon = dscr("bon", [T, 8])
    H2 = dscr("H2", [9, 128, 8, 512], BF16)

    ident_f = sb("ident_f", [128, 128])
    ident_b = sb("ident_b", [128, 128], BF16)
    ones_f = sb("ones_f", [128, 128])
    sc = sb("sc", [128, 16])
    mods = [[sb("mod%d_%d" % (w, j), [128, D]) for j in range(6)] for w in range(2)]
    psum = [Tile(stack.enter_context(nc.psum_tensor("ps%d" % i, [128, 512], F32))) for i in range(8)]
    ps_rr = [0]
    uid = [0]

    def next_ps():
        p = psum[ps_rr[0] % 8]
        ps_rr[0] += 1
        return p

    P.dma("sp", ident_f[:], ident_d[:, :], writes=[ident_f.res])
    P.op("dve", lambda e: e.tensor_copy(out=ident_b[:], in_=ident_f[:]), reads=[ident_f.res], writes=[ident_b.res])
    P.op("pool", lambda e: e.memset(ones_f[:], 1.0), writes=[ones_f.res])
    P.dma("sp", sc[:], cvec[:, :], writes=[sc.res])
    P.op("act", lambda e: e.activation(out=sc[:], in_=sc[:], func=AF.Silu), reads=[sc.res], writes=[sc.res])
    for i in range(8):
        P.dma("pool", xs[i * 512:(i + 1) * 512, :], x_in[i * 512:(i + 1) * 512, :], writes=[P.R("xs", 4 * i + j) for j in range(4)])
    P.dma("pool", xs[L:T, :], ctx_in[:, :], writes=[P.R("xs", 32), P.R("xs", 33)])

    for li in range(n_layers):
        last = li == DEPTH - 1
        with contextlib.ExitStack() as st:
            wst = [sb("adaw%d" % i, [128, 8, 512], st=st) for i in range(2)]
            lrep = sb("lrep", [128, 16, 128], st=st)
            P.op("dve", lambda e: e.tensor_copy(out=lrep[:], in_=sc[:].unsqueeze(2).to_broadcast([128, 16, 128])),
                 reads=[sc.res], writes=[lrep.res])
            bst = sb("adab", [1, 6 * D], st=st)
            gst = sb("g1bc", [128, D], st=st)
            g2st = sb("g2bc", [128, D], st=st)
            P.dma("sp", bst[:], ada_b[li:li + 1, :], writes=[bst.res])
            P.dma("sp", gst[:], norm1_g[li, :].partition_broadcast(128), writes=[gst.res])
            P.dma("sp", g2st[:], norm2_g[li, :].partition_broadcast(128), writes=[g2st.res])
            aw = ada_w[li].rearrange("(kc p) n -> p kc n", p=128)
            for pc in range(12):
                w = wst[pc % 2]
                P.dma("sp" if pc % 2 == 0 else "pool", w[:], aw[:, :, pc * 512:(pc + 1) * 512], writes=[w.res])
                j, half = pc // 2, pc % 2
                for which in range(2):
                    ps = next_ps()
                    for kc in range(8):
                        P.op("pe", lambda e, kc=kc, ps=ps, w=w, which=which: e.matmul(
                            ps[:], lrep[:, which * 8 + kc, :], w[:, kc, :], start=(kc == 0), stop=False),
                            reads=[lrep.res, w.res], writes=[ps.res])
                    P.op("pe", lambda e, ps=ps, pc=pc: e.matmul(
                        ps[:], ones_f[0:1, :], bst[0:1, pc * 512:(pc + 1) * 512], start=False, stop=True),
                        reads=[ones_f.res, bst.res], writes=[ps.res])
                    m = mods[which][j]
                    P.op("act", lambda e, m=m, ps=ps, half=half: e.copy(out=m[:, half * 512:(half + 1) * 512], in_=ps[:]),
                         reads=[ps.res], writes=[m.res])
            for which in range(2):
                for (j, g) in ((1, gst), (4, g2st)):
                    m = mods[which][j]
                    P.op("dve", lambda e, m=m, g=g: e.scalar_tensor_tensor(
                        out=m[:], in0=m[:], scalar=1.0, in1=g[:], op0=ALU.add, op1=ALU.mult),
                        reads=[m.res, g.res], writes=[m.res])
        P.barrier()
        if stop_after == "mods":
            dbg_t = nc.dram_tensor("dbg_mods", [12, 128, D], F32, kind="ExternalOutput").ap()
            for which in range(2):
                for j in range(6):
                    P.dma("sp", dbg_t[which * 6 + j], mods[which][j][:], reads=[mods[which][j].res])
            P.finish()
            return

        with contextlib.ExitStack() as stL:
            def make_loader(st_):
                uid[0] += 1
                stage = [sb("stage%d_%d" % (uid[0], i), [128, 1792], st=st_) for i in range(2)]
                stage_rr = [0]

                def load_cast(dst, src_fn, ncols, piece, nk=8, col0=0):
                    for c0 in range(0, ncols, piece):
                        c1 = min(ncols, c0 + piece)
                        stg = stage[stage_rr[0] % 2]
                        q = "sp" if stage_rr[0] % 2 == 0 else "act"
                        stage_rr[0] += 1
                        view = stg[:, 0:nk * (c1 - c0)].rearrange("p (k n) -> p k n", k=nk)
                        P.dma(q, view, src_fn(c0, c1), writes=[stg.res])
                        P.op("pool", lambda e, view=view, c0=c0, c1=c1: e.tensor_copy(out=dst[:, 0:nk, col0 + c0:col0 + c1], in_=view),
                             reads=[stg.res], writes=[dst.res])
                return load_cast

            eps_t = sb("eps_t", [128, 1], st=stL)
            P.op("pool", lambda e: e.memset(eps_t[:], EPS), writes=[eps_t.res])

            with contextlib.ExitStack() as stA:
                load_cast = make_loader(stA)
                KT = sb("KT", [128, T], BF16, st=stA)
                Vaug = sb("Vaug", [128, NT, 2, 65], BF16, st=stA)
                P.op("pool", lambda e: e.memset(Vaug[:, :, :, 64:65], 1.0), writes=[Vaug.res])
                with contextlib.ExitStack() as stB:
                    win_b = sb("win_b", [128, 8, IN_W], BF16, st=stB)
                    load_cast(win_b, lambda c0, c1: w_in[li].rearrange("(kc p) n -> p kc n", p=128)[:, :, c0:c1], IN_W, 224)
                    gain = sb("gain", [128, 128], st=stB)
                    P.dma("sp", gain[:], qk_gain[li, :].partition_broadcast(128), writes=[gain.res])
                    xt2 = [sb("xt%d" % i, [128, D], st=stB) for i in range(2)]
                    sq = sb("sq", [128, D], st=stB)
                    hf = sq
                    hb2 = [sb("hb%d" % i, [128, D], BF16, st=stB) for i in range(2)]
                    hTg2 = [sb("hTg%d" % i, [128, 8, 512], BF16, st=stB) for i in range(2)]
                    st10 = sb("st10", [128, 24], st=stB)
                    qkf = sb("qkf", [128, 10, 64], st=stB)
                    qkb2 = [sb("qkb%d" % i, [128, 10, 64], BF16, st=stB) for i in range(2)]
                    rt = [sb("rt%d" % i, [128, 10, 32], st=stB) for i in range(4)]
                    rp2 = [sb("rp%d" % i, [128, 64], st=stB) for i in range(2)]
                    pst3 = [sb("pst%d" % i, [128, 512], st=stB) for i in range(2)]
                    pst_rr = 0
                    for t in range(NT):
                        which = 0 if t < NLT else 1
                        M = mods[which]
                        grp, tin = t // 4, t % 4
                        xt, hb, hTg, qkb = xt2[t % 2], hb2[t % 2], hTg2[grp % 2], qkb2[t % 2]
                        if t == 0:
                            P.dma("sp", xt[:], xs[0:128, :], reads=[P.R("xs", 0)], writes=[xt.res])
                        if t + 1 < NT:
                            P.dma("sp", xt2[(t + 1) % 2][:], xs[(t + 1) * 128:(t + 2) * 128, :], reads=[P.R("xs", t + 1)],
                                  writes=[xt2[(t + 1) % 2].res])
                        P.op("act", lambda e, xt=xt: e.activation(out=sq[:], in_=xt[:], func=AF.Square),
                             reads=[xt.res], writes=[sq.res])
                        P.op("dve", lambda e: e.tensor_reduce(out=st10[:, 0:1], in_=sq[:], axis=AX.X, op=ALU.add),
                             reads=[sq.res], writes=[st10.res])
                        P.op("act", lambda e: e.activation(out=st10[:, 1:2], in_=st10[:, 0:1], func=AF.Sqrt,
                                                           bias=eps_t[:, 0:1], scale=1.0 / D),
                             reads=[st10.res, eps_t.res], writes=[st10.res])
                        P.op("dve", lambda e: e.reciprocal(out=st10[:, 1:2], in_=st10[:, 1:2]),
                             reads=[st10.res], writes=[st10.res])
                        P.op("dve", lambda e, xt=xt, M=M: e.scalar_tensor_tensor(
                            out=hf[:], in0=xt[:], scalar=st10[:, 1:2], in1=M[1][:], op0=ALU.mult, op1=ALU.mult),
                            reads=[xt.res, st10.res, M[1].res], writes=[hf.res])
                        P.op("pool", lambda e, hb=hb, M=M: e.tensor_tensor(out=hb[:], in0=hf[:], in1=M[0][:], op=ALU.add),
                             reads=[hf.res, M[0].res], writes=[hb.res])
                        psT = psum[0 + (t % 2)]
                        for kc in range(8):
                            P.op("pe", lambda e, kc=kc, psT=psT, hb=hb: e.transpose(
                                out=psT[:].bitcast(BF16)[:, kc * 128:(kc + 1) * 128], in_=hb[:, kc * 128:(kc + 1) * 128],
                                identity=ident_b[:]), reads=[hb.res, ident_b.res], writes=[psT.res])
                        P.op("act", lambda e, psT=psT, hTg=hTg, tin=tin: e.copy(
                            out=hTg[:, :, tin * 128:(tin + 1) * 128],
                            in_=psT[:].bitcast(BF16).rearrange("p (k n) -> p k n", k=8)),
                            reads=[psT.res], writes=[hTg.res])
                        psA, psB = psum[2], psum[3]
                        for kc in range(8):
                            P.op("pe", lambda e, kc=kc, hTg=hTg, tin=tin: e.matmul(
                                psA[:], hTg[:, kc, tin * 128:(tin + 1) * 128], win_b[:, kc, 0:512],
                                start=(kc == 0), stop=(kc == 7)), reads=[hTg.res, win_b.res], writes=[psA.res])
                        for kc in range(8):
                            P.op("pe", lambda e, kc=kc, hTg=hTg, tin=tin: e.matmul(
                                psB[:, 0:256], hTg[:, kc, tin * 128:(tin + 1) * 128], win_b[:, kc, 512:768],
                                start=(kc == 0), stop=(kc == 7)), reads=[hTg.res, win_b.res], writes=[psB.res])
                        P.op("act", lambda e: e.activation(out=sq[:, 0:512], in_=psA[:], func=AF.Square),
                             reads=[psA.res], writes=[sq.res])
                        P.op("act", lambda e: e.activation(out=sq[:, 512:640], in_=psB[:, 0:128], func=AF.Square),
                             reads=[psB.res], writes=[sq.res])
                        P.op("act", lambda e, t=t: e.copy(out=Vaug[:, t, :, 0:64],
                                                          in_=psB[:, 128:256].rearrange("p (g d) -> p g d", g=2)),
                             reads=[psB.res], writes=[Vaug.res])
                        P.op("dve", lambda e: e.tensor_reduce(
                            out=st10[:, 4:14], in_=sq[:, 0:640].rearrange("p (h d) -> p h d", h=10), axis=AX.X, op=ALU.add),
                            reads=[sq.res], writes=[st10.res])
                        P.op("act", lambda e: e.activation(out=st10[:, 14:24], in_=st10[:, 4:14], func=AF.Sqrt,
                                                           bias=eps_t[:, 0:1], scale=1.0 / HD),
                             reads=[st10.res, eps_t.res], writes=[st10.res])
                        P.op("dve", lambda e: e.reciprocal(out=st10[:, 14:24], in_=st10[:, 14:24]),
                             reads=[st10.res], writes=[st10.res])
                        P.op("dve", lambda e: e.tensor_tensor(
                            out=qkf[:, 0:8, :], in0=psA[:].rearrange("p (h d) -> p h d", h=8),
                            in1=st10[:, 14:22].unsqueeze(2).to_broadcast([128, 8, 64]), op=ALU.mult),
                            reads=[psA.res, st10.res], writes=[qkf.res])
                        P.op("dve", lambda e: e.tensor_tensor(
                            out=qkf[:, 8:10, :], in0=psB[:, 0:128].rearrange("p (h d) -> p h d", h=2),
                            in1=st10[:, 22:24].unsqueeze(2).to_broadcast([128, 2, 64]), op=ALU.mult),
                            reads=[psB.res, st10.res], writes=[qkf.res])
                        P.op("dve", lambda e: e.tensor_tensor(
                            out=qkf[:, 0:8, :], in0=qkf[:, 0:8, :],
                            in1=gain[:, 0:64].unsqueeze(1).to_broadcast([128, 8, 64]), op=ALU.mult),
                            reads=[qkf.res, gain.res], writes=[qkf.res])
                        P.op("dve", lambda e: e.tensor_tensor(
                            out=qkf[:, 8:10, :], in0=qkf[:, 8:10, :],
                            in1=gain[:, 64:128].unsqueeze(1).to_broadcast([128, 2, 64]), op=ALU.mult),
                            reads=[qkf.res, gain.res], writes=[qkf.res])
                        if t < NLT:
                            rp = rp2[t % 2]
                            P.dma("act", rp[:], rope[t * 128:(t + 1) * 128, :], writes=[rp.res])
                            cosb = rp[:, 0:32].unsqueeze(1).to_broadcast([128, 10, 32])
                            sinb = rp[:, 32:64].unsqueeze(1).to_broadcast([128, 10, 32])
                            x1, x2 = qkf[:, :, 0:32], qkf[:, :, 32:64]
                            P.op("dve", lambda e, cosb=cosb, x1=x1: e.tensor_tensor(out=rt[0][:], in0=x1, in1=cosb, op=ALU.mult),
                                 reads=[qkf.res, rp.res], writes=[rt[0].res])
                            P.op("dve", lambda e, sinb=sinb, x2=x2: e.tensor_tensor(out=rt[1][:], in0=x2, in1=sinb, op=ALU.mult),
                                 reads=[qkf.res, rp.res], writes=[rt[1].res])
                            P.op("dve", lambda e, qkb=qkb: e.tensor_tensor(out=qkb[:, :, 0:32], in0=rt[0][:], in1=rt[1][:], op=ALU.subtract),
                                 reads=[rt[0].res, rt[1].res], writes=[qkb.res])
                            P.op("pool", lambda e, sinb=sinb, x1=x1: e.tensor_tensor(out=rt[2][:], in0=x1, in1=sinb, op=ALU.mult),
                                 reads=[qkf.res, rp.res], writes=[rt[2].res])
                            P.op("pool", lambda e, cosb=cosb, x2=x2: e.tensor_tensor(out=rt[3][:], in0=x2, in1=cosb, op=ALU.mult),
                                 reads=[qkf.res, rp.res], writes=[rt[3].res])
                            P.op("pool", lambda e, qkb=qkb: e.tensor_tensor(out=qkb[:, :, 32:64], in0=rt[2][:], in1=rt[3][:], op=ALU.add),
                                 reads=[rt[2].res, rt[3].res], writes=[qkb.res])
                        else:
                            P.op("dve", lambda e, qkb=qkb: e.tensor_copy(out=qkb[:], in_=qkf[:]),
                                 reads=[qkf.res], writes=[qkb.res])
                        for g in range(2):
                            P.dma("act" if g == 0 else "sp",
                                  qs[t * 128:(t + 1) * 128, :].rearrange("p (j g d) -> p g j d", j=4, g=2)[:, g, :, :],
                                  qkb[:, 4 * g:4 * g + 4, :], reads=[qkb.res], writes=[P.R("qs", t)])
                        psK = psum[4 + (t % 2)]
                        P.op("pe", lambda e, psK=psK, qkb=qkb: e.transpose(
                            out=psK[:].bitcast(BF16)[:, 0:128], in_=qkb[:, 8:10, :].rearrange("p g d -> p (g d)"),
                            identity=ident_b[:]), reads=[qkb.res, ident_b.res], writes=[psK.res])
                        P.op("act", lambda e, psK=psK, t=t: e.copy(
                            out=KT[:, t * 128:(t + 1) * 128], in_=psK[:].bitcast(BF16)[:, 0:128]),
                            reads=[psK.res], writes=[KT.res])
                        if tin == 3 or t == NT - 1:
                            N = (tin + 1) * 128
                            g0 = grp * 512
                            for c in range(15):
                                ps = psum[6 + (c % 2)]
                                for kc in range(8):
                                    P.op("pe", lambda e, kc=kc, c=c, ps=ps, hTg=hTg, N=N: e.matmul(
                                        ps[:, 0:N], win_b[:, kc, ATT_IN + c * 128:ATT_IN + (c + 1) * 128], hTg[:, kc, 0:N],
                                        start=(kc == 0), stop=(kc == 7)), reads=[hTg.res, win_b.res], writes=[ps.res])
                                pst = pst3[pst_rr % 2]
                                pst_rr += 1
                                P.op("act" if c % 2 == 0 else "dve",
                                     (lambda e, ps=ps, pst=pst, N=N: e.copy(out=pst[:, 0:N], in_=ps[:, 0:N])) if c % 2 == 0 else
                                     (lambda e, ps=ps, pst=pst, N=N: e.tensor_copy(out=pst[:, 0:N], in_=ps[:, 0:N])),
                                     reads=[ps.res], writes=[pst.res])
                                P.dma("sp", pT[c * 128:(c + 1) * 128, g0:g0 + N], pst[:, 0:N],
                                      reads=[pst.res], writes=[P.R("pT", grp)])
                P.barrier()
                if stop_after == "phaseB":
                    dK = nc.dram_tensor("dbg_KT", [128, T], BF16, kind="ExternalOutput").ap()
                    dV = nc.dram_tensor("dbg_V", [128, NT, 2, 65], BF16, kind="ExternalOutput").ap()
                    P.dma("sp", dK, KT[:], reads=[KT.res])
                    P.dma("sp", dV, Vaug[:], reads=[Vaug.res])
                    P.finish()
                    return

                with contextlib.ExitStack() as stC:
                    wo_h = sb("wo_h", [64, 8, D], BF16, st=stC)
                    woh_v = w_out[li][0:512, :].rearrange("(h d) n -> d h n", d=64)
                    wstg2 = [sb("wstg2_%d" % i, [64, 8, 224], st=stC) for i in range(2)]
                    for pi, c0 in enumerate(range(0, D, 224)):
                        c1 = min(D, c0 + 224)
                        wsg = wstg2[pi % 2]
                        P.dma("sp" if pi % 2 == 0 else "act", wsg[:, :, 0:c1 - c0], woh_v[:, :, c0:c1], writes=[wsg.res])
                        P.op("pool", lambda e, wsg=wsg, c0=c0, c1=c1: e.tensor_copy(out=wo_h[:, :, c0:c1], in_=wsg[:, :, 0:c1 - c0]),
                             reads=[wsg.res], writes=[wo_h.res])
                    qt2 = [sb("qt%d" % i, [128, 512], BF16, st=stC) for i in range(2)]
                    qT2 = [sb("qT%d" % i, [128, 4, 128], BF16, st=stC) for i in range(2)]
                    Eb = [sb("Eb%d" % i, [128, 512], BF16, st=stC) for i in range(4)]
                    e_rr = 0
                    rcp = sb("rcp", [65, 512], st=stC)
                    nm2 = [sb("nm%d" % i, [64, 512], st=stC) for i in range(2)]
                    attT2 = [sb("attT%d" % i, [64, 8, 128], BF16, st=stC) for i in range(2)]
                    xa2 = [sb("xa%d" % i, [128, D], st=stC) for i in range(2)]
                    tmp = sb("tmpy", [128, D], st=stC)
                    s_rr = [0, 0]
                    for t in range(NT):
                        which = 0 if t < NLT else 1
                        M = mods[which]
                        qt, qT, attT, xa = qt2[t % 2], qT2[t % 2], attT2[t % 2], xa2[t % 2]
                        if t == 0:
                            P.dma("sp", qt[:], qs[0:128, :], reads=[P.R("qs", 0)], writes=[qt.res])
                        if t + 1 < NT:
                            P.dma("sp", qt2[(t + 1) % 2][:], qs[(t + 1) * 128:(t + 2) * 128, :], reads=[P.R("qs", t + 1)],
                                  writes=[qt2[(t + 1) % 2].res])
                        psQ = psum[7]
                        for jh in range(4):
                            P.op("pe", lambda e, jh=jh, qt=qt: e.transpose(
                                out=psQ[:].bitcast(BF16)[:, jh * 128:(jh + 1) * 128], in_=qt[:, jh * 128:(jh + 1) * 128],
                                identity=ident_b[:]), reads=[qt.res, ident_b.res], writes=[psQ.res])
                        P.op("dve", lambda e, qT=qT: e.tensor_copy(
                            out=qT[:], in_=psQ[:].bitcast(BF16)[:, 0:512].rearrange("p (j n) -> p j n", j=4)),
                            reads=[psQ.res], writes=[qT.res])
                        chunks = list(range(NT)) if t < NLT else [32, 33]

                        def score(sc_, g, qT=qT):
                            psS = psum[2 * g + s_rr[g] % 2]
                            s_rr[g] += 1
                            sl = slice(g * 64, (g + 1) * 64)
                            P.op("pe", lambda e, psS=psS, sl=sl, sc_=sc_, qT=qT: e.matmul(
                                psS[:], KT[sl, sc_ * 128:(sc_ + 1) * 128],
                                qT[sl, :, :].rearrange("p h n -> p (h n)"), start=True, stop=True),
                                reads=[KT.res, qT.res], writes=[psS.res])
                            return psS
                        pend = [[score(chunks[0], g)] for g in range(2)]
                        for ci, sc_ in enumerate(chunks):
                            cur = [pend[g].pop(0) for g in range(2)]
                            if ci + 1 < len(chunks):
                                for g in range(2):
                                    pend[g].append(score(chunks[ci + 1], g))
                            for g in range(2):
                                E = Eb[e_rr % 4]
                                e_rr += 1
                                psS, psO = cur[g], psum[4 + g]
                                P.op("act", lambda e, E=E, psS=psS: e.activation(out=E[:], in_=psS[:], func=AF.Exp, scale=0.125),
                                     reads=[psS.res], writes=[E.res])
                                P.op("pe", lambda e, E=E, psO=psO, sc_=sc_, g=g, ci=ci, n=len(chunks): e.matmul(
                                    psO[0:65, :], Vaug[:, sc_, g, :], E[:], start=(ci == 0), stop=(ci == n - 1)),
                                    reads=[E.res, Vaug.res], writes=[psO.res])
                        if last and t >= NLT:
                            continue
                        for g in range(2):
                            psO, nm = psum[4 + g], nm2[g]
                            P.op("dve", lambda e, psO=psO: e.reciprocal(out=rcp[64:65, :], in_=psO[64:65, :]),
                                 reads=[psO.res], writes=[rcp.res])
                            P.op("act", lambda e, psO=psO, nm=nm: e.copy(out=nm[:], in_=psO[0:64, :]), reads=[psO.res], writes=[nm.res])
                            psBc = psum[6]
                            P.op("pe", lambda e, psBc=psBc: e.matmul(psBc[0:64, :], ones_f[64:65, 0:64], rcp[64:65, :], start=True, stop=True),
                                 reads=[ones_f.res, rcp.res], writes=[psBc.res])
                            P.op("dve", lambda e, psBc=psBc, nm=nm, g=g, attT=attT: e.tensor_tensor(
                                out=attT[:, 4 * g:4 * g + 4, :].rearrange("p h n -> p (h n)"), in0=nm[:], in1=psBc[0:64, :], op=ALU.mult),
                                reads=[nm.res, psBc.res], writes=[attT.res])
                        if last and t >= NLT:
                            continue
                        P.dma("act", xa[:], xs[t * 128:(t + 1) * 128, :], reads=[P.R("xs", t)], writes=[xa.res])
                        for half in range(2):
                            psY = psum[6 + half]
                            for h in range(8):
                                P.op("pe", lambda e, h=h, half=half, psY=psY, attT=attT: e.matmul(
                                    psY[:], attT[:, h, :], wo_h[:, h, half * 512:(half + 1) * 512],
                                    start=(h == 0), stop=(h == 7)), reads=[attT.res, wo_h.res], writes=[psY.res])
                            P.op("dve", lambda e, half=half, psY=psY, M=M: e.tensor_tensor(
                                out=tmp[:, half * 512:(half + 1) * 512], in0=psY[:], in1=M[2][:, half * 512:(half + 1) * 512],
                                op=ALU.mult), reads=[psY.res, M[2].res], writes=[tmp.res])
                        P.op("pool", lambda e, xa=xa: e.tensor_tensor(out=xa[:], in0=xa[:], in1=tmp[:], op=ALU.add),
                             reads=[xa.res, tmp.res], writes=[xa.res])
                        P.dma("sp", xs[t * 128:(t + 1) * 128, :], xa[:], reads=[xa.res], writes=[P.R("xs", t)])
            P.barrier()
            if stop_after == "attn":
                P.finish()
                return

            NB = 256
            blocks = [(i * NB, i == 0, i == L // NB - 1) for i in range(L // NB)] + [(L, True, True)]
            GC = sb("GC", [128, 2, 4, NCH], st=stL)
            with contextlib.ExitStack() as stR:
                rwv_t = sb("rwv_t", [128, 43], st=stR)
                P.dma("sp", rwv_t[:], rwv[li], writes=[rwv_t.res])
                omka = sb("omka", [128, 8], st=stR)
                P.op("dve", lambda e: e.tensor_scalar(out=omka[:, 0:4], in0=rwv_t[:, 4:8], scalar1=-1.0, scalar2=1.0,
                                                      op0=ALU.mult, op1=ALU.add), reads=[rwv_t.res], writes=[omka.res])
                P.op("dve", lambda e: e.tensor_scalar(out=omka[:, 4:8], in0=rwv_t[:, 4:8], scalar1=-2.0, scalar2=2.0,
                                                      op0=ALU.mult, op1=ALU.add), reads=[rwv_t.res], writes=[omka.res])
                c12 = sb("c12", [128, 1], st=stR)
                P.op("pool", lambda e: e.memset(c12[:], 1e-12), writes=[c12.res])
                c128 = sb("c128", [128, 386], st=stR)
                P.dma("sp", c128[:], cst128[:, :], writes=[c128.res])
                c64 = sb("c64", [64, 2048], BF16, st=stR)
                P.dma("act", c64[:], cst64[:, :], writes=[c64.res])
                BDo, SEL, RST = c128[:, 0:128], c128[:, 128:130], c128[:, 130:386]
                selrk = sb("selrk", [128, 4, 2], st=stR)
                P.op("dve", lambda e: e.tensor_tensor(
                    out=selrk[:], in0=SEL.unsqueeze(1).to_broadcast([128, 4, 2]),
                    in1=rwv_t[:, 8:12].unsqueeze(2).to_broadcast([128, 4, 2]), op=ALU.mult),
                    reads=[c128.res, rwv_t.res], writes=[selrk.res])
                gst = [sb("gst%d" % i, [128, 512], st=stR) for i in range(1)]
                wstg = gst[0]
                lw3 = []
                for nm, src in (("w2b", rw_w2), ("a2b", rw_a2), ("g2b", rw_g2)):
                    wt = sb(nm, [128, 512], BF16, st=stR)
                    P.dma("sp", wstg[:], src[li], writes=[wstg.res])
                    P.op("pool", lambda e, wt=wt: e.tensor_copy(out=wt[:], in_=wstg[:]), reads=[wstg.res], writes=[wt.res])
                    lw3.append(wt)
                w2b, a2b, g2b = lw3
                PH = sb("PH", [128, 15, NB + 2], st=stR)
                S = sb("S", [128, 15, NB], st=stR)
                KKN = sb("KKN", [128, 4, NB], st=stR)
                LW = sb("LW", [128, 4, NB], st=stR)
                A2 = [sb("A%d" % d, [128, 4, NB], st=stR) for d in range(2)]
                KD = sb("KD", [128, 4, NB], st=stR)
                CU = sb("CU", [128, 4, NB], st=stR)
                EXC = sb("EXC", [128, 4, NB], st=stR)
                EP, EN, EX = (sb(n, [128, 4, NB], st=stR) for n in ("EP", "EN", "EX"))
                TW = sb("TW", [128, NB], BF16, st=stR)
                ADb = sb("ADb", [128, NB], BF16, st=stR)
                SG = sb("SG", [128, NB], BF16, st=stR)
                vb = sb("vb", [128, 4, NB], BF16, st=stR)
                FMs = sb("FMs", [128, 4, 4, 4, 64], BF16, st=stR)
                TMs = sb("TMs", [64, 4, 2, 512], BF16, st=stR)
                Vs = sb("Vs", [64, 4, 512], BF16, st=stR)
                BQm = [sb("BQm%d" % n, [64, 8, 2, 64], BF16, st=stR) for n in range(4)]
                KQm = [sb("KQm%d" % n, [64, 8, 2, 64], BF16, st=stR) for n in range(4)]
                Pm = [[sb("Pm%d_%d" % (n, i), [128, 4, 128], BF16, st=stR) for i in range(2)] for n in range(4)]
                PTm = [[sb("PTm%d_%d" % (n, i), [128, 4, 128], BF16, st=stR) for i in range(2)] for n in range(4)]
                WTm = [[sb("WTm%d_%d" % (n, i), [128, 4, 128], BF16, st=stR) for i in range(2)] for n in range(4)]
                QBD = sb("QBD", [128, 4, 4, 2, 64], BF16, st=stR)
                BBD = sb("BBD", [128, 4, 4, 2, 64], BF16, st=stR)
                P.op("pool", lambda e: e.memset(QBD[:], 0.0), writes=[QBD.res])
                P.op("pool", lambda e: e.memset(BBD[:], 0.0), writes=[BBD.res])
                cbd_t = sb("cbd_t", [128, 3, 512], BF16, st=stR)
                P.dma("sp", cbd_t[:], cstbd[:, :, :], writes=[cbd_t.res])
                bst_ = sb("bst_", [128, 16], st=stR)
                ev = [0]

                def evac(fn_act, fn_dve, reads, writes):
                    if ev[0] % 2 == 0:
                        P.op("act", fn_act, reads=reads, writes=writes)
                    else:
                        P.op("dve", fn_dve, reads=reads, writes=writes)
                    ev[0] += 1

                mskB = [c64[:, d * 512:(d + 1) * 512] for d in range(2)]
                mskK = [c64[:, 1024 + d * 512:1024 + (d + 1) * 512] for d in range(2)]
                pTv = pT.rearrange("(c p) t -> p c t", p=128)
                import os as _os
                CUT = int(_os.environ.get("R1A_CUT", "0"))
                cut_hit = [False]

                def cut(k):
                    if CUT == k:
                        cut_hit[0] = True
                    return cut_hit[0]
                for bi, (g0, s0, s1) in enumerate(blocks):
                    if cut_hit[0]:
                        break
                    ch0 = g0 // CH
                    grp_lo, grp_hi = max(g0 - 1, 0) // 512, min(g0 + NB, T - 1) // 512
                    rd = [P.R("pT", g) for g in range(grp_lo, grp_hi + 1)]
                    lo = g0 - (0 if s0 else 1)
                    hi = g0 + NB + (0 if s1 else 1)
                    off = 1 if s0 else 0
                    if s0:
                        P.op("pool", lambda e: e.memset(PH[:, :, 0:1], 0.0), writes=[PH.res])
                    if s1:
                        P.op("pool", lambda e: e.memset(PH[:, :, NB + 1:NB + 2], 0.0), writes=[PH.res])
                    for c3 in range(5):
                        P.dma("sp" if c3 % 2 == 0 else "act", PH[:, 3 * c3:3 * c3 + 3, off:off + hi - lo], pTv[:, 3 * c3:3 * c3 + 3, lo:hi],
                              reads=rd, writes=[PH.res])
                    pc_ = PH[:, :, 1:NB + 1]
                    P.op("pool", lambda e: e.tensor_tensor(out=S[:], in0=PH[:, :, 0:NB], in1=PH[:, :, 2:NB + 2], op=ALU.add),
                         reads=[PH.res], writes=[S.res])
                    P.op("dve", lambda e, pc_=pc_: e.scalar_tensor_tensor(out=S[:], in0=S[:], scalar=0.5, in1=pc_,
                                                                         op0=ALU.mult, op1=ALU.subtract),
                         reads=[S.res, PH.res], writes=[S.res])
                    P.op("pool", lambda e: e.tensor_tensor(out=S[:], in0=S[:],
                                                           in1=rwv_t[:, 28:43].unsqueeze(2).to_broadcast([128, 15, NB]), op=ALU.mult),
                         reads=[S.res, rwv_t.res], writes=[S.res])
                    P.op("dve", lambda e, pc_=pc_: e.tensor_tensor(out=S[:], in0=S[:], in1=pc_, op=ALU.add),
                         reads=[S.res, PH.res], writes=[S.res])
                    r_, k_, v_ = S[:, 0:4, :], S[:, 4:8, :], S[:, 8:12, :]
                    if cut(1):
                        break
                    for c in range(4):
                        P.op("dve", lambda e, c=c: e.tensor_scalar(out=KKN[:, c, :], in0=S[:, 4 + c, :], scalar1=rwv_t[:, c:c + 1],
                                                                   scalar2=None, op0=ALU.mult),
                             reads=[S.res, rwv_t.res], writes=[KKN.res])
                    P.op("act", lambda e: e.activation(out=EX[:], in_=KKN[:], func=AF.Square), reads=[KKN.res], writes=[EX.res])
                    for hf_ in range(2):
                        ps = next_ps()
                        for cc in range(2):
                            c = 2 * hf_ + cc
                            P.op("pe", lambda e, ps=ps, c=c, cc=cc: e.matmul(ps[:, cc * NB:(cc + 1) * NB], BDo, EX[:, c, :],
                                                                            start=True, stop=True),
                                 reads=[c128.res, EX.res], writes=[ps.res])
                        P.op("act", lambda e, ps=ps, hf_=hf_: e.activation(
                            out=EP[:, 2 * hf_:2 * hf_ + 2, :], in_=ps[:].rearrange("p (c n) -> p c n", c=2), func=AF.Sqrt,
                            bias=c12[:, 0:1], scale=1.0), reads=[ps.res, c12.res], writes=[EP.res])
                    P.op("dve", lambda e: e.reciprocal(out=EP[:], in_=EP[:]), reads=[EP.res], writes=[EP.res])
                    P.op("dve", lambda e: e.tensor_tensor(out=KKN[:], in0=KKN[:], in1=EP[:], op=ALU.mult),
                         reads=[KKN.res, EP.res], writes=[KKN.res])
                    if cut(2):
                        break
                    P.op("act", lambda e: e.activation(out=TW[:], in_=S[:, 12, :], func=AF.Tanh), reads=[S.res], writes=[TW.res])
                    P.op("act", lambda e: e.copy(out=ADb[:], in_=S[:, 13, :]), reads=[S.res], writes=[ADb.res])
                    P.op("act", lambda e: e.activation(out=SG[:], in_=S[:, 14, :], func=AF.Sigmoid), reads=[S.res], writes=[SG.res])
                    P.op("act", lambda e, v_=v_: e.copy(out=vb[:], in_=v_), reads=[S.res], writes=[vb.res])
                    for tt in range(NB // 128):
                        ps = next_ps()
                        P.op("pe", lambda e, ps=ps, tt=tt: e.matmul(ps[:], SG[:, tt * 128:(tt + 1) * 128], g2b[:], start=True, stop=True),
                             reads=[SG.res, g2b.res], writes=[ps.res])
                        gs = gst[0]
                        evac(lambda e, ps=ps, gs=gs: e.copy(out=gs[:], in_=ps[:]),
                             lambda e, ps=ps, gs=gs: e.tensor_copy(out=gs[:], in_=ps[:]), [ps.res], [gs.res])
                        P.dma("pool", gtm[g0 + tt * 128:g0 + (tt + 1) * 128, :], gs[:], reads=[gs.res],
                              writes=[P.R("gtm", (g0 + tt * 128) // 128)])
                    if cut(3):
                        break
                    for d in range(2):
                        A = A2[d]
                        for c in range(4):
                            ps = next_ps()
                            P.op("pe", lambda e, ps=ps, c=c, d=d: e.matmul(
                                ps[:, 0:NB], w2b[d * 64:(d + 1) * 64, c * 128:(c + 1) * 128], TW[d * 64:(d + 1) * 64, :],
                                start=True, stop=True), reads=[w2b.res, TW.res], writes=[ps.res])
                            P.op("pe", lambda e, ps=ps, c=c, d=d: e.matmul(
                                ps[:, NB:2 * NB], a2b[d * 64:(d + 1) * 64, c * 128:(c + 1) * 128], ADb[d * 64:(d + 1) * 64, :],
                                start=True, stop=True), reads=[a2b.res, ADb.res], writes=[ps.res])
                            P.op("act", lambda e, ps=ps, c=c, d=d: e.activation(
                                out=LW[:, c, :], in_=ps[:, 0:NB], func=AF.Sigmoid, bias=rwv_t[:, 12 + 4 * d + c:13 + 4 * d + c], scale=1.0),
                                reads=[ps.res, rwv_t.res], writes=[LW.res])
                            P.op("act", lambda e, ps=ps, c=c, d=d, A=A: e.activation(
                                out=A[:, c, :], in_=ps[:, NB:2 * NB], func=AF.Sigmoid, bias=rwv_t[:, 20 + 4 * d + c:21 + 4 * d + c], scale=1.0),
                                reads=[ps.res, rwv_t.res], writes=[A.res])
                            P.op("dve", lambda e, c=c, A=A: e.tensor_scalar(
                                out=KD[:, c, :], in0=A[:, c, :], scalar1=rwv_t[:, 4 + c:5 + c], scalar2=omka[:, c:c + 1],
                                op0=ALU.mult, op1=ALU.add), reads=[A.res, rwv_t.res, omka.res], writes=[KD.res])
                            P.op("dve", lambda e, c=c: e.tensor_tensor_scan(
                                out=CU[:, c, :], data0=RST, data1=LW[:, c, :], initial=0.0, op0=ALU.mult, op1=ALU.add),
                                reads=[c128.res, LW.res], writes=[CU.res])
                        P.op("pool", lambda e, k_=k_: e.tensor_tensor(out=KD[:], in0=KD[:], in1=k_, op=ALU.mult),
                             reads=[KD.res, S.res], writes=[KD.res])
                        cu4 = CU[:].rearrange("p c (n t) -> p c n t", t=CH)
                        if cut(4):
                            break
                        if d == 0:
                            P.op("pool", lambda e: e.tensor_tensor(out=EXC[:], in0=CU[:], in1=LW[:], op=ALU.subtract),
                                 reads=[CU.res, LW.res], writes=[EXC.res])
                            INC = CU
                        else:
                            P.op("pool", lambda e, cu4=cu4: e.tensor_tensor(
                                out=EXC[:].rearrange("p c (n t) -> p c n t", t=CH),
                                in0=cu4[:, :, :, CH - 1:CH].to_broadcast([128, 4, NB // CH, CH]), in1=cu4, op=ALU.subtract),
                                reads=[CU.res], writes=[EXC.res])
                            INC = None
                        P.op("act", lambda e, cu4=cu4, d=d, ch0=ch0: e.activation(
                            out=GC[:, d, :, ch0:ch0 + NB // CH], in_=cu4[:, :, :, CH - 1], func=AF.Exp, scale=-DECAY_C),
                            reads=[CU.res], writes=[GC.res])
                        if INC is None:
                            P.op("pool", lambda e: e.tensor_tensor(out=CU[:], in0=EXC[:], in1=LW[:], op=ALU.add),
                                 reads=[EXC.res, LW.res], writes=[CU.res])
                            INC = CU
                        P.op("act", lambda e, INC=INC: e.activation(out=EP[:], in_=INC[:], func=AF.Exp, scale=-DECAY_C),
                             reads=[INC.res], writes=[EP.res])
                        P.op("act", lambda e, INC=INC: e.activation(out=EN[:], in_=INC[:], func=AF.Exp, scale=DECAY_C),
                             reads=[INC.res], writes=[EN.res])
                        P.op("act", lambda e: e.activation(out=EX[:], in_=EXC[:], func=AF.Exp, scale=-DECAY_C),
                             reads=[EXC.res], writes=[EX.res])

                        def fm_out(arr):
                            return FMs[:, :, arr, :, :].rearrange("p n c t -> p c n t")

                        def v4(tl):
                            return tl[:].rearrange("p c (n t) -> p c n t", t=CH)
                        P.op("pool", lambda e: e.tensor_tensor(out=fm_out(0), in0=v4(KKN), in1=v4(EX), op=ALU.mult),
                             reads=[KKN.res, EX.res], writes=[FMs.res])
                        P.op("dve", lambda e, r_=r_: e.tensor_tensor(out=fm_out(1), in0=r_.rearrange("p c (n t) -> p c n t", t=CH),
                                                                     in1=v4(EP), op=ALU.mult),
                             reads=[S.res, EP.res], writes=[FMs.res])
                        P.op("pool", lambda e: e.tensor_tensor(out=fm_out(2), in0=v4(KD), in1=v4(EN), op=ALU.mult),
                             reads=[KD.res, EN.res], writes=[FMs.res])
                        P.op("pool", lambda e, A=A: e.tensor_tensor(out=EP[:], in0=KKN[:], in1=A[:], op=ALU.mult),
                             reads=[KKN.res, A.res], writes=[EP.res])
                        P.op("dve", lambda e: e.tensor_tensor(out=fm_out(3), in0=v4(EP), in1=v4(EN), op=ALU.mult),
                             reads=[EP.res, EN.res], writes=[FMs.res])
                        for hh in range(2):
                            P.dma("sp" if hh == 0 else "act", FM[d][ch0:ch0 + 4][:, :, hh, :].rearrange("n k f -> k n f"),
                                  FMs[hh * 64:(hh + 1) * 64, :, 0:2, :, :].rearrange("p n a c t -> p n (a c t)"),
                                  reads=[FMs.res], writes=[P.R("FM", d, bi)])
                        if cut(5):
                            break
                        for n in range(4):
                            for a in range(2):
                                ps = next_ps()
                                for c in range(4):
                                    P.op("pe", lambda e, ps=ps, n=n, a=a, c=c: e.transpose(
                                        out=ps[:].bitcast(BF16)[0:64, c * 128:(c + 1) * 128], in_=FMs[:, n, 2 + a, c, :],
                                        identity=ident_b[:]), reads=[FMs.res, ident_b.res], writes=[ps.res])
                                sgn = 1.0 if a == 0 else -1.0
                                evac(lambda e, ps=ps, n=n, a=a, sgn=sgn: e.mul(out=TMs[:, n, a, :], in_=ps[:].bitcast(BF16)[0:64, 0:512], mul=sgn),
                                     lambda e, ps=ps, n=n, a=a, sgn=sgn: e.tensor_scalar(out=TMs[:, n, a, :], in0=ps[:].bitcast(BF16)[0:64, 0:512],
                                                                                           scalar1=sgn, scalar2=None, op0=ALU.mult),
                                     [ps.res], [TMs.res])
                            if d == 0:
                                ps = next_ps()
                                for c in range(4):
                                    P.op("pe", lambda e, ps=ps, n=n, c=c: e.transpose(
                                        out=ps[:].bitcast(BF16)[0:64, c * 128:(c + 1) * 128], in_=vb[:, c, n * CH:(n + 1) * CH],
                                        identity=ident_b[:]), reads=[vb.res, ident_b.res], writes=[ps.res])
                                evac(lambda e, ps=ps, n=n: e.copy(out=Vs[:, n, :], in_=ps[:].bitcast(BF16)[0:64, 0:512]),
                                     lambda e, ps=ps, n=n: e.tensor_copy(out=Vs[:, n, :], in_=ps[:].bitcast(BF16)[0:64, 0:512]),
                                     [ps.res], [Vs.res])
                        if _os.environ.get("NO_TMDMA") is None:
                            P.dma("act", TM[d][ch0:ch0 + 4].rearrange("n p f -> p n f"), TMs[:].rearrange("p n a f -> p n (a f)"),
                                  reads=[TMs.res], writes=[P.R("TM", d, bi)])
                            if d == 0:
                                P.dma("sp", VM[ch0:ch0 + 4].rearrange("n p f -> p n f"), Vs[:], reads=[Vs.res], writes=[P.R("VM", bi)])
                        if cut(6):
                            break
                        for hh in range(2):
                            sl = slice(hh * 64, (hh + 1) * 64)
                            P.op("pool", lambda e, sl=sl, hh=hh: e.tensor_copy(out=QBD[sl, :, :, hh, :], in_=FMs[sl, :, 0, :, :]),
                                 reads=[FMs.res], writes=[QBD.res])
                            P.op("dve", lambda e, sl=sl, hh=hh: e.tensor_copy(out=BBD[sl, :, :, hh, :], in_=FMs[sl, :, 3, :, :]),
                                 reads=[FMs.res], writes=[BBD.res])
                        for n in range(4):
                            for hh in range(2):
                                sl = slice(hh * 64, (hh + 1) * 64)
                                psb, psk = next_ps(), next_ps()
                                for c in range(4):
                                    P.op("pe", lambda e, psb=psb, n=n, c=c, sl=sl: e.matmul(
                                        psb[0:64, c * 128:(c + 1) * 128], FMs[sl, n, 3, c, :], FMs[sl, n, 0:2, c, :],
                                        start=True, stop=True), reads=[FMs.res], writes=[psb.res])
                                    P.op("pe", lambda e, psk=psk, n=n, c=c, sl=sl: e.matmul(
                                        psk[0:64, c * 128:(c + 1) * 128], FMs[sl, n, 2, c, :], FMs[sl, n, 0:2, c, :],
                                        start=True, stop=True), reads=[FMs.res], writes=[psk.res])
                                bq = BQm[n][:].rearrange("p (c hh) a t -> p c hh a t", hh=2)[:, :, hh, :, :]
                                kq = KQm[n][:].rearrange("p (c hh) a t -> p c hh a t", hh=2)[:, :, hh, :, :]
                                P.op("dve", lambda e, psb=psb, bq=bq, d=d: e.tensor_tensor(
                                    out=bq, in0=psb[0:64, :].rearrange("p (c a t) -> p c a t", c=4, a=2),
                                    in1=mskB[d].rearrange("p (c a t) -> p c a t", c=4, a=2), op=ALU.mult),
                                    reads=[psb.res, c64.res], writes=[BQm[n].res])
                                P.op("dve", lambda e, psk=psk, kq=kq, d=d: e.tensor_tensor(
                                    out=kq, in0=psk[0:64, :].rearrange("p (c a t) -> p c a t", c=4, a=2),
                                    in1=mskK[d].rearrange("p (c a t) -> p c a t", c=4, a=2), op=ALU.mult),
                                    reads=[psk.res, c64.res], writes=[KQm[n].res])
                            psnt, psn = next_ps(), next_ps()
                            for c in range(4):
                                qb = QBD[:, n, c, :, :].rearrange("p a t -> p (a t)")
                                bb = BBD[:, n, c, :, :].rearrange("p a t -> p (a t)")
                                P.op("pe", lambda e, psnt=psnt, c=c, qb=qb, bb=bb: e.matmul(
                                    psnt[:, c * 128:(c + 1) * 128], bb, qb, start=True, stop=True),
                                    reads=[QBD.res, BBD.res], writes=[psnt.res])
                                P.op("pe", lambda e, psn=psn, c=c, qb=qb, bb=bb: e.matmul(
                                    psn[:, c * 128:(c + 1) * 128], qb, bb, start=True, stop=True),
                                    reads=[QBD.res, BBD.res], writes=[psn.res])
                            P.op("dve", lambda e, psnt=psnt, n=n, d=d: e.tensor_tensor(
                                out=PTm[n][0][:].rearrange("p c t -> p (c t)"), in0=psnt[:], in1=cbd_t[:, d, :], op=ALU.mult),
                                reads=[psnt.res, cbd_t.res], writes=[PTm[n][0].res])
                            P.op("dve", lambda e, psn=psn, n=n, d=d: e.tensor_tensor(
                                out=Pm[n][0][:].rearrange("p c t -> p (c t)"), in0=psn[:], in1=cbd_t[:, 1 - d, :], op=ALU.mult),
                                reads=[psn.res, cbd_t.res], writes=[Pm[n][0].res])
                            P.op("pool", lambda e, n=n: e.tensor_tensor(
                                out=WTm[n][0][:].rearrange("p c t -> p (c t)"), in0=cbd_t[:, 2, :],
                                in1=PTm[n][0][:].rearrange("p c t -> p (c t)"), op=ALU.subtract),
                                reads=[cbd_t.res, PTm[n][0].res], writes=[WTm[n][0].res])
                        if cut(7):
                            break
                        for j in range(1, 6):
                            cur, prv = j % 2, (j - 1) % 2
                            for n in range(4):
                                Pp, PTp = Pm[n][prv], PTm[n][prv]
                                ps = next_ps()
                                for c in range(4):
                                    P.op("pe", lambda e, ps=ps, c=c, Pp=Pp, PTp=PTp: e.matmul(
                                        ps[:, c * 128:(c + 1) * 128], PTp[:, c, :], Pp[:, c, :], start=True, stop=True),
                                        reads=[Pp.res, PTp.res], writes=[ps.res])
                                Pn = Pm[n][cur]
                                evac(lambda e, ps=ps, Pn=Pn: e.copy(out=Pn[:].rearrange("p c t -> p (c t)"), in_=ps[:]),
                                     lambda e, ps=ps, Pn=Pn: e.tensor_copy(out=Pn[:].rearrange("p c t -> p (c t)"), in_=ps[:]),
                                     [ps.res], [Pn.res])
                                if j < 5:
                                    ps2 = next_ps()
                                    for c in range(4):
                                        P.op("pe", lambda e, ps2=ps2, c=c, Pp=Pp, PTp=PTp: e.matmul(
                                            ps2[:, c * 128:(c + 1) * 128], Pp[:, c, :], PTp[:, c, :], start=True, stop=True),
                                            reads=[Pp.res, PTp.res], writes=[ps2.res])
                                    PTn = PTm[n][cur]
                                    evac(lambda e, ps2=ps2, PTn=PTn: e.copy(out=PTn[:].rearrange("p c t -> p (c t)"), in_=ps2[:]),
                                         lambda e, ps2=ps2, PTn=PTn: e.tensor_copy(out=PTn[:].rearrange("p c t -> p (c t)"), in_=ps2[:]),
                                         [ps2.res], [PTn.res])
                            for n in range(4):
                                Pn, Wp, Wn = Pm[n][cur], WTm[n][prv], WTm[n][cur]
                                ps = next_ps()
                                for c in range(4):
                                    P.op("pe", lambda e, ps=ps, c=c, Pn=Pn, Wp=Wp: e.matmul(
                                        ps[:, c * 128:(c + 1) * 128], Pn[:, c, :], Wp[:, c, :], start=True, stop=True),
                                        reads=[Pn.res, Wp.res], writes=[ps.res])
                                P.op("dve", lambda e, ps=ps, Wp=Wp, Wn=Wn: e.tensor_tensor(
                                    out=Wn[:].rearrange("p c t -> p (c t)"), in0=ps[:], in1=Wp[:].rearrange("p c t -> p (c t)"),
                                    op=ALU.add), reads=[ps.res, Wp.res], writes=[Wn.res])
                        for n in range(4):
                            Wf = WTm[n][5 % 2]
                            res_ = P.R("MAT", d, ch0 + n)
                            wdst = MAT[d][ch0 + n][:, 0:512].rearrange("s (c hh t) -> s c hh t", c=4, hh=2)
                            for hh in range(2):
                                P.dma("sp" if hh == 0 else "act", wdst[:, :, hh, :], Wf[hh * 64:(hh + 1) * 64, :, hh * 64:(hh + 1) * 64],
                                      reads=[Wf.res], writes=[res_])
                            P.dma("act", MAT[d][ch0 + n][:, 512:1536], KQm[n][:].rearrange("p h a t -> p (h a t)"),
                                  reads=[KQm[n].res], writes=[res_])
                            P.dma("sp", MAT[d][ch0 + n][:, 1536:2560], BQm[n][:].rearrange("p h a t -> p (h a t)"),
                                  reads=[BQm[n].res], writes=[res_])
                        if cut(8):
                            break
                    if cut_hit[0]:
                        break
                    P.op("pool", lambda e: e.tensor_tensor(out=EXC[:], in0=A2[0][:], in1=A2[1][:], op=ALU.add),
                         reads=[A2[0].res, A2[1].res], writes=[EXC.res])
                    for c in range(4):
                        P.op("dve", lambda e, c=c: e.tensor_scalar(
                            out=EXC[:, c, :], in0=EXC[:, c, :], scalar1=rwv_t[:, 4 + c:5 + c], scalar2=omka[:, 4 + c:5 + c],
                            op0=ALU.mult, op1=ALU.add), reads=[EXC.res, rwv_t.res, omka.res], writes=[EXC.res])
                    P.op("pool", lambda e, k_=k_: e.tensor_tensor(out=EXC[:], in0=EXC[:], in1=k_, op=ALU.mult),
                         reads=[EXC.res, S.res], writes=[EXC.res])
                    P.op("dve", lambda e, r_=r_: e.tensor_tensor(out=EXC[:], in0=EXC[:], in1=r_, op=ALU.mult),
                         reads=[EXC.res, S.res], writes=[EXC.res])
                    ps = next_ps()
                    for tt in range(NB // 128):
                        for c in range(4):
                            P.op("pe", lambda e, ps=ps, tt=tt, c=c: e.matmul(
                                ps[:, tt * 8 + 2 * c:tt * 8 + 2 * c + 2], EXC[:, c, tt * 128:(tt + 1) * 128], selrk[:, c, :],
                                start=True, stop=True), reads=[EXC.res, selrk.res], writes=[ps.res])
                    P.op("dve", lambda e, ps=ps: e.tensor_copy(out=bst_[:], in_=ps[:, 0:16]), reads=[ps.res], writes=[bst_.res])
                    for tt in range(NB // 128):
                        P.dma("sp", bon[g0 + tt * 128:g0 + (tt + 1) * 128, :], bst_[:, tt * 8:(tt + 1) * 8], reads=[bst_.res],
                              writes=[P.R("bon", (g0 + tt * 128) // 128)])
                    if cut(9):
                        break
            P.barrier()
            if stop_after == "r1a":
                dG = nc.dram_tensor("dbg_GC", [128, 2, 4, NCH], F32, kind="ExternalOutput").ap()
                P.dma("sp", dG, GC[:], reads=[GC.res])
                P.finish()
                return

            GCd = dscr("GCd%d" % li, [128, 2, 4, NCH])
            P.dma("sp", GCd, GC[:], reads=[GC.res], writes=[P.R("GCd")])
            with contextlib.ExitStack() as stS:
                GC2 = sb("GC2", [64, 2, 4, 2, NCH], st=stS)
                for hh in range(2):
                    P.dma("sp" if hh == 0 else "act", GC2[:, :, :, hh, :], GCd[hh * 64:(hh + 1) * 64], reads=[P.R("GCd")], writes=[GC2.res])
                Zf = [sb("Zf%d" % d, [64, 8, 64], st=stS) for d in range(2)]
                Zb = [sb("Zb%d" % d, [64, 8, 64], BF16, st=stS) for d in range(2)]
                ztmp = sb("ztmp", [64, 8, 64], st=stS)
                for d in range(2):
                    P.op("pool", lambda e, d=d: e.memset(Zf[d][:], 0.0), writes=[Zf[d].res])
                    P.op("pool", lambda e, d=d: e.memset(Zb[d][:], 0.0), writes=[Zb[d].res])
                FMc = [[sb("FMc%d_%d" % (d, i), [64, 2, 512], BF16, st=stS) for i in range(2)] for d in range(2)]
                TMc = [[sb("TMc%d_%d" % (d, i), [64, 1024], BF16, st=stS) for i in range(2)] for d in range(2)]
                Vc = [[sb("Vc%d_%d" % (d, i), [64, 512], BF16, st=stS) for i in range(2)] for d in range(2)]
                MATc = [[sb("MATc%d_%d" % (d, i), [64, 2560], BF16, st=stS) for i in range(2)] for d in range(2)]
                Xs = [sb("Xs%d" % d, [64, 512], BF16, st=stS) for d in range(2)]
                Us = [sb("Us%d" % d, [64, 512], BF16, st=stS) for d in range(2)]
                Os = [[sb("Os%d_%d" % (d, i), [64, 512], st=stS) for i in range(2)] for d in range(2)]
                order = [list(range(64, 68)) + list(range(0, 64)), list(range(67, 63, -1)) + list(range(63, -1, -1))]
                def scan_load(step_, d_):
                    ch_ = order[d_][step_]
                    i_ = step_ % 2
                    b_ = ch_ // 4
                    P.dma("sp", FMc[d_][i_][:], FM[d_][ch_], reads=[P.R("FM", d_, b_)], writes=[FMc[d_][i_].res])
                    P.dma("act", TMc[d_][i_][:], TM[d_][ch_], reads=[P.R("TM", d_, b_)], writes=[TMc[d_][i_].res])
                    P.dma("act", Vc[d_][i_][:], VM[ch_], reads=[P.R("VM", b_)], writes=[Vc[d_][i_].res])
                    P.dma("sp", MATc[d_][i_][:], MAT[d_][ch_], reads=[P.R("MAT", d_, ch_)], writes=[MATc[d_][i_].res])
                scan_load(0, 0)
                scan_load(0, 1)
                for step in range(NCH):
                    for d in range(2):
                        ch = order[d][step]
                        i2 = step % 2
                        bi = ch // 4
                        fm, tm, vc, mat, osb = FMc[d][i2], TMc[d][i2], Vc[d][i2], MATc[d][i2], Os[d][i2]
                        if step + 1 < NCH:
                            scan_load(step + 1, d)
                        psX, psU, psO, psZ = psum[4 * d], psum[4 * d + 1], psum[4 * d + 2], psum[4 * d + 3]
                        zb, zf, xs_, us_ = Zb[d], Zf[d], Xs[d], Us[d]
                        for h in range(8):
                            c, hh = h // 2, h % 2
                            P.op("pe", lambda e, h=h, c=c, hh=hh, fm=fm, zb=zb, psX=psX: e.matmul(
                                psX[0:64, h * 64:(h + 1) * 64], fm[:, hh, c * 64:(c + 1) * 64], zb[:, h, :],
                                start=(h == 0), stop=False), reads=[fm.res, zb.res], writes=[psX.res])
                            P.op("pe", lambda e, h=h, mat=mat, vc=vc, psX=psX: e.matmul(
                                psX[0:64, h * 64:(h + 1) * 64], mat[:, 512 + h * 128:512 + h * 128 + 64], vc[:, h * 64:(h + 1) * 64],
                                start=False, stop=(h == 7)), reads=[mat.res, vc.res], writes=[psX.res])
                        P.op("act", lambda e, xs_=xs_, psX=psX: e.copy(out=xs_[:], in_=psX[0:64, :]), reads=[psX.res], writes=[xs_.res])
                        for h in range(8):
                            P.op("pe", lambda e, h=h, mat=mat, xs_=xs_, psU=psU: e.matmul(
                                psU[0:64, h * 64:(h + 1) * 64], mat[:, h * 64:(h + 1) * 64], xs_[:, h * 64:(h + 1) * 64],
                                start=(h == 0), stop=(h == 7)), reads=[mat.res, xs_.res], writes=[psU.res])
                        P.op("dve", lambda e, us_=us_, psU=psU: e.tensor_copy(out=us_[:], in_=psU[0:64, :]), reads=[psU.res], writes=[us_.res])
                        for h in range(8):
                            c, hh = h // 2, h % 2
                            P.op("pe", lambda e, h=h, c=c, hh=hh, fm=fm, zb=zb, psO=psO: e.matmul(
                                psO[0:64, h * 64:(h + 1) * 64], fm[:, hh, 256 + c * 64:256 + (c + 1) * 64], zb[:, h, :],
                                start=(h == 0), stop=False), reads=[fm.res, zb.res], writes=[psO.res])
                            P.op("pe", lambda e, h=h, mat=mat, vc=vc, psO=psO: e.matmul(
                                psO[0:64, h * 64:(h + 1) * 64], mat[:, 512 + h * 128 + 64:512 + h * 128 + 128], vc[:, h * 64:(h + 1) * 64],
                                start=False, stop=False), reads=[mat.res, vc.res], writes=[psO.res])
                            P.op("pe", lambda e, h=h, mat=mat, us_=us_, psO=psO: e.matmul(
                                psO[0:64, h * 64:(h + 1) * 64], mat[:, 1536 + h * 128 + 64:1536 + h * 128 + 128], us_[:, h * 64:(h + 1) * 64],
                                start=False, stop=(h == 7)), reads=[mat.res, us_.res], writes=[psO.res])
                        P.op("act", lambda e, osb=osb, psO=psO: e.copy(out=osb[:], in_=psO[0:64, :]), reads=[psO.res], writes=[osb.res])
                        P.dma("act", Od[d][ch * CH:(ch + 1) * CH, :], osb[:], reads=[osb.res], writes=[P.R("Od", d, ch)])
                        for h in range(8):
                            P.op("pe", lambda e, h=h, tm=tm, vc=vc, psZ=psZ: e.matmul(
                                psZ[0:64, h * 64:(h + 1) * 64], tm[:, h * 64:(h + 1) * 64], vc[:, h * 64:(h + 1) * 64],
                                start=(h == 0), stop=False), reads=[tm.res, vc.res], writes=[psZ.res])
                            P.op("pe", lambda e, h=h, tm=tm, us_=us_, psZ=psZ: e.matmul(
                                psZ[0:64, h * 64:(h + 1) * 64], tm[:, 512 + h * 64:512 + (h + 1) * 64], us_[:, h * 64:(h + 1) * 64],
                                start=False, stop=(h == 7)), reads=[tm.res, us_.res], writes=[psZ.res])
                        P.op("dve", lambda e, psZ=psZ, zf=zf: e.tensor_tensor(
                            out=ztmp[:], in0=psZ[0:64, :].rearrange("p (h v) -> p h v", h=8), in1=zf[:], op=ALU.add),
                            reads=[psZ.res, zf.res], writes=[ztmp.res])
                        gcv = GC2[:, d, :, :, ch:ch + 1].rearrange("p c hh o -> p (c hh) o").to_broadcast([64, 8, 64])
                        P.op("dve", lambda e, zf=zf, gcv=gcv: e.tensor_tensor(out=zf[:], in0=ztmp[:], in1=gcv, op=ALU.mult),
                             reads=[ztmp.res, GC2.res], writes=[zf.res])
                        P.op("act", lambda e, zb=zb, zf=zf: e.copy(out=zb[:], in_=zf[:]), reads=[zf.res], writes=[zb.res])
            P.barrier()
            if stop_after == "scan":
                P.finish()
                return

            with contextlib.ExitStack() as stO:
                load_cast = make_loader(stO)
                wo_b = sb("wo_b2", [128, 8, D], BF16, st=stO)
                load_cast(wo_b, lambda c0, c1: w_out[li].rearrange("(kc p) n -> p kc n", p=128)[:, :, c0:c1], D, 224)
                gnw = sb("gnw", [128, 2, 512], st=stO)
                P.dma("sp", gnw[:].rearrange("p a f -> p (a f)"), gn_wb[li].rearrange("a f -> (a f)").partition_broadcast(128),
                      writes=[gnw.res])
                gne = sb("gne", [128, 1], st=stO)
                P.op("pool", lambda e: e.memset(gne[:], GN_EPS), writes=[gne.res])
                o0 = [sb("o0_%d" % i, [128, 8, 64], st=stO) for i in range(2)]
                o1 = [sb("o1_%d" % i, [128, 8, 64], st=stO) for i in range(2)]
                vt2 = [sb("vt%d" % i, [128, 8, 64], BF16, st=stO) for i in range(2)]
                gt2 = [sb("gt%d" % i, [128, 512], st=stO) for i in range(2)]
                bt2 = [sb("bt%d" % i, [128, 8], st=stO) for i in range(2)]
                xr2 = [sb("xr%d" % i, [128, D], st=stO) for i in range(2)]
                sqr = sb("sqr", [128, 8, 64], st=stO)
                tb = sb("tb", [128, 8, 64], st=stO)
                stt = sb("stt", [128, 32], st=stO)
                rwb2 = [sb("rwb%d" % i, [128, 512], BF16, st=stO) for i in range(2)]
                rwT2 = [sb("rwT%d" % i, [128, 4, 128], BF16, st=stO) for i in range(2)]
                tmpy = sb("tmpy2", [128, D], st=stO)
                VMf = VM.rearrange("n p f -> (n p) f")
                for t in range(NLT if last else NT):
                    which = 0 if t < NLT else 1
                    M = mods[which]
                    a0, a1, vt, gt, bt, xr, rwb, rwT = o0[t % 2], o1[t % 2], vt2[t % 2], gt2[t % 2], bt2[t % 2], xr2[t % 2], rwb2[t % 2], rwT2[t % 2]
                    rows = slice(t * 128, (t + 1) * 128)

                    def ro_load(t_):
                        r_ = slice(t_ * 128, (t_ + 1) * 128)
                        i_ = t_ % 2
                        P.dma("sp", o0[i_][:].rearrange("p h v -> p (h v)"), Od[0][r_, :], reads=[P.R("Od", 0, 2 * t_), P.R("Od", 0, 2 * t_ + 1)], writes=[o0[i_].res])
                        P.dma("act", o1[i_][:].rearrange("p h v -> p (h v)"), Od[1][r_, :], reads=[P.R("Od", 1, 2 * t_), P.R("Od", 1, 2 * t_ + 1)], writes=[o1[i_].res])
                        P.dma("sp", vt2[i_][:].rearrange("p h v -> p (h v)"), VMf[r_, :], reads=[P.R("VM", (2 * t_) // 4)], writes=[vt2[i_].res])
                        P.dma("act", gt2[i_][:], gtm[r_, :], reads=[P.R("gtm", t_)], writes=[gt2[i_].res])
                        P.dma("sp", bt2[i_][:], bon[r_, :], reads=[P.R("bon", t_)], writes=[bt2[i_].res])
                        P.dma("act", xr2[i_][:], xs[r_, :], reads=[P.R("xs", t_)], writes=[xr2[i_].res])
                    if t == 0:
                        ro_load(0)
                    if t + 1 < (NLT if last else NT):
                        ro_load(t + 1)
                    P.op("pool", lambda e, a0=a0, a1=a1: e.tensor_tensor(out=a0[:], in0=a0[:], in1=a1[:], op=ALU.add),
                         reads=[a0.res, a1.res], writes=[a0.res])
                    P.op("dve", lambda e, a0=a0: e.tensor_reduce(out=stt[:, 0:8], in_=a0[:], axis=AX.X, op=ALU.add),
                         reads=[a0.res], writes=[stt.res])
                    P.op("dve", lambda e: e.tensor_scalar(out=stt[:, 8:16], in0=stt[:, 0:8], scalar1=1.0 / HD, scalar2=None, op0=ALU.mult),
                         reads=[stt.res], writes=[stt.res])
                    P.op("dve", lambda e, a0=a0: e.tensor_tensor(out=a0[:], in0=a0[:], in1=stt[:, 8:16].unsqueeze(2).to_broadcast([128, 8, 64]),
                                                                  op=ALU.subtract), reads=[a0.res, stt.res], writes=[a0.res])
                    P.op("act", lambda e, a0=a0: e.activation(out=sqr[:], in_=a0[:], func=AF.Square), reads=[a0.res], writes=[sqr.res])
                    P.op("dve", lambda e: e.tensor_reduce(out=stt[:, 16:24], in_=sqr[:], axis=AX.X, op=ALU.add),
                         reads=[sqr.res], writes=[stt.res])
                    P.op("act", lambda e: e.activation(out=stt[:, 24:32], in_=stt[:, 16:24], func=AF.Sqrt, bias=gne[:, 0:1], scale=1.0 / HD),
                         reads=[stt.res, gne.res], writes=[stt.res])
                    P.op("dve", lambda e: e.reciprocal(out=stt[:, 24:32], in_=stt[:, 24:32]), reads=[stt.res], writes=[stt.res])
                    P.op("dve", lambda e, a0=a0: e.tensor_tensor(out=a0[:], in0=a0[:], in1=stt[:, 24:32].unsqueeze(2).to_broadcast([128, 8, 64]),
                                                                  op=ALU.mult), reads=[a0.res, stt.res], writes=[a0.res])
                    a0f = a0[:].rearrange("p h v -> p (h v)")
                    P.op("pool", lambda e, a0=a0, a0f=a0f: e.tensor_tensor(out=a0f, in0=a0f, in1=gnw[:, 0, :], op=ALU.mult),
                         reads=[a0.res, gnw.res], writes=[a0.res])
                    P.op("pool", lambda e, a0=a0, a0f=a0f: e.tensor_tensor(out=a0f, in0=a0f, in1=gnw[:, 1, :], op=ALU.add),
                         reads=[a0.res, gnw.res], writes=[a0.res])
                    P.op("dve", lambda e, vt=vt, bt=bt: e.tensor_tensor(out=tb[:], in0=vt[:], in1=bt[:].unsqueeze(2).to_broadcast([128, 8, 64]),
                                                                        op=ALU.mult), reads=[vt.res, bt.res], writes=[tb.res])
                    P.op("pool", lambda e, a0=a0: e.tensor_tensor(out=a0[:], in0=a0[:], in1=tb[:], op=ALU.add),
                         reads=[a0.res, tb.res], writes=[a0.res])
                    P.op("dve", lambda e, a0f=a0f, a0=a0, gt=gt, rwb=rwb: e.tensor_tensor(out=rwb[:], in0=a0f, in1=gt[:], op=ALU.mult),
                         reads=[a0.res, gt.res], writes=[rwb.res])
                    psT = psum[0 + (t % 2)]
                    for c in range(4):
                        P.op("pe", lambda e, c=c, rwb=rwb, psT=psT: e.transpose(
                            out=psT[:].bitcast(BF16)[:, c * 128:(c + 1) * 128], in_=rwb[:, c * 128:(c + 1) * 128], identity=ident_b[:]),
                            reads=[rwb.res, ident_b.res], writes=[psT.res])
                    P.op("act", lambda e, rwT=rwT, psT=psT: e.copy(
                        out=rwT[:], in_=psT[:].bitcast(BF16)[:, 0:512].rearrange("p (c n) -> p c n", c=4)),
                        reads=[psT.res], writes=[rwT.res])
                    for half in range(2):
                        psY = psum[2 + 2 * (t % 2) + half]
                        for c in range(4):
                            P.op("pe", lambda e, c=c, half=half, psY=psY, rwT=rwT: e.matmul(
                                psY[:], rwT[:, c, :], wo_b[:, 4 + c, half * 512:(half + 1) * 512],
                                start=(c == 0), stop=(c == 3)), reads=[rwT.res, wo_b.res], writes=[psY.res])
                        P.op("dve", lambda e, half=half, psY=psY, M=M: e.tensor_tensor(
                            out=tmpy[:, half * 512:(half + 1) * 512], in0=psY[:], in1=M[2][:, half * 512:(half + 1) * 512],
                            op=ALU.mult), reads=[psY.res, M[2].res], writes=[tmpy.res])
                    P.op("pool", lambda e, xr=xr: e.tensor_tensor(out=xr[:], in0=xr[:], in1=tmpy[:], op=ALU.add),
                         reads=[xr.res, tmpy.res], writes=[xr.res])
                    P.dma("sp", xs[rows, :], xr[:], reads=[xr.res], writes=[P.R("xs", t)])
            P.barrier()
            if stop_after == "rw":
                P.finish()
                return

            is_moe = li % 2 == 1
            jj = li // 2
            n_tiles = NLT if last else NT
            G_all = sb("G_all", [128, NT, 8], st=stL)
            with contextlib.ExitStack() as stN:
                xt2 = [sb("nxt%d" % i, [128, D], st=stN) for i in range(2)]
                sq = sb("nsq", [128, D], st=stN)
                hf2 = sb("nhf2", [128, D], st=stN)
                hb2 = [sb("nhb%d" % i, [128, D], BF16, st=stN) for i in range(2)]
                hTg2 = [sb("nhTg%d" % i, [128, 8, 512], BF16, st=stN) for i in range(2)]
                st10 = sb("nst", [128, 48], st=stN)
                if is_moe:
                    wr = sb("wr", [128, 8, NE], st=stN)
                    P.dma("sp", wr[:], moe_router[jj].rearrange("(kc p) e -> p kc e", p=128), writes=[wr.res])
                    hTf = sb("hTf", [128, 8, 128], st=stN)
                for t in range(n_tiles):
                    which = 0 if t < NLT else 1
                    M = mods[which]
                    grp, tin = t // 4, t % 4
                    xt, hb, hTg = xt2[t % 2], hb2[t % 2], hTg2[grp % 2]
                    if t == 0:
                        P.dma("sp", xt[:], xs[0:128, :], reads=[P.R("xs", 0)], writes=[xt.res])
                    if t + 1 < n_tiles:
                        P.dma("sp", xt2[(t + 1) % 2][:], xs[(t + 1) * 128:(t + 2) * 128, :], reads=[P.R("xs", t + 1)],
                              writes=[xt2[(t + 1) % 2].res])
                    P.op("act", lambda e, xt=xt: e.activation(out=sq[:], in_=xt[:], func=AF.Square), reads=[xt.res], writes=[sq.res])
                    P.op("dve", lambda e: e.tensor_reduce(out=st10[:, 0:1], in_=sq[:], axis=AX.X, op=ALU.add),
                         reads=[sq.res], writes=[st10.res])
                    P.op("act", lambda e: e.activation(out=st10[:, 1:2], in_=st10[:, 0:1], func=AF.Sqrt, bias=eps_t[:, 0:1], scale=1.0 / D),
                         reads=[st10.res, eps_t.res], writes=[st10.res])
                    P.op("dve", lambda e: e.reciprocal(out=st10[:, 1:2], in_=st10[:, 1:2]), reads=[st10.res], writes=[st10.res])
                    P.op("dve", lambda e, xt=xt, M=M: e.scalar_tensor_tensor(
                        out=sq[:], in0=xt[:], scalar=st10[:, 1:2], in1=M[4][:], op0=ALU.mult, op1=ALU.mult),
                        reads=[xt.res, st10.res, M[4].res], writes=[sq.res])
                    if is_moe:
                        P.op("pool", lambda e, M=M: e.tensor_tensor(out=hf2[:], in0=sq[:], in1=M[3][:], op=ALU.add),
                             reads=[sq.res, M[3].res], writes=[hf2.res])
                        P.op("act", lambda e, hb=hb: e.copy(out=hb[:], in_=hf2[:]), reads=[hf2.res], writes=[hb.res])
                    else:
                        P.op("pool", lambda e, hb=hb, M=M: e.tensor_tensor(out=hb[:], in0=sq[:], in1=M[3][:], op=ALU.add),
                             reads=[sq.res, M[3].res], writes=[hb.res])
                    psT = psum[t % 2]
                    for kc in range(8):
                        P.op("pe", lambda e, kc=kc, psT=psT, hb=hb: e.transpose(
                            out=psT[:].bitcast(BF16)[:, kc * 128:(kc + 1) * 128], in_=hb[:, kc * 128:(kc + 1) * 128],
                            identity=ident_b[:]), reads=[hb.res, ident_b.res], writes=[psT.res])
                    P.op("act", lambda e, psT=psT, hTg=hTg, tin=tin: e.copy(
                        out=hTg[:, :, tin * 128:(tin + 1) * 128], in_=psT[:].bitcast(BF16).rearrange("p (k n) -> p k n", k=8)),
                        reads=[psT.res], writes=[hTg.res])
                    if is_moe:
                        for hf_ in range(2):
                            psF = psum[2 + hf_]
                            for k4 in range(4):
                                kc = 4 * hf_ + k4
                                P.op("pe", lambda e, psF=psF, k4=k4, kc=kc: e.transpose(
                                    out=psF[:, k4 * 128:(k4 + 1) * 128], in_=hf2[:, kc * 128:(kc + 1) * 128], identity=ident_f[:]),
                                    reads=[hf2.res, ident_f.res], writes=[psF.res])
                            P.op("dve" if hf_ == 0 else "act",
                                 (lambda e, psF=psF, hf_=hf_: e.tensor_copy(out=hTf[:, 4 * hf_:4 * hf_ + 4, :],
                                                                          in_=psF[:].rearrange("p (k n) -> p k n", k=4))) if hf_ == 0 else
                                 (lambda e, psF=psF, hf_=hf_: e.copy(out=hTf[:, 4 * hf_:4 * hf_ + 4, :],
                                                                    in_=psF[:].rearrange("p (k n) -> p k n", k=4))),
                                 reads=[psF.res], writes=[hTf.res])
                        psL = psum[4 + (t % 2)]
                        for kc in range(8):
                            P.op("pe", lambda e, kc=kc, psL=psL: e.matmul(psL[:, 0:NE], hTf[:, kc, :], wr[:, kc, :],
                                                                         start=(kc == 0), stop=(kc == 7)),
                                 reads=[hTf.res, wr.res], writes=[psL.res])
                        lg, m8, ng, ex, gm = st10[:, 8:16], st10[:, 16:24], st10[:, 24:25], st10[:, 32:40], st10[:, 40:48]
                        P.op("dve", lambda e, psL=psL, lg=lg: e.tensor_copy(out=lg, in_=psL[:, 0:NE]), reads=[psL.res], writes=[st10.res])
                        P.op("dve", lambda e, lg=lg, m8=m8: e.max(out=m8, in_=lg), reads=[st10.res], writes=[st10.res])
                        P.op("dve", lambda e, ng=ng, m8=m8: e.tensor_scalar(out=ng, in0=m8[:, 0:1], scalar1=-1.0, scalar2=None, op0=ALU.mult),
                             reads=[st10.res], writes=[st10.res])
                        P.op("act", lambda e, ex=ex, lg=lg, ng=ng: e.activation(out=ex, in_=lg, func=AF.Exp, bias=ng, scale=1.0),
                             reads=[st10.res], writes=[st10.res])
                        P.op("dve", lambda e, gm=gm, lg=lg, m8=m8, ex=ex: e.scalar_tensor_tensor(
                            out=gm, in0=lg, scalar=m8[:, 1:2], in1=ex, op0=ALU.is_ge, op1=ALU.mult),
                            reads=[st10.res], writes=[st10.res])
                        P.op("dve", lambda e, gm=gm: e.tensor_reduce(out=st10[:, 25:26], in_=gm, axis=AX.X, op=ALU.add),
                             reads=[st10.res], writes=[st10.res])
                        P.op("dve", lambda e: e.reciprocal(out=st10[:, 25:26], in_=st10[:, 25:26]), reads=[st10.res], writes=[st10.res])
                        P.op("dve", lambda e, gm=gm, t=t: e.tensor_scalar(out=G_all[:, t, :], in0=gm, scalar1=st10[:, 25:26], scalar2=None,
                                                                           op0=ALU.mult), reads=[st10.res], writes=[G_all.res])
                    if tin == 3 or t == n_tiles - 1:
                        N = (tin + 1) * 128
                        P.dma("act", H2[grp][:, :, 0:N], hTg[:, :, 0:N], reads=[hTg.res], writes=[P.R("H2", grp)])
            P.barrier()
            if stop_after == "norm2":
                dG = nc.dram_tensor("dbg_G", [128, NT, 8], F32, kind="ExternalOutput").ap()
                P.dma("sp", dG, G_all[:], reads=[G_all.res])
                P.finish()
                return

            groups = [(g, g * 512, 512, 0) for g in range(8)] + ([] if last else [(8, L, LC, 1)])
            if is_moe:
                experts = [(moe_wg[jj][e], moe_wu[jj][e], moe_wd[jj][e], DFE // 128, e) for e in range(NE)]
                split = [7, 7, 7, 7]
            else:
                experts = [(ffn_wg[jj], ffn_wu[jj], ffn_wd[jj], DFF // 128, None)]
                split = [8, 8, 6]
            subs = []
            for (wgs, wus, wds, nfc, e) in experts:
                f0 = 0
                for nf in split:
                    subs.append((wgs, wus, wds, f0, nf, e))
                    f0 += nf
                assert f0 == nfc
            with contextlib.ExitStack() as stF:
                load_cast = make_loader(stF)
                WG = [sb("WG%d" % i, [128, 8, 1024], BF16, st=stF) for i in range(2)]
                WU = [sb("WU%d" % i, [128, 8, 1024], BF16, st=stF) for i in range(2)]
                WD = [sb("WD%d" % i, [128, 8, 1024], BF16, st=stF) for i in range(2)]
                h2g = [sb("h2g%d" % i, [128, 8, 512], BF16, st=stF) for i in range(2)]
                aT = sb("aT", [128, 8, 512], BF16, st=stF)
                sgt = [sb("sgt%d" % i, [128, 512], st=stF) for i in range(2)]
                xf2 = [sb("xf%d" % i, [128, D], st=stF) for i in range(2)]
                dl2 = [sb("dl%d" % i, [128, D], st=stF) for i in range(2)]

                def load_tasks(si):
                    wgs, wus, wds, f0, nf, e = subs[si]
                    b = si % 2
                    tasks = []
                    for (dst, src) in ((WG[b], wgs), (WU[b], wus)):
                        v = src.rearrange("(kc p) n -> p kc n", p=128)
                        for c0 in range(0, nf * 128, 224):
                            c1 = min(nf * 128, c0 + 224)

                            tasks.append(lambda dst=dst, v=v, c0=c0, c1=c1, f0=f0: load_cast(
                                dst, lambda a, b_, v=v, f0=f0, c0=c0: v[:, :, f0 * 128 + c0 + a:f0 * 128 + c0 + b_], c1 - c0, 224, nk=8, col0=c0))
                    vd = wds[f0 * 128:(f0 + nf) * 128, :].rearrange("(f p) n -> p f n", p=128)
                    for c0 in range(0, D, 224):
                        c1 = min(D, c0 + 224)
                        tasks.append(lambda vd=vd, c0=c0, c1=c1, nf=nf, b=b: load_cast(
                            WD[b], lambda a, b_, vd=vd, c0=c0: vd[:, :, c0 + a:c0 + b_], c1 - c0, 224, nk=nf, col0=c0))
                    return tasks

                for tsk in load_tasks(0):
                    tsk()
                xrr = 0
                for si, (wgs, wus, wds, f0, nf, e) in enumerate(subs):
                    b = si % 2
                    nxt = load_tasks(si + 1) if si + 1 < len(subs) else []
                    per_g = (len(nxt) + len(groups) - 1) // len(groups)
                    tiles_seq = [(t0 // 128 + tt) for (g, t0, N, which) in groups for tt in range(N // 128)]

                    def load_hg(gi_):
                        g_, t0_, N_, w_ = groups[gi_]
                        P.dma("sp", h2g[gi_ % 2][:, :, 0:N_], H2[g_][:, :, 0:N_], reads=[P.R("H2", g_)], writes=[h2g[gi_ % 2].res])

                    def load_xf(k_):
                        t_ = tiles_seq[k_]
                        P.dma("act", xf2[k_ % 2][:], xs[t_ * 128:(t_ + 1) * 128, :], reads=[P.R("xs", t_)], writes=[xf2[k_ % 2].res])
                    load_hg(0)
                    load_xf(0)
                    xk = 0
                    for gi, (g, t0, N, which) in enumerate(groups):
                        M = mods[which]
                        hg = h2g[gi % 2]
                        if gi + 1 < len(groups):
                            load_hg(gi + 1)
                        for f in range(nf):
                            psG, psU = psum[(2 * f) % 4], psum[(2 * f + 1) % 4]
                            for kc in range(8):
                                P.op("pe", lambda e_, kc=kc, f=f, psG=psG, hg=hg, N=N, b=b: e_.matmul(
                                    psG[:, 0:N], WG[b][:, kc, f * 128:(f + 1) * 128], hg[:, kc, 0:N], start=(kc == 0), stop=(kc == 7)),
                                    reads=[WG[b].res, hg.res], writes=[psG.res])
                            for kc in range(8):
                                P.op("pe", lambda e_, kc=kc, f=f, psU=psU, hg=hg, N=N, b=b: e_.matmul(
                                    psU[:, 0:N], WU[b][:, kc, f * 128:(f + 1) * 128], hg[:, kc, 0:N], start=(kc == 0), stop=(kc == 7)),
                                    reads=[WU[b].res, hg.res], writes=[psU.res])
                            sg_ = sgt[f % 2]
                            P.op("act", lambda e_, sg_=sg_, psG=psG, N=N: e_.activation(out=sg_[:, 0:N], in_=psG[:, 0:N], func=AF.Silu),
                                 reads=[psG.res], writes=[sg_.res])
                            P.op("dve", lambda e_, sg_=sg_, psU=psU, f=f, N=N: e_.tensor_tensor(
                                out=aT[:, f, 0:N], in0=sg_[:, 0:N], in1=psU[:, 0:N], op=ALU.mult),
                                reads=[sg_.res, psU.res], writes=[aT.res])
                        for tt in range(N // 128):
                            t = t0 // 128 + tt
                            xf, dl = xf2[xk % 2], dl2[xk % 2]
                            rows = slice(t * 128, (t + 1) * 128)
                            for half in range(2):
                                psY = psum[4 + (2 * tt + half) % 4]
                                for f in range(nf):
                                    P.op("pe", lambda e_, f=f, tt=tt, half=half, psY=psY, b=b, nf=nf: e_.matmul(
                                        psY[:], aT[:, f, tt * 128:(tt + 1) * 128], WD[b][:, f, half * 512:(half + 1) * 512],
                                        start=(f == 0), stop=(f == nf - 1)), reads=[aT.res, WD[b].res], writes=[psY.res])
                                hs = slice(half * 512, (half + 1) * 512)
                                if e is None:
                                    P.op("dve", lambda e_, psY=psY, dl=dl, hs=hs, M=M: e_.tensor_tensor(
                                        out=dl[:, hs], in0=psY[:], in1=M[5][:, hs], op=ALU.mult),
                                        reads=[psY.res, M[5].res], writes=[dl.res])
                                else:
                                    P.op("dve", lambda e_, psY=psY, dl=dl, hs=hs, M=M, t=t, e=e: e_.scalar_tensor_tensor(
                                        out=dl[:, hs], in0=psY[:], scalar=G_all[:, t, e:e + 1], in1=M[5][:, hs],
                                        op0=ALU.mult, op1=ALU.mult), reads=[psY.res, M[5].res, G_all.res], writes=[dl.res])
                            P.op("dve", lambda e_, xf=xf, dl=dl: e_.tensor_tensor(out=xf[:], in0=xf[:], in1=dl[:], op=ALU.add),
                                 reads=[xf.res, dl.res], writes=[xf.res])
                            P.dma("sp", xs[rows, :], xf[:], reads=[xf.res], writes=[P.R("xs", t)])
                            xk += 1
                            if xk < len(tiles_seq):
                                load_xf(xk)
                        for tsk in nxt[gi * per_g:(gi + 1) * per_g]:
                            tsk()
            P.barrier()
            if stop_after == "ffn":
                P.finish()
                return
    for i in range(8):
        P.dma("sp" if i % 2 == 0 else "act", y_out[i * 512:(i + 1) * 512, :], xs[i * 512:(i + 1) * 512, :],
              reads=[P.R("xs", 4 * i + j) for j in range(4)], writes=[P.R("y", i)])
    P.finish()


def _prep_inputs(inputs):
    f = lambda a: np.ascontiguousarray(np.asarray(a, dtype=np.float32))
    com = {}
    for k in ("ada_w", "ada_b", "norm1_g", "norm2_g", "w_in", "w_out", "ffn_wg", "ffn_wu", "ffn_wd", "moe_router", "moe_wg", "moe_wu", "moe_wd"):
        com[k] = f(inputs[k])
    com["qk_gain"] = f(np.concatenate([inputs["q_gain"], inputs["k_gain"]], axis=1))
    rows = L // 64
    row = np.repeat(np.arange(rows, dtype=np.float32), 64)
    col = np.tile(np.arange(64, dtype=np.float32), rows)
    inv = (10000.0 ** (-np.arange(0, 32, 2, dtype=np.float32) / 32)).astype(np.float32)
    ang = np.concatenate([row[:, None] * inv, col[:, None] * inv], axis=-1).astype(np.float32)
    com["rope"] = f(np.concatenate([np.cos(ang), np.sin(ang)], axis=-1))
    com["ident"] = np.eye(128, dtype=np.float32)
    col = lambda v, n: np.asarray(v, np.float32).reshape(n, 128).T
    rwv = []
    for i in range(DEPTH):
        parts = [col(inputs["rw_kk"][i], 4), col(inputs["rw_ka"][i], 4), col(inputs["rw_rk"][i], 4),
                 col(inputs["rw_w0"][i][0], 4), col(inputs["rw_w0"][i][1], 4),
                 col(inputs["rw_a0"][i][0], 4), col(inputs["rw_a0"][i][1], 4), col(inputs["shift_mu"][i], 15)]
        rwv.append(np.concatenate(parts, axis=1))
    com["rwv"] = f(np.stack(rwv))
    com["rw_w2"] = f(np.asarray(inputs["rw_w2"]).reshape(DEPTH, 128, 512))
    com["rw_a2"] = f(np.asarray(inputs["rw_a2"]).reshape(DEPTH, 128, 512))
    com["rw_g2"] = f(inputs["rw_g2"])
    com["gn_wb"] = f(np.stack([inputs["rw_gn_w"], inputs["rw_gn_b"]], axis=1))
    c128 = np.zeros((128, 386), np.float32)
    c128[:64, :64] = 1.0
    c128[64:, 64:128] = 1.0
    c128[:64, 128] = 1.0
    c128[64:, 129] = 1.0
    c128[:, 130:386] = 1.0
    c128[:, 130:386:CH] = 0.0
    com["cst128"] = c128
    si, ti = np.meshgrid(np.arange(CH), np.arange(CH), indexing="ij")
    strict = [(si < ti), (si > ti)]
    incl = [(si <= ti), (si >= ti)]
    c64 = np.zeros((64, 7, 512), np.float32)
    for d in range(2):
        mb = np.stack([strict[d].astype(np.float32), -incl[d].astype(np.float32)], axis=1)
        mk = np.stack([strict[d].astype(np.float32), incl[d].astype(np.float32)], axis=1)
        c64[:, d] = np.tile(mb.reshape(64, 1, 128), (1, 4, 1)).reshape(64, 512)
        c64[:, 2 + d] = np.tile(mk.reshape(64, 1, 128), (1, 4, 1)).reshape(64, 512)
        mn = strict[1 - d].astype(np.float32)
        c64[:, 4 + d] = np.tile(mn.reshape(64, 1, 64), (1, 8, 1)).reshape(64, 512)
    c64[:, 6] = np.tile(np.eye(64, dtype=np.float32).reshape(64, 1, 64), (1, 8, 1)).reshape(64, 512)
    com["cst64"] = np.ascontiguousarray(c64.reshape(64, 3584)[:, 0:2048]).astype(ml_dtypes.bfloat16)
    cbd = np.zeros((128, 3, 4, 2, 64), np.float32)
    for hh in range(2):
        rows = slice(hh * 64, (hh + 1) * 64)
        for d in range(2):
            cbd[rows, d] = strict[d].astype(np.float32)[:, None, None, :]
        cbd[rows, 2, :, hh, :] = np.eye(64, dtype=np.float32)[:, None, :]
    com["cstbd"] = cbd.reshape(128, 3, 512).astype(ml_dtypes.bfloat16)
    return com


def kernel(**inputs):
    com = _prep_inputs(inputs)
    x = np.asarray(inputs["x"], dtype=np.float32)
    ctx = np.asarray(inputs["ctx"], dtype=np.float32)
    c = np.asarray(inputs["c"], dtype=np.float32)
    c_ctx = np.asarray(inputs["c_ctx"], dtype=np.float32)
    nc = bass.Bass("TRN2", target_bir_lowering=False)
    build(nc)
    in_maps = []
    for b in range(8):
        m = dict(com)
        m["x"] = np.ascontiguousarray(x[b])
        m["ctx"] = np.ascontiguousarray(ctx[b])
        m["cvec"] = np.ascontiguousarray(
            np.concatenate([c[b].reshape(8, 128).T, c_ctx.reshape(8, 128).T], axis=1))
        in_maps.append(m)
    res = run_bass_kernel_spmd(nc, in_maps, core_ids=list(range(8)))
    return np.stack([r["y"] for r in res.results], axis=0)
```

```python
import contextlib
import numpy as np
import ml_dtypes
import concourse.bass as bass
import concourse.mybir as mybir
from concourse.bass_utils import run_bass_kernel_spmd

F32 = mybir.dt.float32
BF16 = mybir.dt.bfloat16
AF = mybir.ActivationFunctionType
ALU = mybir.AluOpType
AX = mybir.AxisListType

D = 1024
L = 4096
LC = 256
T = L + LC
NT = T // 128
NLT = L // 128
DEPTH = 4
HD = 64
ATT_IN = 768
SHIFT_W = 1920
IN_W = 2688
DFF = 2816
DFE = 3584
NE = 8
EPS = 1e-6
GN_EPS = 64e-5
CH = 64
NCH = T // CH
DECAY_C = float(np.exp(-0.5))


class Res:
    __slots__ = ("w", "r")

    def __init__(self):
        self.w = None
        self.r = {}


class Prog:
    LIMIT = 30000

    def __init__(self, nc, stack):
        self.nc = nc
        self.stack = stack
        self.eng = {"pe": nc.tensor, "act": nc.scalar, "dve": nc.vector, "pool": nc.gpsimd, "sp": nc.sync}
        self.sem = {}
        self.val = {}
        self.cur = {}
        self.nsem = 0
        self.waited = {e: {} for e in self.eng}
        for e in ("pe", "act", "dve", "pool"):
            self._fresh(e)
        self.slots = {}
        self.rr = {}
        for q, n in (("sp", 8), ("act", 4), ("pool", 6)):
            self.slots[q] = []
            self.rr[q] = 0
            for i in range(n):
                b = "d_%s%d" % (q, i)
                self._fresh(b)
                self.slots[q].append(b)
        self.dram_res = {}
        self.n_inst = 0

    def _fresh(self, base):
        ep = self.cur[base][1] + 1 if base in self.cur else 0
        k = (base, ep)
        self.nsem += 1
        self.sem[k] = self.stack.enter_context(self.nc.semaphore("s%d_%s_%d" % (self.nsem, base, ep)))
        self.val[k] = 0
        self.cur[base] = k
        return k

    def R(self, *key):
        r = self.dram_res.get(key)
        if r is None:
            r = self.dram_res[key] = Res()
        return r

    def _deps(self, eng, reads, writes, extra=()):
        need = {}
        for k, v in extra:
            if v > need.get(k, 0):
                need[k] = v
        for t in reads:
            if t.w is not None:
                k, v = t.w
                if v > need.get(k, 0):
                    need[k] = v
        for t in writes:
            if t.w is not None:
                k, v = t.w
                if k[0] != eng and v > need.get(k, 0):
                    need[k] = v
            for k, v in t.r.items():
                if k[0] != eng and v > need.get(k, 0):
                    need[k] = v
        wd = self.waited[eng]
        e = self.eng[eng]
        for k, v in need.items():
            if k[0] == eng and eng == "pe":
                continue
            if wd.get(k, 0) >= v:
                continue
            wd[k] = v
            e.wait_ge(self.sem[k], v)
            self.n_inst += 1

    def _mark(self, ev, reads, writes):
        k, v = ev
        for t in reads:
            if v > t.r.get(k, 0):
                t.r[k] = v
        for t in writes:
            t.w = ev
            t.r = {}

    def op(self, eng, fn, reads=(), writes=()):
        self._deps(eng, reads, writes)
        ins = fn(self.eng[eng])
        k = self.cur[eng]
        if self.val[k] >= self.LIMIT:
            k = self._fresh(eng)
        self.val[k] += 1
        ins.then_inc(self.sem[k], 1)
        self.n_inst += 1
        self._mark((k, self.val[k]), reads, writes)

    def dma(self, q, out, in_, reads=(), writes=(), **kw):
        sl = self.slots[q]
        b = sl[self.rr[q] % len(sl)]
        self.rr[q] += 1
        k = self.cur[b]
        self._deps(q, reads, writes, extra=((k, self.val[k]),))
        ins = self.eng[q].dma_start(out=out, in_=in_, **kw)
        if self.val[k] >= self.LIMIT:
            k = self._fresh(b)
        self.val[k] += 16
        ins.then_inc(self.sem[k], 16)
        self.n_inst += 1
        self._mark((k, self.val[k]), reads, writes)

    def barrier(self):
        for e in self.eng:
            wd = self.waited[e]
            for k, v in self.val.items():
                if v > 0 and wd.get(k, 0) < v and not (k[0] == e and e == "pe"):
                    wd[k] = v
                    self.eng[e].wait_ge(self.sem[k], v)

    def finish(self):
        wd = self.waited["sp"]
        for k, v in self.val.items():
            if v > 0 and wd.get(k, 0) < v:
                wd[k] = v
                self.eng["sp"].wait_ge(self.sem[k], v)


class Tile:
    def __init__(self, t):
        self.t = t
        self.res = Res()

    def __getitem__(self, key):
        return self.t[key]


def build(nc, n_layers=DEPTH, stop_after=None, dbg=()):
    stack = contextlib.ExitStack()
    with stack:
        _build(nc, stack, n_layers, stop_after, dbg)
    return nc


def _build(nc, stack, n_layers, stop_after, dbg):
    P = Prog(nc, stack)

    def din(name, shape, dt=F32):
        return nc.dram_tensor(name, list(shape), dt, kind="ExternalInput").ap()

    def dscr(name, shape, dt=F32):
        kind = "ExternalOutput" if name in dbg else "Internal"
        return nc.dram_tensor(name, list(shape), dt, kind=kind).ap()

    sb_n = [0]

    def sb(name, shape, dt=F32, st=None):
        sb_n[0] += 1
        return Tile((st or stack).enter_context(nc.sbuf_tensor("%s_%d" % (name, sb_n[0]), list(shape), dt)))

    x_in = din("x", [L, D])
    ctx_in = din("ctx", [LC, D])
    cvec = din("cvec", [128, 16])
    ada_w = din("ada_w", [DEPTH, D, 6 * D])
    ada_b = din("ada_b", [DEPTH, 6 * D])
    norm1_g = din("norm1_g", [DEPTH, D])
    norm2_g = din("norm2_g", [DEPTH, D])
    w_in = din("w_in", [DEPTH, D, IN_W])
    w_out = din("w_out", [DEPTH, D, D])
    qk_gain = din("qk_gain", [DEPTH, 2 * HD])
    rope = din("rope", [L, 64])
    ident_d = din("ident", [128, 128])
    rwv = din("rwv", [DEPTH, 128, 43])
    rw_w2 = din("rw_w2", [DEPTH, 128, 512])
    rw_a2 = din("rw_a2", [DEPTH, 128, 512])
    rw_g2 = din("rw_g2", [DEPTH, 128, 512])
    gn_wb = din("gn_wb", [DEPTH, 2, 512])
    ffn_wg = din("ffn_wg", [2, D, DFF])
    ffn_wu = din("ffn_wu", [2, D, DFF])
    ffn_wd = din("ffn_wd", [2, DFF, D])
    moe_router = din("moe_router", [2, D, NE])
    moe_wg = din("moe_wg", [2, NE, D, DFE])
    moe_wu = din("moe_wu", [2, NE, D, DFE])
    moe_wd = din("moe_wd", [2, NE, DFE, D])
    cst128 = din("cst128", [128, 386])
    cst64 = din("cst64", [64, 3584], BF16)
    y_out = nc.dram_tensor("y", [L, D], F32, kind="ExternalOutput").ap()

    xs = dscr("xs", [T, D])
    pT = dscr("pT", [SHIFT_W, T])
    qs = dscr("qs", [T, 512], BF16)
    FM = [dscr("FM%d" % d, [NCH, 64, 2, 512], BF16) for d in range(2)]
    TM = [dscr("TM%d" % d, [NCH, 64, 1024], BF16) for d in range(2)]
    VM = dscr("VM", [NCH, 64, 512], BF16)
    MAT = [dscr("MAT%d" % d, [NCH, 64, 2560], BF16) for d in range(2)]
    Od = [dscr("Od%d" % d, [T, 512]) for d in range(2)]
    gtm = dscr("gtm", [T, 512])
    bon = dscr("bon", [T, 8])
    H2 = dscr("H2", [9, 128, 8, 512], BF16)

    ident_f = sb("ident_f", [128, 128])
    ident_b = sb("ident_b", [128, 128], BF16)
    ones_f = sb("ones_f", [128, 128])
    sc = sb("sc", [128, 16])
    mods = [[sb("mod%d_%d" % (w, j), [128, D]) for j in range(6)] for w in range(2)]
    psum = [Tile(stack.enter_context(nc.psum_tensor("ps%d" % i, [128, 512], F32))) for i in range(8)]
    ps_rr = [0]
    uid = [0]

    def next_ps():
        p = psum[ps_rr[0] % 8]
        ps_rr[0] += 1
        return p

    P.dma("sp", ident_f[:], ident_d[:, :], writes=[ident_f.res])
    P.op("dve", lambda e: e.tensor_copy(out=ident_b[:], in_=ident_f[:]), reads=[ident_f.res], writes=[ident_b.res])
    P.op("pool", lambda e: e.memset(ones_f[:], 1.0), writes=[ones_f.res])
    P.dma("sp", sc[:], cvec[:, :], writes=[sc.res])
    P.op("act", lambda e: e.activation(out=sc[:], in_=sc[:], func=AF.Silu), reads=[sc.res], writes=[sc.res])
    for i in range(8):
        P.dma("pool", xs[i * 512:(i + 1) * 512, :], x_in[i * 512:(i + 1) * 512, :], writes=[P.R("xs", 4 * i + j) for j in range(4)])
    P.dma("pool", xs[L:T, :], ctx_in[:, :], writes=[P.R("xs", 32), P.R("xs", 33)])

    for li in range(n_layers):
        last = li == DEPTH - 1
        with contextlib.ExitStack() as st:
            wst = [sb("adaw%d" % i, [128, 8, 512], st=st) for i in range(2)]
            lrep = sb("lrep", [128, 16, 128], st=st)
            P.op("dve", lambda e: e.tensor_copy(out=lrep[:], in_=sc[:].unsqueeze(2).to_broadcast([128, 16, 128])),
                 reads=[sc.res], writes=[lrep.res])
            bst = sb("adab", [1, 6 * D], st=st)
            gst = sb("g1bc", [128, D], st=st)
            g2st = sb("g2bc", [128, D], st=st)
            P.dma("sp", bst[:], ada_b[li:li + 1, :], writes=[bst.res])
            P.dma("sp", gst[:], norm1_g[li, :].partition_broadcast(128), writes=[gst.res])
            P.dma("sp", g2st[:], norm2_g[li, :].partition_broadcast(128), writes=[g2st.res])
            aw = ada_w[li].rearrange("(kc p) n -> p kc n", p=128)
            for pc in range(12):
                w = wst[pc % 2]
                P.dma("sp" if pc % 2 == 0 else "pool", w[:], aw[:, :, pc * 512:(pc + 1) * 512], writes=[w.res])
                j, half = pc // 2, pc % 2
                for which in range(2):
                    ps = next_ps()
                    for kc in range(8):
                        P.op("pe", lambda e, kc=kc, ps=ps, w=w, which=which: e.matmul(
                            ps[:], lrep[:, which * 8 + kc, :], w[:, kc, :], start=(kc == 0), stop=False),
                            reads=[lrep.res, w.res], writes=[ps.res])
                    P.op("pe", lambda e, ps=ps, pc=pc: e.matmul(
                        ps[:], ones_f[0:1, :], bst[0:1, pc * 512:(pc + 1) * 512], start=False, stop=True),
                        reads=[ones_f.res, bst.res], writes=[ps.res])
                    m = mods[which][j]
                    P.op("act", lambda e, m=m, ps=ps, half=half: e.copy(out=m[:, half * 512:(half + 1) * 512], in_=ps[:]),
                         reads=[ps.res], writes=[m.res])
            for which in range(2):
                for (j, g) in ((1, gst), (4, g2st)):
                    m = mods[which][j]
                    P.op("dve", lambda e, m=m, g=g: e.scalar_tensor_tensor(
                        out=m[:], in0=m[:], scalar=1.0, in1=g[:], op0=ALU.add, op1=ALU.mult),
                        reads=[m.res, g.res], writes=[m.res])
        P.barrier()
        if stop_after == "mods":
            dbg_t = nc.dram_tensor("dbg_mods", [12, 128, D], F32, kind="ExternalOutput").ap()
            for which in range(2):
                for j in range(6):
                    P.dma("sp", dbg_t[which * 6 + j], mods[which][j][:], reads=[mods[which][j].res])
            P.finish()
            return

        with contextlib.ExitStack() as stL:
            def make_loader(st_):
                uid[0] += 1
                stage = [sb("stage%d_%d" % (uid[0], i), [128, 1792], st=st_) for i in range(2)]
                stage_rr = [0]

                def load_cast(dst, src_fn, ncols, piece, nk=8, col0=0):
                    for c0 in range(0, ncols, piece):
                        c1 = min(ncols, c0 + piece)
                        stg = stage[stage_rr[0] % 2]
                        q = "sp" if stage_rr[0] % 2 == 0 else "act"
                        stage_rr[0] += 1
                        view = stg[:, 0:nk * (c1 - c0)].rearrange("p (k n) -> p k n", k=nk)
                        P.dma(q, view, src_fn(c0, c1), writes=[stg.res])
                        P.op("pool", lambda e, view=view, c0=c0, c1=c1: e.tensor_copy(out=dst[:, 0:nk, col0 + c0:col0 + c1], in_=view),
                             reads=[stg.res], writes=[dst.res])
                return load_cast

            eps_t = sb("eps_t", [128, 1], st=stL)
            P.op("pool", lambda e: e.memset(eps_t[:], EPS), writes=[eps_t.res])

            with contextlib.ExitStack() as stA:
                load_cast = make_loader(stA)
                KT = sb("KT", [128, T], BF16, st=stA)
                Vaug = sb("Vaug", [128, NT, 2, 65], BF16, st=stA)
                P.op("pool", lambda e: e.memset(Vaug[:, :, :, 64:65], 1.0), writes=[Vaug.res])
                with contextlib.ExitStack() as stB:
                    win_b = sb("win_b", [128, 8, IN_W], BF16, st=stB)
                    load_cast(win_b, lambda c0, c1: w_in[li].rearrange("(kc p) n -> p kc n", p=128)[:, :, c0:c1], IN_W, 224)
                    gain = sb("gain", [128, 128], st=stB)
                    P.dma("sp", gain[:], qk_gain[li, :].partition_broadcast(128), writes=[gain.res])
                    xt2 = [sb("xt%d" % i, [128, D], st=stB) for i in range(2)]
                    sq = sb("sq", [128, D], st=stB)
                    hf = sq
                    hb2 = [sb("hb%d" % i, [128, D], BF16, st=stB) for i in range(2)]
                    hTg2 = [sb("hTg%d" % i, [128, 8, 512], BF16, st=stB) for i in range(2)]
                    st10 = sb("st10", [128, 24], st=stB)
                    qkf = sb("qkf", [128, 10, 64], st=stB)
                    qkb2 = [sb("qkb%d" % i, [128, 10, 64], BF16, st=stB) for i in range(2)]
                    rt = [sb("rt%d" % i, [128, 10, 32], st=stB) for i in range(4)]
                    rp2 = [sb("rp%d" % i, [128, 64], st=stB) for i in range(2)]
                    pst3 = [sb("pst%d" % i, [128, 512], st=stB) for i in range(2)]
                    pst_rr = 0
                    for t in range(NT):
                        which = 0 if t < NLT else 1
                        M = mods[which]
                        grp, tin = t // 4, t % 4
                        xt, hb, hTg, qkb = xt2[t % 2], hb2[t % 2], hTg2[grp % 2], qkb2[t % 2]
                        if t == 0:
                            P.dma("sp", xt[:], xs[0:128, :], reads=[P.R("xs", 0)], writes=[xt.res])
                        if t + 1 < NT:
                            P.dma("sp", xt2[(t + 1) % 2][:], xs[(t + 1) * 128:(t + 2) * 128, :], reads=[P.R("xs", t + 1)],
                                  writes=[xt2[(t + 1) % 2].res])
                        P.op("act", lambda e, xt=xt: e.activation(out=sq[:], in_=xt[:], func=AF.Square),
                             reads=[xt.res], writes=[sq.res])
                        P.op("dve", lambda e: e.tensor_reduce(out=st10[:, 0:1], in_=sq[:], axis=AX.X, op=ALU.add),
                             reads=[sq.res], writes=[st10.res])
                        P.op("act", lambda e: e.activation(out=st10[:, 1:2], in_=st10[:, 0:1], func=AF.Sqrt,
                                                           bias=eps_t[:, 0:1], scale=1.0 / D),
                             reads=[st10.res, eps_t.res], writes=[st10.res])
                        P.op("dve", lambda e: e.reciprocal(out=st10[:, 1:2], in_=st10[:, 1:2]),
                             reads=[st10.res], writes=[st10.res])
                        P.op("dve", lambda e, xt=xt, M=M: e.scalar_tensor_tensor(
                            out=hf[:], in0=xt[:], scalar=st10[:, 1:2], in1=M[1][:], op0=ALU.mult, op1=ALU.mult),
                            reads=[xt.res, st10.res, M[1].res], writes=[hf.res])
                        P.op("dve", lambda e, hb=hb, M=M: e.tensor_tensor(out=hb[:], in0=hf[:], in1=M[0][:], op=ALU.add),
                             reads=[hf.res, M[0].res], writes=[hb.res])
                        psT = psum[0 + (t % 2)]
                        for kc in range(8):
                            P.op("pe", lambda e, kc=kc, psT=psT, hb=hb: e.transpose(
                                out=psT[:].bitcast(BF16)[:, kc * 128:(kc + 1) * 128], in_=hb[:, kc * 128:(kc + 1) * 128],
                                identity=ident_b[:]), reads=[hb.res, ident_b.res], writes=[psT.res])
                        P.op("act", lambda e, psT=psT, hTg=hTg, tin=tin: e.copy(
                            out=hTg[:, :, tin * 128:(tin + 1) * 128],
                            in_=psT[:].bitcast(BF16).rearrange("p (k n) -> p k n", k=8)),
                            reads=[psT.res], writes=[hTg.res])
                        psA, psB = psum[2], psum[3]
                        for kc in range(8):
                            P.op("pe", lambda e, kc=kc, hTg=hTg, tin=tin: e.matmul(
                                psA[:], hTg[:, kc, tin * 128:(tin + 1) * 128], win_b[:, kc, 0:512],
                                start=(kc == 0), stop=(kc == 7)), reads=[hTg.res, win_b.res], writes=[psA.res])
                        for kc in range(8):
                            P.op("pe", lambda e, kc=kc, hTg=hTg, tin=tin: e.matmul(
                                psB[:, 0:256], hTg[:, kc, tin * 128:(tin + 1) * 128], win_b[:, kc, 512:768],
                                start=(kc == 0), stop=(kc == 7)), reads=[hTg.res, win_b.res], writes=[psB.res])
                        P.op("act", lambda e: e.activation(out=sq[:, 0:512], in_=psA[:], func=AF.Square),
                             reads=[psA.res], writes=[sq.res])
                        P.op("act", lambda e: e.activation(out=sq[:, 512:640], in_=psB[:, 0:128], func=AF.Square),
                             reads=[psB.res], writes=[sq.res])
                        P.op("act", lambda e, t=t: e.copy(out=Vaug[:, t, :, 0:64],
                                                          in_=psB[:, 128:256].rearrange("p (g d) -> p g d", g=2)),
                             reads=[psB.res], writes=[Vaug.res])
                        P.op("dve", lambda e: e.tensor_reduce(
                            out=st10[:, 4:14], in_=sq[:, 0:640].rearrange("p (h d) -> p h d", h=10), axis=AX.X, op=ALU.add),
                            reads=[sq.res], writes=[st10.res])
                        P.op("act", lambda e: e.activation(out=st10[:, 14:24], in_=st10[:, 4:14], func=AF.Sqrt,
                                                           bias=eps_t[:, 0:1], scale=1.0 / HD),
                             reads=[st10.res, eps_t.res], writes=[st10.res])
                        P.op("dve", lambda e: e.reciprocal(out=st10[:, 14:24], in_=st10[:, 14:24]),
                             reads=[st10.res], writes=[st10.res])
                        P.op("dve", lambda e: e.tensor_tensor(
                            out=qkf[:, 0:8, :], in0=psA[:].rearrange("p (h d) -> p h d", h=8),
                            in1=st10[:, 14:22].unsqueeze(2).to_broadcast([128, 8, 64]), op=ALU.mult),
                            reads=[psA.res, st10.res], writes=[qkf.res])
                        P.op("dve", lambda e: e.tensor_tensor(
                            out=qkf[:, 8:10, :], in0=psB[:, 0:128].rearrange("p (h d) -> p h d", h=2),
                            in1=st10[:, 22:24].unsqueeze(2).to_broadcast([128, 2, 64]), op=ALU.mult),
                            reads=[psB.res, st10.res], writes=[qkf.res])
                        P.op("dve", lambda e: e.tensor_tensor(
                            out=qkf[:, 0:8, :], in0=qkf[:, 0:8, :],
                            in1=gain[:, 0:64].unsqueeze(1).to_broadcast([128, 8, 64]), op=ALU.mult),
                            reads=[qkf.res, gain.res], writes=[qkf.res])
                        P.op("dve", lambda e: e.tensor_tensor(
                            out=qkf[:, 8:10, :], in0=qkf[:, 8:10, :],
                            in1=gain[:, 64:128].unsqueeze(1).to_broadcast([128, 2, 64]), op=ALU.mult),
                            reads=[qkf.res, gain.res], writes=[qkf.res])
                        if t < NLT:
                            rp = rp2[t % 2]
                            P.dma("act", rp[:], rope[t * 128:(t + 1) * 128, :], writes=[rp.res])
                            cosb = rp[:, 0:32].unsqueeze(1).to_broadcast([128, 10, 32])
                            sinb = rp[:, 32:64].unsqueeze(1).to_broadcast([128, 10, 32])
                            x1, x2 = qkf[:, :, 0:32], qkf[:, :, 32:64]
                            P.op("dve", lambda e, cosb=cosb, x1=x1: e.tensor_tensor(out=rt[0][:], in0=x1, in1=cosb, op=ALU.mult),
                                 reads=[qkf.res, rp.res], writes=[rt[0].res])
                            P.op("dve", lambda e, sinb=sinb, x2=x2: e.tensor_tensor(out=rt[1][:], in0=x2, in1=sinb, op=ALU.mult),
                                 reads=[qkf.res, rp.res], writes=[rt[1].res])
                            P.op("dve", lambda e, qkb=qkb: e.tensor_tensor(out=qkb[:, :, 0:32], in0=rt[0][:], in1=rt[1][:], op=ALU.subtract),
                                 reads=[rt[0].res, rt[1].res], writes=[qkb.res])
                            P.op("pool", lambda e, sinb=sinb, x1=x1: e.tensor_tensor(out=rt[2][:], in0=x1, in1=sinb, op=ALU.mult),
                                 reads=[qkf.res, rp.res], writes=[rt[2].res])
                            P.op("pool", lambda e, cosb=cosb, x2=x2: e.tensor_tensor(out=rt[3][:], in0=x2, in1=cosb, op=ALU.mult),
                                 reads=[qkf.res, rp.res], writes=[rt[3].res])
                            P.op("pool", lambda e, qkb=qkb: e.tensor_tensor(out=qkb[:, :, 32:64], in0=rt[2][:], in1=rt[3][:], op=ALU.add),
                                 reads=[rt[2].res, rt[3].res], writes=[qkb.res])
                        else:
                            P.op("dve", lambda e, qkb=qkb: e.tensor_copy(out=qkb[:], in_=qkf[:]),
                                 reads=[qkf.res], writes=[qkb.res])
                        for g in range(2):
                            P.dma("act" if g == 0 else "sp",
                                  qs[t * 128:(t + 1) * 128, :].rearrange("p (j g d) -> p g j d", j=4, g=2)[:, g, :, :],
                                  qkb[:, 4 * g:4 * g + 4, :], reads=[qkb.res], writes=[P.R("qs", t)])
                        psK = psum[4 + (t % 2)]
                        P.op("pe", lambda e, psK=psK, qkb=qkb: e.transpose(
                            out=psK[:].bitcast(BF16)[:, 0:128], in_=qkb[:, 8:10, :].rearrange("p g d -> p (g d)"),
                            identity=ident_b[:]), reads=[qkb.res, ident_b.res], writes=[psK.res])
                        P.op("act", lambda e, psK=psK, t=t: e.copy(
                            out=KT[:, t * 128:(t + 1) * 128], in_=psK[:].bitcast(BF16)[:, 0:128]),
                            reads=[psK.res], writes=[KT.res])
                        if tin == 3 or t == NT - 1:
                            N = (tin + 1) * 128
                            g0 = grp * 512
                            for c in range(15):
                                ps = psum[6 + (c % 2)]
                                for kc in range(8):
                                    P.op("pe", lambda e, kc=kc, c=c, ps=ps, hTg=hTg, N=N: e.matmul(
                                        ps[:, 0:N], win_b[:, kc, ATT_IN + c * 128:ATT_IN + (c + 1) * 128], hTg[:, kc, 0:N],
                                        start=(kc == 0), stop=(kc == 7)), reads=[hTg.res, win_b.res], writes=[ps.res])
                                pst = pst3[pst_rr % 2]
                                pst_rr += 1
                                P.op("act" if c % 2 == 0 else "dve",
                                     (lambda e, ps=ps, pst=pst, N=N: e.copy(out=pst[:, 0:N], in_=ps[:, 0:N])) if c % 2 == 0 else
                                     (lambda e, ps=ps, pst=pst, N=N: e.tensor_copy(out=pst[:, 0:N], in_=ps[:, 0:N])),
                                     reads=[ps.res], writes=[pst.res])
                                P.dma("sp", pT[c * 128:(c + 1) * 128, g0:g0 + N], pst[:, 0:N],
                                      reads=[pst.res], writes=[P.R("pT", grp)])
                P.barrier()
                if stop_after == "phaseB":
                    dK = nc.dram_tensor("dbg_KT", [128, T], BF16, kind="ExternalOutput").ap()
                    dV = nc.dram_tensor("dbg_V", [128, NT, 2, 65], BF16, kind="ExternalOutput").ap()
                    P.dma("sp", dK, KT[:], reads=[KT.res])
                    P.dma("sp", dV, Vaug[:], reads=[Vaug.res])
                    P.finish()
                    return

                with contextlib.ExitStack() as stC:
                    wo_h = sb("wo_h", [64, 8, D], BF16, st=stC)
                    woh_v = w_out[li][0:512, :].rearrange("(h d) n -> d h n", d=64)
                    wstg2 = [sb("wstg2_%d" % i, [64, 8, 224], st=stC) for i in range(2)]
                    for pi, c0 in enumerate(range(0, D, 224)):
                        c1 = min(D, c0 + 224)
                        wsg = wstg2[pi % 2]
                        P.dma("sp" if pi % 2 == 0 else "act", wsg[:, :, 0:c1 - c0], woh_v[:, :, c0:c1], writes=[wsg.res])
                        P.op("pool", lambda e, wsg=wsg, c0=c0, c1=c1: e.tensor_copy(out=wo_h[:, :, c0:c1], in_=wsg[:, :, 0:c1 - c0]),
                             reads=[wsg.res], writes=[wo_h.res])
                    qt2 = [sb("qt%d" % i, [128, 512], BF16, st=stC) for i in range(2)]
                    qT2 = [sb("qT%d" % i, [128, 4, 128], BF16, st=stC) for i in range(2)]
                    Eb = [sb("Eb%d" % i, [128, 512], BF16, st=stC) for i in range(4)]
                    e_rr = 0
                    rcp = sb("rcp", [65, 512], st=stC)
                    nm2 = [sb("nm%d" % i, [64, 512], st=stC) for i in range(2)]
                    attT2 = [sb("attT%d" % i, [64, 8, 128], BF16, st=stC) for i in range(2)]
                    xa2 = [sb("xa%d" % i, [128, D], st=stC) for i in range(2)]
                    tmp = sb("tmpy", [128, D], st=stC)
                    s_rr = [0, 0]
                    for t in range(NT):
                        which = 0 if t < NLT else 1
                        M = mods[which]
                        qt, qT, attT, xa = qt2[t % 2], qT2[t % 2], attT2[t % 2], xa2[t % 2]
                        if t == 0:
                            P.dma("sp", qt[:], qs[0:128, :], reads=[P.R("qs", 0)], writes=[qt.res])
                        if t + 1 < NT:
                            P.dma("sp", qt2[(t + 1) % 2][:], qs[(t + 1) * 128:(t + 2) * 128, :], reads=[P.R("qs", t + 1)],
                                  writes=[qt2[(t + 1) % 2].res])
                        psQ = psum[7]
                        for jh in range(4):
                            P.op("pe", lambda e, jh=jh, qt=qt: e.transpose(
                                out=psQ[:].bitcast(BF16)[:, jh * 128:(jh + 1) * 128], in_=qt[:, jh * 128:(jh + 1) * 128],
                                identity=ident_b[:]), reads=[qt.res, ident_b.res], writes=[psQ.res])
                        P.op("dve", lambda e, qT=qT: e.tensor_copy(
                            out=qT[:], in_=psQ[:].bitcast(BF16)[:, 0:512].rearrange("p (j n) -> p j n", j=4)),
                            reads=[psQ.res], writes=[qT.res])
                        chunks = list(range(NT)) if t < NLT else [32, 33]

                        def score(sc_, g, qT=qT):
                            psS = psum[2 * g + s_rr[g] % 2]
                            s_rr[g] += 1
                            sl = slice(g * 64, (g + 1) * 64)
                            P.op("pe", lambda e, psS=psS, sl=sl, sc_=sc_, qT=qT: e.matmul(
                                psS[:], KT[sl, sc_ * 128:(sc_ + 1) * 128],
                                qT[sl, :, :].rearrange("p h n -> p (h n)"), start=True, stop=True),
                                reads=[KT.res, qT.res], writes=[psS.res])
                            return psS
                        pend = [[score(chunks[0], g)] for g in range(2)]
                        for ci, sc_ in enumerate(chunks):
                            cur = [pend[g].pop(0) for g in range(2)]
                            if ci + 1 < len(chunks):
                                for g in range(2):
                                    pend[g].append(score(chunks[ci + 1], g))
                            for g in range(2):
                                E = Eb[e_rr % 4]
                                e_rr += 1
                                psS, psO = cur[g], psum[4 + g]
                                P.op("act", lambda e, E=E, psS=psS: e.activation(out=E[:], in_=psS[:], func=AF.Exp, scale=0.125),
                                     reads=[psS.res], writes=[E.res])
                                P.op("pe", lambda e, E=E, psO=psO, sc_=sc_, g=g, ci=ci, n=len(chunks): e.matmul(
                                    psO[0:65, :], Vaug[:, sc_, g, :], E[:], start=(ci == 0), stop=(ci == n - 1)),
                                    reads=[E.res, Vaug.res], writes=[psO.res])
                        if last and t >= NLT:
                            continue
                        for g in range(2):
                            psO, nm = psum[4 + g], nm2[g]
                            P.op("dve", lambda e, psO=psO: e.reciprocal(out=rcp[64:65, :], in_=psO[64:65, :]),
                                 reads=[psO.res], writes=[rcp.res])
                            P.op("dve", lambda e, psO=psO, nm=nm: e.tensor_copy(out=nm[:], in_=psO[0:64, :]), reads=[psO.res], writes=[nm.res])
                            psBc = psum[6]
                            P.op("pe", lambda e, psBc=psBc: e.matmul(psBc[0:64, :], ones_f[64:65, 0:64], rcp[64:65, :], start=True, stop=True),
                                 reads=[ones_f.res, rcp.res], writes=[psBc.res])
                            P.op("dve", lambda e, psBc=psBc, nm=nm, g=g, attT=attT: e.tensor_tensor(
                                out=attT[:, 4 * g:4 * g + 4, :].rearrange("p h n -> p (h n)"), in0=nm[:], in1=psBc[0:64, :], op=ALU.mult),
                                reads=[nm.res, psBc.res], writes=[attT.res])
                        if last and t >= NLT:
                            continue
                        P.dma("act", xa[:], xs[t * 128:(t + 1) * 128, :], reads=[P.R("xs", t)], writes=[xa.res])
                        for half in range(2):
                            psY = psum[6 + half]
                            for h in range(8):
                                P.op("pe", lambda e, h=h, half=half, psY=psY, attT=attT: e.matmul(
                                    psY[:], attT[:, h, :], wo_h[:, h, half * 512:(half + 1) * 512],
                                    start=(h == 0), stop=(h == 7)), reads=[attT.res, wo_h.res], writes=[psY.res])
                            P.op("dve", lambda e, half=half, psY=psY, M=M: e.tensor_tensor(
                                out=tmp[:, half * 512:(half + 1) * 512], in0=psY[:], in1=M[2][:, half * 512:(half + 1) * 512],
                                op=ALU.mult), reads=[psY.res, M[2].res], writes=[tmp.res])
                        P.op("dve", lambda e, xa=xa: e.tensor_tensor(out=xa[:], in0=xa[:], in1=tmp[:], op=ALU.add),
                             reads=[xa.res, tmp.res], writes=[xa.res])
                        P.dma("sp", xs[t * 128:(t + 1) * 128, :], xa[:], reads=[xa.res], writes=[P.R("xs", t)])
            P.barrier()
            if stop_after == "attn":
                P.finish()
                return

            NB = 256
            blocks = [(i * NB, i == 0, i == L // NB - 1) for i in range(L // NB)] + [(L, True, True)]
            GC = sb("GC", [128, 2, 4, NCH], st=stL)
            with contextlib.ExitStack() as stR:
                rwv_t = sb("rwv_t", [128, 43], st=stR)
                P.dma("sp", rwv_t[:], rwv[li], writes=[rwv_t.res])
                omka = sb("omka", [128, 8], st=stR)
                P.op("dve", lambda e: e.tensor_scalar(out=omka[:, 0:4], in0=rwv_t[:, 4:8], scalar1=-1.0, scalar2=1.0,
                                                      op0=ALU.mult, op1=ALU.add), reads=[rwv_t.res], writes=[omka.res])
                P.op("dve", lambda e: e.tensor_scalar(out=omka[:, 4:8], in0=rwv_t[:, 4:8], scalar1=-2.0, scalar2=2.0,
                                                      op0=ALU.mult, op1=ALU.add), reads=[rwv_t.res], writes=[omka.res])
                c12 = sb("c12", [128, 1], st=stR)
                P.op("pool", lambda e: e.memset(c12[:], 1e-12), writes=[c12.res])
                c128 = sb("c128", [128, 386], st=stR)
                P.dma("sp", c128[:], cst128[:, :], writes=[c128.res])
                c64 = sb("c64", [64, 3584], BF16, st=stR)
                P.dma("act", c64[:], cst64[:, :], writes=[c64.res])
                BDo, SEL, RST = c128[:, 0:128], c128[:, 128:130], c128[:, 130:386]
                selrk = sb("selrk", [128, 4, 2], st=stR)
                P.op("dve", lambda e: e.tensor_tensor(
                    out=selrk[:], in0=SEL.unsqueeze(1).to_broadcast([128, 4, 2]),
                    in1=rwv_t[:, 8:12].unsqueeze(2).to_broadcast([128, 4, 2]), op=ALU.mult),
                    reads=[c128.res, rwv_t.res], writes=[selrk.res])
                lw3 = []
                wstg = sb("wstg", [128, 512], st=stR)
                for nm, src in (("w2b", rw_w2), ("a2b", rw_a2), ("g2b", rw_g2)):
                    wt = sb(nm, [128, 512], BF16, st=stR)
                    P.dma("sp", wstg[:], src[li], writes=[wstg.res])
                    P.op("pool", lambda e, wt=wt: e.tensor_copy(out=wt[:], in_=wstg[:]), reads=[wstg.res], writes=[wt.res])
                    lw3.append(wt)
                w2b, a2b, g2b = lw3
                PH = sb("PH", [128, 15, NB + 2], st=stR)
                S = sb("S", [128, 15, NB], st=stR)
                KKN = sb("KKN", [128, 4, NB], st=stR)
                LW = sb("LW", [128, 4, NB], st=stR)
                A2 = [sb("A%d" % d, [128, 4, NB], st=stR) for d in range(2)]
                KD = sb("KD", [128, 4, NB], st=stR)
                BDt = sb("BDt", [128, 4, NB], st=stR)
                CU = sb("CU", [128, 4, NB], st=stR)
                EXC = sb("EXC", [128, 4, NB], st=stR)
                EP, EN, EX = (sb(n, [128, 4, NB], st=stR) for n in ("EP", "EN", "EX"))
                TW = sb("TW", [128, NB], BF16, st=stR)
                ADb = sb("ADb", [128, NB], BF16, st=stR)
                SG = sb("SG", [128, NB], BF16, st=stR)
                vb = sb("vb", [128, 4, NB], BF16, st=stR)
                gst = [sb("gst%d" % i, [128, 512], st=stR) for i in range(1)]
                FMs = sb("FMs", [128, 4, 4, 4, 64], BF16, st=stR)
                TMs = sb("TMs", [64, 4, 2, 512], BF16, st=stR)
                Vs = sb("Vs", [64, 4, 512], BF16, st=stR)
                BQm = [sb("BQm%d" % n, [64, 8, 2, 64], BF16, st=stR) for n in range(4)]
                KQm = [sb("KQm%d" % n, [64, 8, 2, 64], BF16, st=stR) for n in range(4)]
                Pm = [[sb("Pm%d_%d" % (n, i), [64, 8, 64], BF16, st=stR) for i in range(2)] for n in range(4)]
                PTm = [[sb("PTm%d_%d" % (n, i), [64, 8, 64], BF16, st=stR) for i in range(2)] for n in range(4)]
                WTm = [[sb("WTm%d_%d" % (n, i), [64, 8, 64], BF16, st=stR) for i in range(2)] for n in range(4)]
                bst_ = sb("bst_", [128, 16], st=stR)
                ev = [0]

                def evac(fn_act, fn_dve, reads, writes):
                    if ev[0] % 2 == 0:
                        P.op("act", fn_act, reads=reads, writes=writes)
                    else:
                        P.op("dve", fn_dve, reads=reads, writes=writes)
                    ev[0] += 1

                mskB = [c64[:, d * 512:(d + 1) * 512] for d in range(2)]
                mskK = [c64[:, 1024 + d * 512:1024 + (d + 1) * 512] for d in range(2)]
                mskN = [c64[:, 2048 + d * 512:2048 + (d + 1) * 512] for d in range(2)]
                id8 = c64[:, 3072:3584]
                pTv = pT.rearrange("(c p) t -> p c t", p=128)
                import os as _os
                CUT = int(_os.environ.get("R1A_CUT", "0"))
                cut_hit = [False]

                def cut(k):
                    if CUT == k:
                        cut_hit[0] = True
                    return cut_hit[0]
                for bi, (g0, s0, s1) in enumerate(blocks):
                    if cut_hit[0]:
                        break
                    ch0 = g0 // CH
                    grp_lo, grp_hi = max(g0 - 1, 0) // 512, min(g0 + NB, T - 1) // 512
                    rd = [P.R("pT", g) for g in range(grp_lo, grp_hi + 1)]
                    lo = g0 - (0 if s0 else 1)
                    hi = g0 + NB + (0 if s1 else 1)
                    off = 1 if s0 else 0
                    if s0:
                        P.op("pool", lambda e: e.memset(PH[:, :, 0:1], 0.0), writes=[PH.res])
                    if s1:
                        P.op("pool", lambda e: e.memset(PH[:, :, NB + 1:NB + 2], 0.0), writes=[PH.res])
                    for c3 in range(5):
                        P.dma("sp" if c3 % 2 == 0 else "act", PH[:, 3 * c3:3 * c3 + 3, off:off + hi - lo], pTv[:, 3 * c3:3 * c3 + 3, lo:hi],
                              reads=rd, writes=[PH.res])
                    pc_ = PH[:, :, 1:NB + 1]
                    P.op("pool", lambda e: e.tensor_tensor(out=S[:], in0=PH[:, :, 0:NB], in1=PH[:, :, 2:NB + 2], op=ALU.add),
                         reads=[PH.res], writes=[S.res])
                    P.op("dve", lambda e, pc_=pc_: e.scalar_tensor_tensor(out=S[:], in0=S[:], scalar=0.5, in1=pc_,
                                                                         op0=ALU.mult, op1=ALU.subtract),
                         reads=[S.res, PH.res], writes=[S.res])
                    P.op("pool", lambda e: e.tensor_tensor(out=S[:], in0=S[:],
                                                           in1=rwv_t[:, 28:43].unsqueeze(2).to_broadcast([128, 15, NB]), op=ALU.mult),
                         reads=[S.res, rwv_t.res], writes=[S.res])
                    P.op("dve", lambda e, pc_=pc_: e.tensor_tensor(out=S[:], in0=S[:], in1=pc_, op=ALU.add),
                         reads=[S.res, PH.res], writes=[S.res])
                    r_, k_, v_ = S[:, 0:4, :], S[:, 4:8, :], S[:, 8:12, :]
                    if cut(1):
                        break
                    for c in range(4):
                        P.op("dve", lambda e, c=c: e.tensor_scalar(out=KKN[:, c, :], in0=S[:, 4 + c, :], scalar1=rwv_t[:, c:c + 1],
                                                                   scalar2=None, op0=ALU.mult),
                             reads=[S.res, rwv_t.res], writes=[KKN.res])
                    P.op("act", lambda e: e.activation(out=EX[:], in_=KKN[:], func=AF.Square), reads=[KKN.res], writes=[EX.res])
                    for hf_ in range(2):
                        ps = next_ps()
                        for cc in range(2):
                            c = 2 * hf_ + cc
                            P.op("pe", lambda e, ps=ps, c=c, cc=cc: e.matmul(ps[:, cc * NB:(cc + 1) * NB], BDo, EX[:, c, :],
                                                                            start=True, stop=True),
                                 reads=[c128.res, EX.res], writes=[ps.res])
                        P.op("act", lambda e, ps=ps, hf_=hf_: e.activation(
                            out=EP[:, 2 * hf_:2 * hf_ + 2, :], in_=ps[:].rearrange("p (c n) -> p c n", c=2), func=AF.Sqrt,
                            bias=c12[:, 0:1], scale=1.0), reads=[ps.res, c12.res], writes=[EP.res])
                    P.op("dve", lambda e: e.reciprocal(out=EP[:], in_=EP[:]), reads=[EP.res], writes=[EP.res])
                    P.op("dve", lambda e: e.tensor_tensor(out=KKN[:], in0=KKN[:], in1=EP[:], op=ALU.mult),
                         reads=[KKN.res, EP.res], writes=[KKN.res])
                    if cut(2):
                        break
                    P.op("act", lambda e: e.activation(out=TW[:], in_=S[:, 12, :], func=AF.Tanh), reads=[S.res], writes=[TW.res])
                    P.op("act", lambda e: e.copy(out=ADb[:], in_=S[:, 13, :]), reads=[S.res], writes=[ADb.res])
                    P.op("act", lambda e: e.activation(out=SG[:], in_=S[:, 14, :], func=AF.Sigmoid), reads=[S.res], writes=[SG.res])
                    P.op("act", lambda e, v_=v_: e.copy(out=vb[:], in_=v_), reads=[S.res], writes=[vb.res])
                    for tt in range(NB // 128):
                        ps = next_ps()
                        P.op("pe", lambda e, ps=ps, tt=tt: e.matmul(ps[:], SG[:, tt * 128:(tt + 1) * 128], g2b[:], start=True, stop=True),
                             reads=[SG.res, g2b.res], writes=[ps.res])
                        gs = gst[0]
                        evac(lambda e, ps=ps, gs=gs: e.copy(out=gs[:], in_=ps[:]),
                             lambda e, ps=ps, gs=gs: e.tensor_copy(out=gs[:], in_=ps[:]), [ps.res], [gs.res])
                        P.dma("pool", gtm[g0 + tt * 128:g0 + (tt + 1) * 128, :], gs[:], reads=[gs.res],
                              writes=[P.R("gtm", (g0 + tt * 128) // 128)])
                    if cut(3):
                        break
                    for d in range(2):
                        A = A2[d]
                        for c in range(4):
                            ps = next_ps()
                            P.op("pe", lambda e, ps=ps, c=c, d=d: e.matmul(
                                ps[:, 0:NB], w2b[d * 64:(d + 1) * 64, c * 128:(c + 1) * 128], TW[d * 64:(d + 1) * 64, :],
                                start=True, stop=True), reads=[w2b.res, TW.res], writes=[ps.res])
                            P.op("pe", lambda e, ps=ps, c=c, d=d: e.matmul(
                                ps[:, NB:2 * NB], a2b[d * 64:(d + 1) * 64, c * 128:(c + 1) * 128], ADb[d * 64:(d + 1) * 64, :],
                                start=True, stop=True), reads=[a2b.res, ADb.res], writes=[ps.res])
                            P.op("act", lambda e, ps=ps, c=c, d=d: e.activation(
                                out=LW[:, c, :], in_=ps[:, 0:NB], func=AF.Sigmoid, bias=rwv_t[:, 12 + 4 * d + c:13 + 4 * d + c], scale=1.0),
                                reads=[ps.res, rwv_t.res], writes=[LW.res])
                            P.op("act", lambda e, ps=ps, c=c, d=d, A=A: e.activation(
                                out=A[:, c, :], in_=ps[:, NB:2 * NB], func=AF.Sigmoid, bias=rwv_t[:, 20 + 4 * d + c:21 + 4 * d + c], scale=1.0),
                                reads=[ps.res, rwv_t.res], writes=[A.res])
                            P.op("dve", lambda e, c=c, A=A: e.tensor_scalar(
                                out=KD[:, c, :], in0=A[:, c, :], scalar1=rwv_t[:, 4 + c:5 + c], scalar2=omka[:, c:c + 1],
                                op0=ALU.mult, op1=ALU.add), reads=[A.res, rwv_t.res, omka.res], writes=[KD.res])
                            P.op("dve", lambda e, c=c: e.tensor_tensor_scan(
                                out=CU[:, c, :], data0=RST, data1=LW[:, c, :], initial=0.0, op0=ALU.mult, op1=ALU.add),
                                reads=[c128.res, LW.res], writes=[CU.res])
                        P.op("pool", lambda e, k_=k_: e.tensor_tensor(out=KD[:], in0=KD[:], in1=k_, op=ALU.mult),
                             reads=[KD.res, S.res], writes=[KD.res])
                        P.op("pool", lambda e, A=A: e.tensor_tensor(out=BDt[:], in0=KKN[:], in1=A[:], op=ALU.mult),
                             reads=[KKN.res, A.res], writes=[BDt.res])
                        cu4 = CU[:].rearrange("p c (n t) -> p c n t", t=CH)
                        if cut(4):
                            break
                        if d == 0:
                            P.op("pool", lambda e: e.tensor_tensor(out=EXC[:], in0=CU[:], in1=LW[:], op=ALU.subtract),
                                 reads=[CU.res, LW.res], writes=[EXC.res])
                            INC = CU
                        else:
                            P.op("pool", lambda e, cu4=cu4: e.tensor_tensor(
                                out=EXC[:].rearrange("p c (n t) -> p c n t", t=CH),
                                in0=cu4[:, :, :, CH - 1:CH].to_broadcast([128, 4, NB // CH, CH]), in1=cu4, op=ALU.subtract),
                                reads=[CU.res], writes=[EXC.res])
                            INC = None
                        P.op("act", lambda e, cu4=cu4, d=d, ch0=ch0: e.activation(
                            out=GC[:, d, :, ch0:ch0 + NB // CH], in_=cu4[:, :, :, CH - 1], func=AF.Exp, scale=-DECAY_C),
                            reads=[CU.res], writes=[GC.res])
                        if INC is None:
                            P.op("pool", lambda e: e.tensor_tensor(out=CU[:], in0=EXC[:], in1=LW[:], op=ALU.add),
                                 reads=[EXC.res, LW.res], writes=[CU.res])
                            INC = CU
                        P.op("act", lambda e, INC=INC: e.activation(out=EP[:], in_=INC[:], func=AF.Exp, scale=-DECAY_C),
                             reads=[INC.res], writes=[EP.res])
                        P.op("act", lambda e, INC=INC: e.activation(out=EN[:], in_=INC[:], func=AF.Exp, scale=DECAY_C),
                             reads=[INC.res], writes=[EN.res])
                        P.op("act", lambda e: e.activation(out=EX[:], in_=EXC[:], func=AF.Exp, scale=-DECAY_C),
                             reads=[EXC.res], writes=[EX.res])

                        def fm_out(arr):
                            return FMs[:, :, arr, :, :].rearrange("p n c t -> p c n t")

                        def v4(tl):
                            return tl[:].rearrange("p c (n t) -> p c n t", t=CH)
                        P.op("pool", lambda e: e.tensor_tensor(out=fm_out(0), in0=v4(KKN), in1=v4(EX), op=ALU.mult),
                             reads=[KKN.res, EX.res], writes=[FMs.res])
                        P.op("dve", lambda e, r_=r_: e.tensor_tensor(out=fm_out(1), in0=r_.rearrange("p c (n t) -> p c n t", t=CH),
                                                                     in1=v4(EP), op=ALU.mult),
                             reads=[S.res, EP.res], writes=[FMs.res])
                        P.op("pool", lambda e: e.tensor_tensor(out=fm_out(2), in0=v4(KD), in1=v4(EN), op=ALU.mult),
                             reads=[KD.res, EN.res], writes=[FMs.res])
                        P.op("dve", lambda e: e.tensor_tensor(out=fm_out(3), in0=v4(BDt), in1=v4(EN), op=ALU.mult),
                             reads=[BDt.res, EN.res], writes=[FMs.res])
                        for hh in range(2):
                            P.dma("sp" if hh == 0 else "act", FM[d][ch0:ch0 + 4][:, :, hh, :].rearrange("n k f -> k n f"),
                                  FMs[hh * 64:(hh + 1) * 64, :, 0:2, :, :].rearrange("p n a c t -> p n (a c t)"),
                                  reads=[FMs.res], writes=[P.R("FM", d, bi)])
                        if cut(5):
                            break
                        for n in range(4):
                            for a in range(2):
                                ps = next_ps()
                                for c in range(4):
                                    P.op("pe", lambda e, ps=ps, n=n, a=a, c=c: e.transpose(
                                        out=ps[:].bitcast(BF16)[0:64, c * 128:(c + 1) * 128], in_=FMs[:, n, 2 + a, c, :],
                                        identity=ident_b[:]), reads=[FMs.res, ident_b.res], writes=[ps.res])
                                sgn = 1.0 if a == 0 else -1.0
                                evac(lambda e, ps=ps, n=n, a=a, sgn=sgn: e.mul(out=TMs[:, n, a, :], in_=ps[:].bitcast(BF16)[0:64, 0:512], mul=sgn),
                                     lambda e, ps=ps, n=n, a=a, sgn=sgn: e.tensor_scalar(out=TMs[:, n, a, :], in0=ps[:].bitcast(BF16)[0:64, 0:512],
                                                                                           scalar1=sgn, scalar2=None, op0=ALU.mult),
                                     [ps.res], [TMs.res])
                            if d == 0:
                                ps = next_ps()
                                for c in range(4):
                                    P.op("pe", lambda e, ps=ps, n=n, c=c: e.transpose(
                                        out=ps[:].bitcast(BF16)[0:64, c * 128:(c + 1) * 128], in_=vb[:, c, n * CH:(n + 1) * CH],
                                        identity=ident_b[:]), reads=[vb.res, ident_b.res], writes=[ps.res])
                                evac(lambda e, ps=ps, n=n: e.copy(out=Vs[:, n, :], in_=ps[:].bitcast(BF16)[0:64, 0:512]),
                                     lambda e, ps=ps, n=n: e.tensor_copy(out=Vs[:, n, :], in_=ps[:].bitcast(BF16)[0:64, 0:512]),
                                     [ps.res], [Vs.res])
                        if _os.environ.get("NO_TMDMA") is None:
                            P.dma("act", TM[d][ch0:ch0 + 4].rearrange("n p f -> p n f"), TMs[:].rearrange("p n a f -> p n (a f)"),
                                  reads=[TMs.res], writes=[P.R("TM", d, bi)])
                            if d == 0:
                                P.dma("sp", VM[ch0:ch0 + 4].rearrange("n p f -> p n f"), Vs[:], reads=[Vs.res], writes=[P.R("VM", bi)])
                        if cut(6):
                            break
                        for n in range(4):
                            for hh in range(2):
                                sl = slice(hh * 64, (hh + 1) * 64)
                                psb, psk, psn = next_ps(), next_ps(), next_ps()
                                for c in range(4):
                                    P.op("pe", lambda e, psb=psb, n=n, c=c, sl=sl: e.matmul(
                                        psb[0:64, c * 128:(c + 1) * 128], FMs[sl, n, 3, c, :], FMs[sl, n, 0:2, c, :],
                                        start=True, stop=True), reads=[FMs.res], writes=[psb.res])
                                    P.op("pe", lambda e, psk=psk, n=n, c=c, sl=sl: e.matmul(
                                        psk[0:64, c * 128:(c + 1) * 128], FMs[sl, n, 2, c, :], FMs[sl, n, 0:2, c, :],
                                        start=True, stop=True), reads=[FMs.res], writes=[psk.res])
                                    P.op("pe", lambda e, psn=psn, n=n, c=c, sl=sl: e.matmul(
                                        psn[0:64, c * 64:(c + 1) * 64], FMs[sl, n, 0, c, :], FMs[sl, n, 3, c, :],
                                        start=True, stop=True), reads=[FMs.res], writes=[psn.res])
                                bq = BQm[n][:].rearrange("p (c hh) a t -> p c hh a t", hh=2)[:, :, hh, :, :]
                                kq = KQm[n][:].rearrange("p (c hh) a t -> p c hh a t", hh=2)[:, :, hh, :, :]
                                pm = Pm[n][0][:].rearrange("p (c hh) t -> p c hh t", hh=2)[:, :, hh, :]
                                P.op("dve", lambda e, psb=psb, bq=bq, d=d: e.tensor_tensor(
                                    out=bq, in0=psb[0:64, :].rearrange("p (c a t) -> p c a t", c=4, a=2),
                                    in1=mskB[d].rearrange("p (c a t) -> p c a t", c=4, a=2), op=ALU.mult),
                                    reads=[psb.res, c64.res], writes=[BQm[n].res])
                                P.op("dve", lambda e, psk=psk, kq=kq, d=d: e.tensor_tensor(
                                    out=kq, in0=psk[0:64, :].rearrange("p (c a t) -> p c a t", c=4, a=2),
                                    in1=mskK[d].rearrange("p (c a t) -> p c a t", c=4, a=2), op=ALU.mult),
                                    reads=[psk.res, c64.res], writes=[KQm[n].res])
                                P.op("dve", lambda e, psn=psn, pm=pm, d=d: e.tensor_tensor(
                                    out=pm, in0=psn[0:64, 0:256].rearrange("p (c t) -> p c t", c=4),
                                    in1=mskN[d][:, 0:256].rearrange("p (c t) -> p c t", c=4), op=ALU.mult),
                                    reads=[psn.res, c64.res], writes=[Pm[n][0].res])
                            P.op("dve", lambda e, n=n: e.tensor_tensor(
                                out=WTm[n][0][:], in0=id8.rearrange("p (h t) -> p h t", h=8), in1=BQm[n][:, :, 0, :], op=ALU.subtract),
                                reads=[c64.res, BQm[n].res], writes=[WTm[n][0].res])
                        if cut(7):
                            break
                        for j in range(1, 6):
                            cur, prv = j % 2, (j - 1) % 2
                            for n in range(4):
                                Pp = Pm[n][prv]
                                if j == 1:
                                    def PTp(h, n=n):
                                        return BQm[n][:, h, 0, :]
                                    ptres = BQm[n].res
                                else:
                                    def PTp(h, n=n, prv=prv):
                                        return PTm[n][prv][:, h, :]
                                    ptres = PTm[n][prv].res
                                ps = next_ps()
                                for h in range(8):
                                    P.op("pe", lambda e, ps=ps, h=h, Pp=Pp, PTp=PTp: e.matmul(
                                        ps[0:64, h * 64:(h + 1) * 64], PTp(h), Pp[:, h, :], start=True, stop=True),
                                        reads=[Pp.res, ptres], writes=[ps.res])
                                Pn = Pm[n][cur]
                                evac(lambda e, ps=ps, Pn=Pn: e.copy(out=Pn[:].rearrange("p h t -> p (h t)"), in_=ps[0:64, :]),
                                     lambda e, ps=ps, Pn=Pn: e.tensor_copy(out=Pn[:].rearrange("p h t -> p (h t)"), in_=ps[0:64, :]),
                                     [ps.res], [Pn.res])
                                if j < 5:
                                    ps2 = next_ps()
                                    for h in range(8):
                                        P.op("pe", lambda e, ps2=ps2, h=h, Pp=Pp, PTp=PTp: e.matmul(
                                            ps2[0:64, h * 64:(h + 1) * 64], Pp[:, h, :], PTp(h), start=True, stop=True),
                                            reads=[Pp.res, ptres], writes=[ps2.res])
                                    PTn = PTm[n][cur]
                                    evac(lambda e, ps2=ps2, PTn=PTn: e.copy(out=PTn[:].rearrange("p h t -> p (h t)"), in_=ps2[0:64, :]),
                                         lambda e, ps2=ps2, PTn=PTn: e.tensor_copy(out=PTn[:].rearrange("p h t -> p (h t)"), in_=ps2[0:64, :]),
                                         [ps2.res], [PTn.res])
                            for n in range(4):
                                Pn, Wp, Wn = Pm[n][cur], WTm[n][prv], WTm[n][cur]
                                ps = next_ps()
                                for h in range(8):
                                    P.op("pe", lambda e, ps=ps, h=h, Pn=Pn, Wp=Wp: e.matmul(
                                        ps[0:64, h * 64:(h + 1) * 64], Pn[:, h, :], Wp[:, h, :], start=True, stop=True),
                                        reads=[Pn.res, Wp.res], writes=[ps.res])
                                P.op("dve", lambda e, ps=ps, Wp=Wp, Wn=Wn: e.tensor_tensor(
                                    out=Wn[:].rearrange("p h t -> p (h t)"), in0=ps[0:64, :], in1=Wp[:].rearrange("p h t -> p (h t)"),
                                    op=ALU.add), reads=[ps.res, Wp.res], writes=[Wn.res])
                        for n in range(4):
                            Wf = WTm[n][5 % 2]
                            res_ = P.R("MAT", d, ch0 + n)
                            P.dma("sp", MAT[d][ch0 + n][:, 0:512], Wf[:].rearrange("p h t -> p (h t)"), reads=[Wf.res], writes=[res_])
                            P.dma("act", MAT[d][ch0 + n][:, 512:1536], KQm[n][:].rearrange("p h a t -> p (h a t)"),
                                  reads=[KQm[n].res], writes=[res_])
                            P.dma("sp", MAT[d][ch0 + n][:, 1536:2560], BQm[n][:].rearrange("p h a t -> p (h a t)"),
                                  reads=[BQm[n].res], writes=[res_])
                        if cut(8):
                            break
                    if cut_hit[0]:
                        break
                    P.op("pool", lambda e: e.tensor_tensor(out=EXC[:], in0=A2[0][:], in1=A2[1][:], op=ALU.add),
                         reads=[A2[0].res, A2[1].res], writes=[EXC.res])
                    for c in range(4):
                        P.op("dve", lambda e, c=c: e.tensor_scalar(
                            out=EXC[:, c, :], in0=EXC[:, c, :], scalar1=rwv_t[:, 4 + c:5 + c], scalar2=omka[:, 4 + c:5 + c],
                            op0=ALU.mult, op1=ALU.add), reads=[EXC.res, rwv_t.res, omka.res], writes=[EXC.res])
                    P.op("pool", lambda e, k_=k_: e.tensor_tensor(out=EXC[:], in0=EXC[:], in1=k_, op=ALU.mult),
                         reads=[EXC.res, S.res], writes=[EXC.res])
                    P.op("dve", lambda e, r_=r_: e.tensor_tensor(out=EXC[:], in0=EXC[:], in1=r_, op=ALU.mult),
                         reads=[EXC.res, S.res], writes=[EXC.res])
                    ps = next_ps()
                    for tt in range(NB // 128):
                        for c in range(4):
                            P.op("pe", lambda e, ps=ps, tt=tt, c=c: e.matmul(
                                ps[:, tt * 8 + 2 * c:tt * 8 + 2 * c + 2], EXC[:, c, tt * 128:(tt + 1) * 128], selrk[:, c, :],
                                start=True, stop=True), reads=[EXC.res, selrk.res], writes=[ps.res])
                    P.op("dve", lambda e, ps=ps: e.tensor_copy(out=bst_[:], in_=ps[:, 0:16]), reads=[ps.res], writes=[bst_.res])
                    for tt in range(NB // 128):
                        P.dma("sp", bon[g0 + tt * 128:g0 + (tt + 1) * 128, :], bst_[:, tt * 8:(tt + 1) * 8], reads=[bst_.res],
                              writes=[P.R("bon", (g0 + tt * 128) // 128)])
                    if cut(9):
                        break
            P.barrier()
            if stop_after == "r1a":
                dG = nc.dram_tensor("dbg_GC", [128, 2, 4, NCH], F32, kind="ExternalOutput").ap()
                P.dma("sp", dG, GC[:], reads=[GC.res])
                P.finish()
                return

            GCd = dscr("GCd%d" % li, [128, 2, 4, NCH])
            P.dma("sp", GCd, GC[:], reads=[GC.res], writes=[P.R("GCd")])
            with contextlib.ExitStack() as stS:
                GC2 = sb("GC2", [64, 2, 4, 2, NCH], st=stS)
                for hh in range(2):
                    P.dma("sp" if hh == 0 else "act", GC2[:, :, :, hh, :], GCd[hh * 64:(hh + 1) * 64], reads=[P.R("GCd")], writes=[GC2.res])
                Zf = [sb("Zf%d" % d, [64, 8, 64], st=stS) for d in range(2)]
                Zb = [sb("Zb%d" % d, [64, 8, 64], BF16, st=stS) for d in range(2)]
                ztmp = sb("ztmp", [64, 8, 64], st=stS)
                for d in range(2):
                    P.op("pool", lambda e, d=d: e.memset(Zf[d][:], 0.0), writes=[Zf[d].res])
                    P.op("pool", lambda e, d=d: e.memset(Zb[d][:], 0.0), writes=[Zb[d].res])
                FMc = [[sb("FMc%d_%d" % (d, i), [64, 2, 512], BF16, st=stS) for i in range(2)] for d in range(2)]
                TMc = [[sb("TMc%d_%d" % (d, i), [64, 1024], BF16, st=stS) for i in range(2)] for d in range(2)]
                Vc = [[sb("Vc%d_%d" % (d, i), [64, 512], BF16, st=stS) for i in range(2)] for d in range(2)]
                MATc = [[sb("MATc%d_%d" % (d, i), [64, 2560], BF16, st=stS) for i in range(2)] for d in range(2)]
                Xs = [sb("Xs%d" % d, [64, 512], BF16, st=stS) for d in range(2)]
                Us = [sb("Us%d" % d, [64, 512], BF16, st=stS) for d in range(2)]
                Os = [[sb("Os%d_%d" % (d, i), [64, 512], st=stS) for i in range(2)] for d in range(2)]
                order = [list(range(64, 68)) + list(range(0, 64)), list(range(67, 63, -1)) + list(range(63, -1, -1))]
                def scan_load(step_, d_):
                    ch_ = order[d_][step_]
                    i_ = step_ % 2
                    b_ = ch_ // 4
                    P.dma("sp", FMc[d_][i_][:], FM[d_][ch_], reads=[P.R("FM", d_, b_)], writes=[FMc[d_][i_].res])
                    P.dma("act", TMc[d_][i_][:], TM[d_][ch_], reads=[P.R("TM", d_, b_)], writes=[TMc[d_][i_].res])
                    P.dma("act", Vc[d_][i_][:], VM[ch_], reads=[P.R("VM", b_)], writes=[Vc[d_][i_].res])
                    P.dma("sp", MATc[d_][i_][:], MAT[d_][ch_], reads=[P.R("MAT", d_, ch_)], writes=[MATc[d_][i_].res])
                scan_load(0, 0)
                scan_load(0, 1)
                for step in range(NCH):
                    for d in range(2):
                        ch = order[d][step]
                        i2 = step % 2
                        bi = ch // 4
                        fm, tm, vc, mat, osb = FMc[d][i2], TMc[d][i2], Vc[d][i2], MATc[d][i2], Os[d][i2]
                        if step + 1 < NCH:
                            scan_load(step + 1, d)
                        psX, psU, psO, psZ = psum[4 * d], psum[4 * d + 1], psum[4 * d + 2], psum[4 * d + 3]
                        zb, zf, xs_, us_ = Zb[d], Zf[d], Xs[d], Us[d]
                        for h in range(8):
                            c, hh = h // 2, h % 2
                            P.op("pe", lambda e, h=h, c=c, hh=hh, fm=fm, zb=zb, psX=psX: e.matmul(
                                psX[0:64, h * 64:(h + 1) * 64], fm[:, hh, c * 64:(c + 1) * 64], zb[:, h, :],
                                start=(h == 0), stop=False), reads=[fm.res, zb.res], writes=[psX.res])
                            P.op("pe", lambda e, h=h, mat=mat, vc=vc, psX=psX: e.matmul(
                                psX[0:64, h * 64:(h + 1) * 64], mat[:, 512 + h * 128:512 + h * 128 + 64], vc[:, h * 64:(h + 1) * 64],
                                start=False, stop=(h == 7)), reads=[mat.res, vc.res], writes=[psX.res])
                        P.op("act", lambda e, xs_=xs_, psX=psX: e.copy(out=xs_[:], in_=psX[0:64, :]), reads=[psX.res], writes=[xs_.res])
                        for h in range(8):
                            P.op("pe", lambda e, h=h, mat=mat, xs_=xs_, psU=psU: e.matmul(
                                psU[0:64, h * 64:(h + 1) * 64], mat[:, h * 64:(h + 1) * 64], xs_[:, h * 64:(h + 1) * 64],
                                start=(h == 0), stop=(h == 7)), reads=[mat.res, xs_.res], writes=[psU.res])
                        P.op("dve", lambda e, us_=us_, psU=psU: e.tensor_copy(out=us_[:], in_=psU[0:64, :]), reads=[psU.res], writes=[us_.res])
                        for h in range(8):
                            c, hh = h // 2, h % 2
                            P.op("pe", lambda e, h=h, c=c, hh=hh, fm=fm, zb=zb, psO=psO: e.matmul(
                                psO[0:64, h * 64:(h + 1) * 64], fm[:, hh, 256 + c * 64:256 + (c + 1) * 64], zb[:, h, :],
                                start=(h == 0), stop=False), reads=[fm.res, zb.res], writes=[psO.res])
                            P.op("pe", lambda e, h=h, mat=mat, vc=vc, psO=psO: e.matmul(
                                psO[0:64, h * 64:(h + 1) * 64], mat[:, 512 + h * 128 + 64:512 + h * 128 + 128], vc[:, h * 64:(h + 1) * 64],
                                start=False, stop=False), reads=[mat.res, vc.res], writes=[psO.res])
                            P.op("pe", lambda e, h=h, mat=mat, us_=us_, psO=psO: e.matmul(
                                psO[0:64, h * 64:(h + 1) * 64], mat[:, 1536 + h * 128 + 64:1536 + h * 128 + 128], us_[:, h * 64:(h + 1) * 64],
                                start=False, stop=(h == 7)), reads=[mat.res, us_.res], writes=[psO.res])
                        P.op("act", lambda e, osb=osb, psO=psO: e.copy(out=osb[:], in_=psO[0:64, :]), reads=[psO.res], writes=[osb.res])
                        P.dma("act", Od[d][ch * CH:(ch + 1) * CH, :], osb[:], reads=[osb.res], writes=[P.R("Od", d, ch)])
                        for h in range(8):
                            P.op("pe", lambda e, h=h, tm=tm, vc=vc, psZ=psZ: e.matmul(
                                psZ[0:64, h * 64:(h + 1) * 64], tm[:, h * 64:(h + 1) * 64], vc[:, h * 64:(h + 1) * 64],
                                start=(h == 0), stop=False), reads=[tm.res, vc.res], writes=[psZ.res])
                            P.op("pe", lambda e, h=h, tm=tm, us_=us_, psZ=psZ: e.matmul(
                                psZ[0:64, h * 64:(h + 1) * 64], tm[:, 512 + h * 64:512 + (h + 1) * 64], us_[:, h * 64:(h + 1) * 64],
                                start=False, stop=(h == 7)), reads=[tm.res, us_.res], writes=[psZ.res])
                        P.op("dve", lambda e, psZ=psZ, zf=zf: e.tensor_tensor(
                            out=ztmp[:], in0=psZ[0:64, :].rearrange("p (h v) -> p h v", h=8), in1=zf[:], op=ALU.add),
                            reads=[psZ.res, zf.res], writes=[ztmp.res])
                        gcv = GC2[:, d, :, :, ch:ch + 1].rearrange("p c hh o -> p (c hh) o").to_broadcast([64, 8, 64])
                        P.op("dve", lambda e, zf=zf, gcv=gcv: e.tensor_tensor(out=zf[:], in0=ztmp[:], in1=gcv, op=ALU.mult),
                             reads=[ztmp.res, GC2.res], writes=[zf.res])
                        P.op("act", lambda e, zb=zb, zf=zf: e.copy(out=zb[:], in_=zf[:]), reads=[zf.res], writes=[zb.res])
            P.barrier()
            if stop_after == "scan":
                P.finish()
                return

            with contextlib.ExitStack() as stO:
                load_cast = make_loader(stO)
                wo_b = sb("wo_b2", [128, 8, D], BF16, st=stO)
                load_cast(wo_b, lambda c0, c1: w_out[li].rearrange("(kc p) n -> p kc n", p=128)[:, :, c0:c1], D, 224)
                gnw = sb("gnw", [128, 2, 512], st=stO)
                P.dma("sp", gnw[:].rearrange("p a f -> p (a f)"), gn_wb[li].rearrange("a f -> (a f)").partition_broadcast(128),
                      writes=[gnw.res])
                gne = sb("gne", [128, 1], st=stO)
                P.op("pool", lambda e: e.memset(gne[:], GN_EPS), writes=[gne.res])
                o0 = [sb("o0_%d" % i, [128, 8, 64], st=stO) for i in range(2)]
                o1 = [sb("o1_%d" % i, [128, 8, 64], st=stO) for i in range(2)]
                vt2 = [sb("vt%d" % i, [128, 8, 64], BF16, st=stO) for i in range(2)]
                gt2 = [sb("gt%d" % i, [128, 512], st=stO) for i in range(2)]
                bt2 = [sb("bt%d" % i, [128, 8], st=stO) for i in range(2)]
                xr2 = [sb("xr%d" % i, [128, D], st=stO) for i in range(2)]
                sqr = sb("sqr", [128, 8, 64], st=stO)
                tb = sb("tb", [128, 8, 64], st=stO)
                stt = sb("stt", [128, 32], st=stO)
                rwb2 = [sb("rwb%d" % i, [128, 512], BF16, st=stO) for i in range(2)]
                rwT2 = [sb("rwT%d" % i, [128, 4, 128], BF16, st=stO) for i in range(2)]
                tmpy = sb("tmpy2", [128, D], st=stO)
                VMf = VM.rearrange("n p f -> (n p) f")
                for t in range(NLT if last else NT):
                    which = 0 if t < NLT else 1
                    M = mods[which]
                    a0, a1, vt, gt, bt, xr, rwb, rwT = o0[t % 2], o1[t % 2], vt2[t % 2], gt2[t % 2], bt2[t % 2], xr2[t % 2], rwb2[t % 2], rwT2[t % 2]
                    rows = slice(t * 128, (t + 1) * 128)

                    def ro_load(t_):
                        r_ = slice(t_ * 128, (t_ + 1) * 128)
                        i_ = t_ % 2
                        P.dma("sp", o0[i_][:].rearrange("p h v -> p (h v)"), Od[0][r_, :], reads=[P.R("Od", 0, 2 * t_), P.R("Od", 0, 2 * t_ + 1)], writes=[o0[i_].res])
                        P.dma("act", o1[i_][:].rearrange("p h v -> p (h v)"), Od[1][r_, :], reads=[P.R("Od", 1, 2 * t_), P.R("Od", 1, 2 * t_ + 1)], writes=[o1[i_].res])
                        P.dma("sp", vt2[i_][:].rearrange("p h v -> p (h v)"), VMf[r_, :], reads=[P.R("VM", (2 * t_) // 4)], writes=[vt2[i_].res])
                        P.dma("act", gt2[i_][:], gtm[r_, :], reads=[P.R("gtm", t_)], writes=[gt2[i_].res])
                        P.dma("sp", bt2[i_][:], bon[r_, :], reads=[P.R("bon", t_)], writes=[bt2[i_].res])
                        P.dma("act", xr2[i_][:], xs[r_, :], reads=[P.R("xs", t_)], writes=[xr2[i_].res])
                    if t == 0:
                        ro_load(0)
                    if t + 1 < (NLT if last else NT):
                        ro_load(t + 1)
                    P.op("pool", lambda e, a0=a0, a1=a1: e.tensor_tensor(out=a0[:], in0=a0[:], in1=a1[:], op=ALU.add),
                         reads=[a0.res, a1.res], writes=[a0.res])
                    P.op("dve", lambda e, a0=a0: e.tensor_reduce(out=stt[:, 0:8], in_=a0[:], axis=AX.X, op=ALU.add),
                         reads=[a0.res], writes=[stt.res])
                    P.op("dve", lambda e: e.tensor_scalar(out=stt[:, 8:16], in0=stt[:, 0:8], scalar1=1.0 / HD, scalar2=None, op0=ALU.mult),
                         reads=[stt.res], writes=[stt.res])
                    P.op("dve", lambda e, a0=a0: e.tensor_tensor(out=a0[:], in0=a0[:], in1=stt[:, 8:16].unsqueeze(2).to_broadcast([128, 8, 64]),
                                                                  op=ALU.subtract), reads=[a0.res, stt.res], writes=[a0.res])
                    P.op("act", lambda e, a0=a0: e.activation(out=sqr[:], in_=a0[:], func=AF.Square), reads=[a0.res], writes=[sqr.res])
                    P.op("dve", lambda e: e.tensor_reduce(out=stt[:, 16:24], in_=sqr[:], axis=AX.X, op=ALU.add),
                         reads=[sqr.res], writes=[stt.res])
                    P.op("act", lambda e: e.activation(out=stt[:, 24:32], in_=stt[:, 16:24], func=AF.Sqrt, bias=gne[:, 0:1], scale=1.0 / HD),
                         reads=[stt.res, gne.res], writes=[stt.res])
                    P.op("dve", lambda e: e.reciprocal(out=stt[:, 24:32], in_=stt[:, 24:32]), reads=[stt.res], writes=[stt.res])
                    P.op("dve", lambda e, a0=a0: e.tensor_tensor(out=a0[:], in0=a0[:], in1=stt[:, 24:32].unsqueeze(2).to_broadcast([128, 8, 64]),
                                                                  op=ALU.mult), reads=[a0.res, stt.res], writes=[a0.res])
                    a0f = a0[:].rearrange("p h v -> p (h v)")
                    P.op("pool", lambda e, a0=a0, a0f=a0f: e.tensor_tensor(out=a0f, in0=a0f, in1=gnw[:, 0, :], op=ALU.mult),
                         reads=[a0.res, gnw.res], writes=[a0.res])
                    P.op("pool", lambda e, a0=a0, a0f=a0f: e.tensor_tensor(out=a0f, in0=a0f, in1=gnw[:, 1, :], op=ALU.add),
                         reads=[a0.res, gnw.res], writes=[a0.res])
                    P.op("dve", lambda e, vt=vt, bt=bt: e.tensor_tensor(out=tb[:], in0=vt[:], in1=bt[:].unsqueeze(2).to_broadcast([128, 8, 64]),
                                                                        op=ALU.mult), reads=[vt.res, bt.res], writes=[tb.res])
                    P.op("pool", lambda e, a0=a0: e.tensor_tensor(out=a0[:], in0=a0[:], in1=tb[:], op=ALU.add),
                         reads=[a0.res, tb.res], writes=[a0.res])
                    P.op("dve", lambda e, a0f=a0f, a0=a0, gt=gt, rwb=rwb: e.tensor_tensor(out=rwb[:], in0=a0f, in1=gt[:], op=ALU.mult),
                         reads=[a0.res, gt.res], writes=[rwb.res])
                    psT = psum[0 + (t % 2)]
                    for c in range(4):
                        P.op("pe", lambda e, c=c, rwb=rwb, psT=psT: e.transpose(
                            out=psT[:].bitcast(BF16)[:, c * 128:(c + 1) * 128], in_=rwb[:, c * 128:(c + 1) * 128], identity=ident_b[:]),
                            reads=[rwb.res, ident_b.res], writes=[psT.res])
                    P.op("act", lambda e, rwT=rwT, psT=psT: e.copy(
                        out=rwT[:], in_=psT[:].bitcast(BF16)[:, 0:512].rearrange("p (c n) -> p c n", c=4)),
                        reads=[psT.res], writes=[rwT.res])
                    for half in range(2):
                        psY = psum[2 + 2 * (t % 2) + half]
                        for c in range(4):
                            P.op("pe", lambda e, c=c, half=half, psY=psY, rwT=rwT: e.matmul(
                                psY[:], rwT[:, c, :], wo_b[:, 4 + c, half * 512:(half + 1) * 512],
                                start=(c == 0), stop=(c == 3)), reads=[rwT.res, wo_b.res], writes=[psY.res])
                        P.op("dve", lambda e, half=half, psY=psY, M=M: e.tensor_tensor(
                            out=tmpy[:, half * 512:(half + 1) * 512], in0=psY[:], in1=M[2][:, half * 512:(half + 1) * 512],
                            op=ALU.mult), reads=[psY.res, M[2].res], writes=[tmpy.res])
                    P.op("pool", lambda e, xr=xr: e.tensor_tensor(out=xr[:], in0=xr[:], in1=tmpy[:], op=ALU.add),
                         reads=[xr.res, tmpy.res], writes=[xr.res])
                    P.dma("sp", xs[rows, :], xr[:], reads=[xr.res], writes=[P.R("xs", t)])
            P.barrier()
            if stop_after == "rw":
                P.finish()
                return

            is_moe = li % 2 == 1
            jj = li // 2
            n_tiles = NLT if last else NT
            G_all = sb("G_all", [128, NT, 8], st=stL)
            with contextlib.ExitStack() as stN:
                xt2 = [sb("nxt%d" % i, [128, D], st=stN) for i in range(2)]
                sq = sb("nsq", [128, D], st=stN)
                hf2 = sb("nhf2", [128, D], st=stN)
                hb2 = [sb("nhb%d" % i, [128, D], BF16, st=stN) for i in range(2)]
                hTg2 = [sb("nhTg%d" % i, [128, 8, 512], BF16, st=stN) for i in range(2)]
                st10 = sb("nst", [128, 48], st=stN)
                if is_moe:
                    wr = sb("wr", [128, 8, NE], st=stN)
                    P.dma("sp", wr[:], moe_router[jj].rearrange("(kc p) e -> p kc e", p=128), writes=[wr.res])
                    hTf = sb("hTf", [128, 8, 128], st=stN)
                for t in range(n_tiles):
                    which = 0 if t < NLT else 1
                    M = mods[which]
                    grp, tin = t // 4, t % 4
                    xt, hb, hTg = xt2[t % 2], hb2[t % 2], hTg2[grp % 2]
                    if t == 0:
                        P.dma("sp", xt[:], xs[0:128, :], reads=[P.R("xs", 0)], writes=[xt.res])
                    if t + 1 < n_tiles:
                        P.dma("sp", xt2[(t + 1) % 2][:], xs[(t + 1) * 128:(t + 2) * 128, :], reads=[P.R("xs", t + 1)],
                              writes=[xt2[(t + 1) % 2].res])
                    P.op("act", lambda e, xt=xt: e.activation(out=sq[:], in_=xt[:], func=AF.Square), reads=[xt.res], writes=[sq.res])
                    P.op("dve", lambda e: e.tensor_reduce(out=st10[:, 0:1], in_=sq[:], axis=AX.X, op=ALU.add),
                         reads=[sq.res], writes=[st10.res])
                    P.op("act", lambda e: e.activation(out=st10[:, 1:2], in_=st10[:, 0:1], func=AF.Sqrt, bias=eps_t[:, 0:1], scale=1.0 / D),
                         reads=[st10.res, eps_t.res], writes=[st10.res])
                    P.op("dve", lambda e: e.reciprocal(out=st10[:, 1:2], in_=st10[:, 1:2]), reads=[st10.res], writes=[st10.res])
                    P.op("dve", lambda e, xt=xt, M=M: e.scalar_tensor_tensor(
                        out=sq[:], in0=xt[:], scalar=st10[:, 1:2], in1=M[4][:], op0=ALU.mult, op1=ALU.mult),
                        reads=[xt.res, st10.res, M[4].res], writes=[sq.res])
                    if is_moe:
                        P.op("dve", lambda e, M=M: e.tensor_tensor(out=hf2[:], in0=sq[:], in1=M[3][:], op=ALU.add),
                             reads=[sq.res, M[3].res], writes=[hf2.res])
                        P.op("act", lambda e, hb=hb: e.copy(out=hb[:], in_=hf2[:]), reads=[hf2.res], writes=[hb.res])
                    else:
                        P.op("dve", lambda e, hb=hb, M=M: e.tensor_tensor(out=hb[:], in0=sq[:], in1=M[3][:], op=ALU.add),
                             reads=[sq.res, M[3].res], writes=[hb.res])
                    psT = psum[t % 2]
                    for kc in range(8):
                        P.op("pe", lambda e, kc=kc, psT=psT, hb=hb: e.transpose(
                            out=psT[:].bitcast(BF16)[:, kc * 128:(kc + 1) * 128], in_=hb[:, kc * 128:(kc + 1) * 128],
                            identity=ident_b[:]), reads=[hb.res, ident_b.res], writes=[psT.res])
                    P.op("act", lambda e, psT=psT, hTg=hTg, tin=tin: e.copy(
                        out=hTg[:, :, tin * 128:(tin + 1) * 128], in_=psT[:].bitcast(BF16).rearrange("p (k n) -> p k n", k=8)),
                        reads=[psT.res], writes=[hTg.res])
                    if is_moe:
                        for hf_ in range(2):
                            psF = psum[2 + hf_]
                            for k4 in range(4):
                                kc = 4 * hf_ + k4
                                P.op("pe", lambda e, psF=psF, k4=k4, kc=kc: e.transpose(
                                    out=psF[:, k4 * 128:(k4 + 1) * 128], in_=hf2[:, kc * 128:(kc + 1) * 128], identity=ident_f[:]),
                                    reads=[hf2.res, ident_f.res], writes=[psF.res])
                            P.op("dve" if hf_ == 0 else "act",
                                 (lambda e, psF=psF, hf_=hf_: e.tensor_copy(out=hTf[:, 4 * hf_:4 * hf_ + 4, :],
                                                                          in_=psF[:].rearrange("p (k n) -> p k n", k=4))) if hf_ == 0 else
                                 (lambda e, psF=psF, hf_=hf_: e.copy(out=hTf[:, 4 * hf_:4 * hf_ + 4, :],
                                                                    in_=psF[:].rearrange("p (k n) -> p k n", k=4))),
                                 reads=[psF.res], writes=[hTf.res])
                        psL = psum[4 + (t % 2)]
                        for kc in range(8):
                            P.op("pe", lambda e, kc=kc, psL=psL: e.matmul(psL[:, 0:NE], hTf[:, kc, :], wr[:, kc, :],
                                                                         start=(kc == 0), stop=(kc == 7)),
                                 reads=[hTf.res, wr.res], writes=[psL.res])
                        lg, m8, ng, ex, gm = st10[:, 8:16], st10[:, 16:24], st10[:, 24:25], st10[:, 32:40], st10[:, 40:48]
                        P.op("dve", lambda e, psL=psL, lg=lg: e.tensor_copy(out=lg, in_=psL[:, 0:NE]), reads=[psL.res], writes=[st10.res])
                        P.op("dve", lambda e, lg=lg, m8=m8: e.max(out=m8, in_=lg), reads=[st10.res], writes=[st10.res])
                        P.op("dve", lambda e, ng=ng, m8=m8: e.tensor_scalar(out=ng, in0=m8[:, 0:1], scalar1=-1.0, scalar2=None, op0=ALU.mult),
                             reads=[st10.res], writes=[st10.res])
                        P.op("act", lambda e, ex=ex, lg=lg, ng=ng: e.activation(out=ex, in_=lg, func=AF.Exp, bias=ng, scale=1.0),
                             reads=[st10.res], writes=[st10.res])
                        P.op("dve", lambda e, gm=gm, lg=lg, m8=m8, ex=ex: e.scalar_tensor_tensor(
                            out=gm, in0=lg, scalar=m8[:, 1:2], in1=ex, op0=ALU.is_ge, op1=ALU.mult),
                            reads=[st10.res], writes=[st10.res])
                        P.op("dve", lambda e, gm=gm: e.tensor_reduce(out=st10[:, 25:26], in_=gm, axis=AX.X, op=ALU.add),
                             reads=[st10.res], writes=[st10.res])
                        P.op("dve", lambda e: e.reciprocal(out=st10[:, 25:26], in_=st10[:, 25:26]), reads=[st10.res], writes=[st10.res])
                        P.op("dve", lambda e, gm=gm, t=t: e.tensor_scalar(out=G_all[:, t, :], in0=gm, scalar1=st10[:, 25:26], scalar2=None,
                                                                           op0=ALU.mult), reads=[st10.res], writes=[G_all.res])
                    if tin == 3 or t == n_tiles - 1:
                        N = (tin + 1) * 128
                        P.dma("act", H2[grp][:, :, 0:N], hTg[:, :, 0:N], reads=[hTg.res], writes=[P.R("H2", grp)])
            P.barrier()
            if stop_after == "norm2":
                dG = nc.dram_tensor("dbg_G", [128, NT, 8], F32, kind="ExternalOutput").ap()
                P.dma("sp", dG, G_all[:], reads=[G_all.res])
                P.finish()
                return

            groups = [(g, g * 512, 512, 0) for g in range(8)] + ([] if last else [(8, L, LC, 1)])
            if is_moe:
                experts = [(moe_wg[jj][e], moe_wu[jj][e], moe_wd[jj][e], DFE // 128, e) for e in range(NE)]
                split = [7, 7, 7, 7]
            else:
                experts = [(ffn_wg[jj], ffn_wu[jj], ffn_wd[jj], DFF // 128, None)]
                split = [8, 8, 6]
            subs = []
            for (wgs, wus, wds, nfc, e) in experts:
                f0 = 0
                for nf in split:
                    subs.append((wgs, wus, wds, f0, nf, e))
                    f0 += nf
                assert f0 == nfc
            with contextlib.ExitStack() as stF:
                load_cast = make_loader(stF)
                WG = [sb("WG%d" % i, [128, 8, 1024], BF16, st=stF) for i in range(2)]
                WU = [sb("WU%d" % i, [128, 8, 1024], BF16, st=stF) for i in range(2)]
                WD = [sb("WD%d" % i, [128, 8, 1024], BF16, st=stF) for i in range(2)]
                h2g = [sb("h2g%d" % i, [128, 8, 512], BF16, st=stF) for i in range(2)]
                aT = sb("aT", [128, 8, 512], BF16, st=stF)
                sgt = [sb("sgt%d" % i, [128, 512], st=stF) for i in range(2)]
                xf2 = [sb("xf%d" % i, [128, D], st=stF) for i in range(2)]
                dl2 = [sb("dl%d" % i, [128, D], st=stF) for i in range(2)]

                def load_tasks(si):
                    wgs, wus, wds, f0, nf, e = subs[si]
                    b = si % 2
                    tasks = []
                    for (dst, src) in ((WG[b], wgs), (WU[b], wus)):
                        v = src.rearrange("(kc p) n -> p kc n", p=128)
                        for c0 in range(0, nf * 128, 224):
                            c1 = min(nf * 128, c0 + 224)

                            tasks.append(lambda dst=dst, v=v, c0=c0, c1=c1, f0=f0: load_cast(
                                dst, lambda a, b_, v=v, f0=f0, c0=c0: v[:, :, f0 * 128 + c0 + a:f0 * 128 + c0 + b_], c1 - c0, 224, nk=8, col0=c0))
                    vd = wds[f0 * 128:(f0 + nf) * 128, :].rearrange("(f p) n -> p f n", p=128)
                    for c0 in range(0, D, 224):
                        c1 = min(D, c0 + 224)
                        tasks.append(lambda vd=vd, c0=c0, c1=c1, nf=nf, b=b: load_cast(
                            WD[b], lambda a, b_, vd=vd, c0=c0: vd[:, :, c0 + a:c0 + b_], c1 - c0, 224, nk=nf, col0=c0))
                    return tasks

                for tsk in load_tasks(0):
                    tsk()
                xrr = 0
                for si, (wgs, wus, wds, f0, nf, e) in enumerate(subs):
                    b = si % 2
                    nxt = load_tasks(si + 1) if si + 1 < len(subs) else []
                    per_g = (len(nxt) + len(groups) - 1) // len(groups)
                    tiles_seq = [(t0 // 128 + tt) for (g, t0, N, which) in groups for tt in range(N // 128)]

                    def load_hg(gi_):
                        g_, t0_, N_, w_ = groups[gi_]
                        P.dma("sp", h2g[gi_ % 2][:, :, 0:N_], H2[g_][:, :, 0:N_], reads=[P.R("H2", g_)], writes=[h2g[gi_ % 2].res])

                    def load_xf(k_):
                        t_ = tiles_seq[k_]
                        P.dma("act", xf2[k_ % 2][:], xs[t_ * 128:(t_ + 1) * 128, :], reads=[P.R("xs", t_)], writes=[xf2[k_ % 2].res])
                    load_hg(0)
                    load_xf(0)
                    xk = 0
                    for gi, (g, t0, N, which) in enumerate(groups):
                        M = mods[which]
                        hg = h2g[gi % 2]
                        if gi + 1 < len(groups):
                            load_hg(gi + 1)
                        for f in range(nf):
                            psG, psU = psum[(2 * f) % 4], psum[(2 * f + 1) % 4]
                            for kc in range(8):
                                P.op("pe", lambda e_, kc=kc, f=f, psG=psG, hg=hg, N=N, b=b: e_.matmul(
                                    psG[:, 0:N], WG[b][:, kc, f * 128:(f + 1) * 128], hg[:, kc, 0:N], start=(kc == 0), stop=(kc == 7)),
                                    reads=[WG[b].res, hg.res], writes=[psG.res])
                            for kc in range(8):
                                P.op("pe", lambda e_, kc=kc, f=f, psU=psU, hg=hg, N=N, b=b: e_.matmul(
                                    psU[:, 0:N], WU[b][:, kc, f * 128:(f + 1) * 128], hg[:, kc, 0:N], start=(kc == 0), stop=(kc == 7)),
                                    reads=[WU[b].res, hg.res], writes=[psU.res])
                            sg_ = sgt[f % 2]
                            P.op("act", lambda e_, sg_=sg_, psG=psG, N=N: e_.activation(out=sg_[:, 0:N], in_=psG[:, 0:N], func=AF.Silu),
                                 reads=[psG.res], writes=[sg_.res])
                            P.op("dve", lambda e_, sg_=sg_, psU=psU, f=f, N=N: e_.tensor_tensor(
                                out=aT[:, f, 0:N], in0=sg_[:, 0:N], in1=psU[:, 0:N], op=ALU.mult),
                                reads=[sg_.res, psU.res], writes=[aT.res])
                        for tt in range(N // 128):
                            t = t0 // 128 + tt
                            xf, dl = xf2[xk % 2], dl2[xk % 2]
                            rows = slice(t * 128, (t + 1) * 128)
                            for half in range(2):
                                psY = psum[4 + (2 * tt + half) % 4]
                                for f in range(nf):
                                    P.op("pe", lambda e_, f=f, tt=tt, half=half, psY=psY, b=b, nf=nf: e_.matmul(
                                        psY[:], aT[:, f, tt * 128:(tt + 1) * 128], WD[b][:, f, half * 512:(half + 1) * 512],
                                        start=(f == 0), stop=(f == nf - 1)), reads=[aT.res, WD[b].res], writes=[psY.res])
                                hs = slice(half * 512, (half + 1) * 512)
                                if e is None:
                                    P.op("dve", lambda e_, psY=psY, dl=dl, hs=hs, M=M: e_.tensor_tensor(
                                        out=dl[:, hs], in0=psY[:], in1=M[5][:, hs], op=ALU.mult),
                                        reads=[psY.res, M[5].res], writes=[dl.res])
                                else:
                                    P.op("dve", lambda e_, psY=psY, dl=dl, hs=hs, M=M, t=t, e=e: e_.scalar_tensor_tensor(
                                        out=dl[:, hs], in0=psY[:], scalar=G_all[:, t, e:e + 1], in1=M[5][:, hs],
                                        op0=ALU.mult, op1=ALU.mult), reads=[psY.res, M[5].res, G_all.res], writes=[dl.res])
                            P.op("dve", lambda e_, xf=xf, dl=dl: e_.tensor_tensor(out=xf[:], in0=xf[:], in1=dl[:], op=ALU.add),
                                 reads=[xf.res, dl.res], writes=[xf.res])
                            P.dma("sp", xs[rows, :], xf[:], reads=[xf.res], writes=[P.R("xs", t)])
                            xk += 1
                            if xk < len(tiles_seq):
                                load_xf(xk)
                        for tsk in nxt[gi * per_g:(gi + 1) * per_g]:
                            tsk()
            P.barrier()
            if stop_after == "ffn":
                P.finish()
                return
    for i in range(8):
        P.dma("sp" if i % 2 == 0 else "act", y_out[i * 512:(i + 1) * 512, :], xs[i * 512:(i + 1) * 512, :],
              reads=[P.R("xs", 4 * i + j) for j in range(4)], writes=[P.R("y", i)])
    P.finish()


def _prep_inputs(inputs):
    f = lambda a: np.ascontiguousarray(np.asarray(a, dtype=np.float32))
    com = {}
    for k in ("ada_w", "ada_b", "norm1_g", "norm2_g", "w_in", "w_out", "ffn_wg", "ffn_wu", "ffn_wd", "moe_router", "moe_wg", "moe_wu", "moe_wd"):
        com[k] = f(inputs[k])
    com["qk_gain"] = f(np.concatenate([inputs["q_gain"], inputs["k_gain"]], axis=1))
    rows = L // 64
    row = np.repeat(np.arange(rows, dtype=np.float32), 64)
    col = np.tile(np.arange(64, dtype=np.float32), rows)
    inv = (10000.0 ** (-np.arange(0, 32, 2, dtype=np.float32) / 32)).astype(np.float32)
    ang = np.concatenate([row[:, None] * inv, col[:, None] * inv], axis=-1).astype(np.float32)
    com["rope"] = f(np.concatenate([np.cos(ang), np.sin(ang)], axis=-1))
    com["ident"] = np.eye(128, dtype=np.float32)
    col = lambda v, n: np.asarray(v, np.float32).reshape(n, 128).T
    rwv = []
    for i in range(DEPTH):
        parts = [col(inputs["rw_kk"][i], 4), col(inputs["rw_ka"][i], 4), col(inputs["rw_rk"][i], 4),
                 col(inputs["rw_w0"][i][0], 4), col(inputs["rw_w0"][i][1], 4),
                 col(inputs["rw_a0"][i][0], 4), col(inputs["rw_a0"][i][1], 4), col(inputs["shift_mu"][i], 15)]
        rwv.append(np.concatenate(parts, axis=1))
    com["rwv"] = f(np.stack(rwv))
    com["rw_w2"] = f(np.asarray(inputs["rw_w2"]).reshape(DEPTH, 128, 512))
    com["rw_a2"] = f(np.asarray(inputs["rw_a2"]).reshape(DEPTH, 128, 512))
    com["rw_g2"] = f(inputs["rw_g2"])
    com["gn_wb"] = f(np.stack([inputs["rw_gn_w"], inputs["rw_gn_b"]], axis=1))
    c128 = np.zeros((128, 386), np.float32)
    c128[:64, :64] = 1.0
    c128[64:, 64:128] = 1.0
    c128[:64, 128] = 1.0
    c128[64:, 129] = 1.0
    c128[:, 130:386] = 1.0
    c128[:, 130:386:CH] = 0.0
    com["cst128"] = c128
    si, ti = np.meshgrid(np.arange(CH), np.arange(CH), indexing="ij")
    strict = [(si < ti), (si > ti)]
    incl = [(si <= ti), (si >= ti)]
    c64 = np.zeros((64, 7, 512), np.float32)
    for d in range(2):
        mb = np.stack([strict[d].astype(np.float32), -incl[d].astype(np.float32)], axis=1)
        mk = np.stack([strict[d].astype(np.float32), incl[d].astype(np.float32)], axis=1)
        c64[:, d] = np.tile(mb.reshape(64, 1, 128), (1, 4, 1)).reshape(64, 512)
        c64[:, 2 + d] = np.tile(mk.reshape(64, 1, 128), (1, 4, 1)).reshape(64, 512)
        mn = strict[1 - d].astype(np.float32)
        c64[:, 4 + d] = np.tile(mn.reshape(64, 1, 64), (1, 8, 1)).reshape(64, 512)
    c64[:, 6] = np.tile(np.eye(64, dtype=np.float32).reshape(64, 1, 64), (1, 8, 1)).reshape(64, 512)
    com["cst64"] = c64.reshape(64, 3584).astype(ml_dtypes.bfloat16)
    return com


def kernel(**inputs):
    com = _prep_inputs(inputs)
    x = np.asarray(inputs["x"], dtype=np.float32)
    ctx = np.asarray(inputs["ctx"], dtype=np.float32)
    c = np.asarray(inputs["c"], dtype=np.float32)
    c_ctx = np.asarray(inputs["c_ctx"], dtype=np.float32)
    nc = bass.Bass("TRN2", target_bir_lowering=False)
    build(nc)
    in_maps = []
    for b in range(8):
        m = dict(com)
        m["x"] = np.ascontiguousarray(x[b])
        m["ctx"] = np.ascontiguousarray(ctx[b])
        m["cvec"] = np.ascontiguousarray(
            np.concatenate([c[b].reshape(8, 128).T, c_ctx.reshape(8, 128).T], axis=1))
        in_maps.append(m)
    res = run_bass_kernel_spmd(nc, in_maps, core_ids=list(range(8)))
    return np.stack([r["y"] for r in res.results], axis=0)
```

```python
import contextlib
import numpy as np
import ml_dtypes
import concourse.bass as bass
import concourse.mybir as mybir
from concourse.bass_utils import run_bass_kernel_spmd

F32 = mybir.dt.float32
BF16 = mybir.dt.bfloat16
AF = mybir.ActivationFunctionType
ALU = mybir.AluOpType
AX = mybir.AxisListType

D = 1024
L = 4096
LC = 256
T = L + LC
NT = T // 128
NLT = L // 128
DEPTH = 4
HD = 64
ATT_IN = 768
SHIFT_W = 1920
IN_W = 2688
DFF = 2816
DFE = 3584
NE = 8
EPS = 1e-6
GN_EPS = 64e-5
CH = 64
NCH = T // CH
DECAY_C = float(np.exp(-0.5))


class Res:
    __slots__ = ("w", "r")

    def __init__(self):
        self.w = None
        self.r = {}


class Prog:
    LIMIT = 30000

    def __init__(self, nc, stack):
        self.nc = nc
        self.stack = stack
        self.eng = {"pe": nc.tensor, "act": nc.scalar, "dve": nc.vector, "pool": nc.gpsimd, "sp": nc.sync}
        self.sem = {}
        self.val = {}
        self.cur = {}
        self.nsem = 0
        self.waited = {e: {} for e in self.eng}
        for e in ("pe", "act", "dve", "pool"):
            self._fresh(e)
        self.slots = {}
        self.rr = {}
        for q, n in (("sp", 8), ("act", 4), ("pool", 6)):
            self.slots[q] = []
            self.rr[q] = 0
            for i in range(n):
                b = "d_%s%d" % (q, i)
                self._fresh(b)
                self.slots[q].append(b)
        self.dram_res = {}
        self.n_inst = 0

    def _fresh(self, base):
        ep = self.cur[base][1] + 1 if base in self.cur else 0
        k = (base, ep)
        self.nsem += 1
        self.sem[k] = self.stack.enter_context(self.nc.semaphore("s%d_%s_%d" % (self.nsem, base, ep)))
        self.val[k] = 0
        self.cur[base] = k
        return k

    def R(self, *key):
        r = self.dram_res.get(key)
        if r is None:
            r = self.dram_res[key] = Res()
        return r

    def _deps(self, eng, reads, writes, extra=()):
        need = {}
        for k, v in extra:
            if v > need.get(k, 0):
                need[k] = v
        for t in reads:
            if t.w is not None:
                k, v = t.w
                if v > need.get(k, 0):
                    need[k] = v
        for t in writes:
            if t.w is not None:
                k, v = t.w
                if k[0] != eng and v > need.get(k, 0):
                    need[k] = v
            for k, v in t.r.items():
                if k[0] != eng and v > need.get(k, 0):
                    need[k] = v
        wd = self.waited[eng]
        e = self.eng[eng]
        for k, v in need.items():
            if k[0] == eng and eng == "pe":
                continue
            if wd.get(k, 0) >= v:
                continue
            wd[k] = v
            e.wait_ge(self.sem[k], v)
            self.n_inst += 1

    def _mark(self, ev, reads, writes):
        k, v = ev
        for t in reads:
            if v > t.r.get(k, 0):
                t.r[k] = v
        for t in writes:
            t.w = ev
            t.r = {}

    def op(self, eng, fn, reads=(), writes=()):
        self._deps(eng, reads, writes)
        ins = fn(self.eng[eng])
        k = self.cur[eng]
        if self.val[k] >= self.LIMIT:
            k = self._fresh(eng)
        self.val[k] += 1
        ins.then_inc(self.sem[k], 1)
        self.n_inst += 1
        self._mark((k, self.val[k]), reads, writes)

    def dma(self, q, out, in_, reads=(), writes=(), **kw):
        sl = self.slots[q]
        b = sl[self.rr[q] % len(sl)]
        self.rr[q] += 1
        k = self.cur[b]
        self._deps(q, reads, writes, extra=((k, self.val[k]),))
        ins = self.eng[q].dma_start(out=out, in_=in_, **kw)
        if self.val[k] >= self.LIMIT:
            k = self._fresh(b)
        self.val[k] += 16
        ins.then_inc(self.sem[k], 16)
        self.n_inst += 1
        self._mark((k, self.val[k]), reads, writes)

    def barrier(self):
        for e in self.eng:
            wd = self.waited[e]
            for k, v in self.val.items():
                if v > 0 and wd.get(k, 0) < v and not (k[0] == e and e == "pe"):
                    wd[k] = v
                    self.eng[e].wait_ge(self.sem[k], v)

    def finish(self):
        wd = self.waited["sp"]
        for k, v in self.val.items():
            if v > 0 and wd.get(k, 0) < v:
                wd[k] = v
                self.eng["sp"].wait_ge(self.sem[k], v)


class Tile:
    def __init__(self, t):
        self.t = t
        self.res = Res()

    def __getitem__(self, key):
        return self.t[key]


def build(nc, n_layers=DEPTH, stop_after=None, dbg=()):
    stack = contextlib.ExitStack()
    with stack:
        _build(nc, stack, n_layers, stop_after, dbg)
    return nc


def _build(nc, stack, n_layers, stop_after, dbg):
    P = Prog(nc, stack)

    def din(name, shape, dt=F32):
        return nc.dram_tensor(name, list(shape), dt, kind="ExternalInput").ap()

    def dscr(name, shape, dt=F32):
        kind = "ExternalOutput" if name in dbg else "Internal"
        return nc.dram_tensor(name, list(shape), dt, kind=kind).ap()

    sb_n = [0]

    def sb(name, shape, dt=F32, st=None):
        sb_n[0] += 1
        return Tile((st or stack).enter_context(nc.sbuf_tensor("%s_%d" % (name, sb_n[0]), list(shape), dt)))

    x_in = din("x", [L, D])
    ctx_in = din("ctx", [LC, D])
    cvec = din("cvec", [128, 16])
    ada_w = din("ada_w", [DEPTH, D, 6 * D])
    ada_b = din("ada_b", [DEPTH, 6 * D])
    norm1_g = din("norm1_g", [DEPTH, D])
    norm2_g = din("norm2_g", [DEPTH, D])
    w_in = din("w_in", [DEPTH, D, IN_W])
    w_out = din("w_out", [DEPTH, D, D])
    qk_gain = din("qk_gain", [DEPTH, 2 * HD])
    rope = din("rope", [L, 64])
    ident_d = din("ident", [128, 128])
    rwv = din("rwv", [DEPTH, 128, 43])
    rw_w2 = din("rw_w2", [DEPTH, 128, 512])
    rw_a2 = din("rw_a2", [DEPTH, 128, 512])
    rw_g2 = din("rw_g2", [DEPTH, 128, 512])
    gn_wb = din("gn_wb", [DEPTH, 2, 512])
    ffn_wg = din("ffn_wg", [2, D, DFF])
    ffn_wu = din("ffn_wu", [2, D, DFF])
    ffn_wd = din("ffn_wd", [2, DFF, D])
    moe_router = din("moe_router", [2, D, NE])
    moe_wg = din("moe_wg", [2, NE, D, DFE])
    moe_wu = din("moe_wu", [2, NE, D, DFE])
    moe_wd = din("moe_wd", [2, NE, DFE, D])
    cst128 = din("cst128", [128, 386])
    cst64 = din("cst64", [64, 3584], BF16)
    y_out = nc.dram_tensor("y", [L, D], F32, kind="ExternalOutput").ap()

    xs = dscr("xs", [T, D])
    pT = dscr("pT", [SHIFT_W, T])
    qs = dscr("qs", [T, 512], BF16)
    FM = [dscr("FM%d" % d, [NCH, 64, 2, 512], BF16) for d in range(2)]
    TM = [dscr("TM%d" % d, [NCH, 64, 1024], BF16) for d in range(2)]
    VM = dscr("VM", [NCH, 64, 512], BF16)
    MAT = [dscr("MAT%d" % d, [NCH, 64, 2560], BF16) for d in range(2)]
    Od = [dscr("Od%d" % d, [T, 512]) for d in range(2)]
    gtm = dscr("gtm", [T, 512])
    bon = dscr("bon", [T, 8])
    H2 = dscr("H2", [9, 128, 8, 512], BF16)

    ident_f = sb("ident_f", [128, 128])
    ident_b = sb("ident_b", [128, 128], BF16)
    ones_f = sb("ones_f", [128, 128])
    sc = sb("sc", [128, 16])
    mods = [[sb("mod%d_%d" % (w, j), [128, D]) for j in range(6)] for w in range(2)]
    psum = [Tile(stack.enter_context(nc.psum_tensor("ps%d" % i, [128, 512], F32))) for i in range(8)]
    ps_rr = [0]
    uid = [0]

    def next_ps():
        p = psum[ps_rr[0] % 8]
        ps_rr[0] += 1
        return p

    P.dma("sp", ident_f[:], ident_d[:, :], writes=[ident_f.res])
    P.op("dve", lambda e: e.tensor_copy(out=ident_b[:], in_=ident_f[:]), reads=[ident_f.res], writes=[ident_b.res])
    P.op("pool", lambda e: e.memset(ones_f[:], 1.0), writes=[ones_f.res])
    P.dma("sp", sc[:], cvec[:, :], writes=[sc.res])
    P.op("act", lambda e: e.activation(out=sc[:], in_=sc[:], func=AF.Silu), reads=[sc.res], writes=[sc.res])
    for i in range(8):
        P.dma("pool", xs[i * 512:(i + 1) * 512, :], x_in[i * 512:(i + 1) * 512, :], writes=[P.R("xs", 4 * i + j) for j in range(4)])
    P.dma("pool", xs[L:T, :], ctx_in[:, :], writes=[P.R("xs", 32), P.R("xs", 33)])

    for li in range(n_layers):
        last = li == DEPTH - 1
        with contextlib.ExitStack() as st:
            wst = [sb("adaw%d" % i, [128, 8, 512], st=st) for i in range(2)]
            lrep = sb("lrep", [128, 16, 128], st=st)
            P.op("dve", lambda e: e.tensor_copy(out=lrep[:], in_=sc[:].unsqueeze(2).to_broadcast([128, 16, 128])),
                 reads=[sc.res], writes=[lrep.res])
            bst = sb("adab", [1, 6 * D], st=st)
            gst = sb("g1bc", [128, D], st=st)
            g2st = sb("g2bc", [128, D], st=st)
            P.dma("sp", bst[:], ada_b[li:li + 1, :], writes=[bst.res])
            P.dma("sp", gst[:], norm1_g[li, :].partition_broadcast(128), writes=[gst.res])
            P.dma("sp", g2st[:], norm2_g[li, :].partition_broadcast(128), writes=[g2st.res])
            aw = ada_w[li].rearrange("(kc p) n -> p kc n", p=128)
            for pc in range(12):
                w = wst[pc % 2]
                P.dma("sp" if pc % 2 == 0 else "pool", w[:], aw[:, :, pc * 512:(pc + 1) * 512], writes=[w.res])
                j, half = pc // 2, pc % 2
                for which in range(2):
                    ps = next_ps()
                    for kc in range(8):
                        P.op("pe", lambda e, kc=kc, ps=ps, w=w, which=which: e.matmul(
                            ps[:], lrep[:, which * 8 + kc, :], w[:, kc, :], start=(kc == 0), stop=False),
                            reads=[lrep.res, w.res], writes=[ps.res])
                    P.op("pe", lambda e, ps=ps, pc=pc: e.matmul(
                        ps[:], ones_f[0:1, :], bst[0:1, pc * 512:(pc + 1) * 512], start=False, stop=True),
                        reads=[ones_f.res, bst.res], writes=[ps.res])
                    m = mods[which][j]
                    P.op("act", lambda e, m=m, ps=ps, half=half: e.copy(out=m[:, half * 512:(half + 1) * 512], in_=ps[:]),
                         reads=[ps.res], writes=[m.res])
            for which in range(2):
                for (j, g) in ((1, gst), (4, g2st)):
                    m = mods[which][j]
                    P.op("dve", lambda e, m=m, g=g: e.scalar_tensor_tensor(
                        out=m[:], in0=m[:], scalar=1.0, in1=g[:], op0=ALU.add, op1=ALU.mult),
                        reads=[m.res, g.res], writes=[m.res])
        P.barrier()
        if stop_after == "mods":
            dbg_t = nc.dram_tensor("dbg_mods", [12, 128, D], F32, kind="ExternalOutput").ap()
            for which in range(2):
                for j in range(6):
                    P.dma("sp", dbg_t[which * 6 + j], mods[which][j][:], reads=[mods[which][j].res])
            P.finish()
            return

        with contextlib.ExitStack() as stL:
            def make_loader(st_):
                uid[0] += 1
                stage = [sb("stage%d_%d" % (uid[0], i), [128, 1792], st=st_) for i in range(2)]
                stage_rr = [0]

                def load_cast(dst, src_fn, ncols, piece, nk=8, col0=0):
                    for c0 in range(0, ncols, piece):
                        c1 = min(ncols, c0 + piece)
                        stg = stage[stage_rr[0] % 2]
                        q = "sp" if stage_rr[0] % 2 == 0 else "act"
                        stage_rr[0] += 1
                        view = stg[:, 0:nk * (c1 - c0)].rearrange("p (k n) -> p k n", k=nk)
                        P.dma(q, view, src_fn(c0, c1), writes=[stg.res])
                        P.op("pool", lambda e, view=view, c0=c0, c1=c1: e.tensor_copy(out=dst[:, 0:nk, col0 + c0:col0 + c1], in_=view),
                             reads=[stg.res], writes=[dst.res])
                return load_cast

            eps_t = sb("eps_t", [128, 1], st=stL)
            P.op("pool", lambda e: e.memset(eps_t[:], EPS), writes=[eps_t.res])

            with contextlib.ExitStack() as stA:
                load_cast = make_loader(stA)
                KT = sb("KT", [128, T], BF16, st=stA)
                Vaug = sb("Vaug", [128, NT, 2, 65], BF16, st=stA)
                P.op("pool", lambda e: e.memset(Vaug[:, :, :, 64:65], 1.0), writes=[Vaug.res])
                with contextlib.ExitStack() as stB:
                    win_b = sb("win_b", [128, 8, IN_W], BF16, st=stB)
                    load_cast(win_b, lambda c0, c1: w_in[li].rearrange("(kc p) n -> p kc n", p=128)[:, :, c0:c1], IN_W, 224)
                    gain = sb("gain", [128, 128], st=stB)
                    P.dma("sp", gain[:], qk_gain[li, :].partition_broadcast(128), writes=[gain.res])
                    xt2 = [sb("xt%d" % i, [128, D], st=stB) for i in range(2)]
                    sq = sb("sq", [128, D], st=stB)
                    hf = sq
                    hb2 = [sb("hb%d" % i, [128, D], BF16, st=stB) for i in range(2)]
                    hTg2 = [sb("hTg%d" % i, [128, 8, 512], BF16, st=stB) for i in range(2)]
                    st10 = sb("st10", [128, 24], st=stB)
                    qkf = sb("qkf", [128, 10, 64], st=stB)
                    qkb2 = [sb("qkb%d" % i, [128, 10, 64], BF16, st=stB) for i in range(2)]
                    rt = [sb("rt%d" % i, [128, 10, 32], st=stB) for i in range(4)]
                    rp2 = [sb("rp%d" % i, [128, 64], st=stB) for i in range(2)]
                    pst3 = [sb("pst%d" % i, [128, 512], st=stB) for i in range(2)]
                    pst_rr = 0
                    for t in range(NT):
                        which = 0 if t < NLT else 1
                        M = mods[which]
                        grp, tin = t // 4, t % 4
                        xt, hb, hTg, qkb = xt2[t % 2], hb2[t % 2], hTg2[grp % 2], qkb2[t % 2]
                        if t == 0:
                            P.dma("sp", xt[:], xs[0:128, :], reads=[P.R("xs", 0)], writes=[xt.res])
                        if t + 1 < NT:
                            P.dma("sp", xt2[(t + 1) % 2][:], xs[(t + 1) * 128:(t + 2) * 128, :], reads=[P.R("xs", t + 1)],
                                  writes=[xt2[(t + 1) % 2].res])
                        P.op("act", lambda e, xt=xt: e.activation(out=sq[:], in_=xt[:], func=AF.Square),
                             reads=[xt.res], writes=[sq.res])
                        P.op("dve", lambda e: e.tensor_reduce(out=st10[:, 0:1], in_=sq[:], axis=AX.X, op=ALU.add),
                             reads=[sq.res], writes=[st10.res])
                        P.op("act", lambda e: e.activation(out=st10[:, 1:2], in_=st10[:, 0:1], func=AF.Sqrt,
                                                           bias=eps_t[:, 0:1], scale=1.0 / D),
                             reads=[st10.res, eps_t.res], writes=[st10.res])
                        P.op("dve", lambda e: e.reciprocal(out=st10[:, 1:2], in_=st10[:, 1:2]),
                             reads=[st10.res], writes=[st10.res])
                        P.op("dve", lambda e, xt=xt, M=M: e.scalar_tensor_tensor(
                            out=hf[:], in0=xt[:], scalar=st10[:, 1:2], in1=M[1][:], op0=ALU.mult, op1=ALU.mult),
                            reads=[xt.res, st10.res, M[1].res], writes=[hf.res])
                        P.op("dve", lambda e, hb=hb, M=M: e.tensor_tensor(out=hb[:], in0=hf[:], in1=M[0][:], op=ALU.add),
                             reads=[hf.res, M[0].res], writes=[hb.res])
                        psT = psum[0 + (t % 2)]
                        for kc in range(8):
                            P.op("pe", lambda e, kc=kc, psT=psT, hb=hb: e.transpose(
                                out=psT[:].bitcast(BF16)[:, kc * 128:(kc + 1) * 128], in_=hb[:, kc * 128:(kc + 1) * 128],
                                identity=ident_b[:]), reads=[hb.res, ident_b.res], writes=[psT.res])
                        P.op("act", lambda e, psT=psT, hTg=hTg, tin=tin: e.copy(
                            out=hTg[:, :, tin * 128:(tin + 1) * 128],
                            in_=psT[:].bitcast(BF16).rearrange("p (k n) -> p k n", k=8)),
                            reads=[psT.res], writes=[hTg.res])
                        psA, psB = psum[2], psum[3]
                        for kc in range(8):
                            P.op("pe", lambda e, kc=kc, hTg=hTg, tin=tin: e.matmul(
                                psA[:], hTg[:, kc, tin * 128:(tin + 1) * 128], win_b[:, kc, 0:512],
                                start=(kc == 0), stop=(kc == 7)), reads=[hTg.res, win_b.res], writes=[psA.res])
                        for kc in range(8):
                            P.op("pe", lambda e, kc=kc, hTg=hTg, tin=tin: e.matmul(
                                psB[:, 0:256], hTg[:, kc, tin * 128:(tin + 1) * 128], win_b[:, kc, 512:768],
                                start=(kc == 0), stop=(kc == 7)), reads=[hTg.res, win_b.res], writes=[psB.res])
                        P.op("act", lambda e: e.activation(out=sq[:, 0:512], in_=psA[:], func=AF.Square),
                             reads=[psA.res], writes=[sq.res])
                        P.op("act", lambda e: e.activation(out=sq[:, 512:640], in_=psB[:, 0:128], func=AF.Square),
                             reads=[psB.res], writes=[sq.res])
                        P.op("act", lambda e, t=t: e.copy(out=Vaug[:, t, :, 0:64],
                                                          in_=psB[:, 128:256].rearrange("p (g d) -> p g d", g=2)),
                             reads=[psB.res], writes=[Vaug.res])
                        P.op("dve", lambda e: e.tensor_reduce(
                            out=st10[:, 4:14], in_=sq[:, 0:640].rearrange("p (h d) -> p h d", h=10), axis=AX.X, op=ALU.add),
                            reads=[sq.res], writes=[st10.res])
                        P.op("act", lambda e: e.activation(out=st10[:, 14:24], in_=st10[:, 4:14], func=AF.Sqrt,
                                                           bias=eps_t[:, 0:1], scale=1.0 / HD),
                             reads=[st10.res, eps_t.res], writes=[st10.res])
                        P.op("dve", lambda e: e.reciprocal(out=st10[:, 14:24], in_=st10[:, 14:24]),
                             reads=[st10.res], writes=[st10.res])
                        P.op("dve", lambda e: e.tensor_tensor(
                            out=qkf[:, 0:8, :], in0=psA[:].rearrange("p (h d) -> p h d", h=8),
                            in1=st10[:, 14:22].unsqueeze(2).to_broadcast([128, 8, 64]), op=ALU.mult),
                            reads=[psA.res, st10.res], writes=[qkf.res])
                        P.op("dve", lambda e: e.tensor_tensor(
                            out=qkf[:, 8:10, :], in0=psB[:, 0:128].rearrange("p (h d) -> p h d", h=2),
                            in1=st10[:, 22:24].unsqueeze(2).to_broadcast([128, 2, 64]), op=ALU.mult),
                            reads=[psB.res, st10.res], writes=[qkf.res])
                        P.op("dve", lambda e: e.tensor_tensor(
                            out=qkf[:, 0:8, :], in0=qkf[:, 0:8, :],
                            in1=gain[:, 0:64].unsqueeze(1).to_broadcast([128, 8, 64]), op=ALU.mult),
                            reads=[qkf.res, gain.res], writes=[qkf.res])
                        P.op("dve", lambda e: e.tensor_tensor(
                            out=qkf[:, 8:10, :], in0=qkf[:, 8:10, :],
                            in1=gain[:, 64:128].unsqueeze(1).to_broadcast([128, 2, 64]), op=ALU.mult),
                            reads=[qkf.res, gain.res], writes=[qkf.res])
                        if t < NLT:
                            rp = rp2[t % 2]
                            P.dma("act", rp[:], rope[t * 128:(t + 1) * 128, :], writes=[rp.res])
                            cosb = rp[:, 0:32].unsqueeze(1).to_broadcast([128, 10, 32])
                            sinb = rp[:, 32:64].unsqueeze(1).to_broadcast([128, 10, 32])
                            x1, x2 = qkf[:, :, 0:32], qkf[:, :, 32:64]
                            P.op("dve", lambda e, cosb=cosb, x1=x1: e.tensor_tensor(out=rt[0][:], in0=x1, in1=cosb, op=ALU.mult),
                                 reads=[qkf.res, rp.res], writes=[rt[0].res])
                            P.op("dve", lambda e, sinb=sinb, x2=x2: e.tensor_tensor(out=rt[1][:], in0=x2, in1=sinb, op=ALU.mult),
                                 reads=[qkf.res, rp.res], writes=[rt[1].res])
                            P.op("dve", lambda e, qkb=qkb: e.tensor_tensor(out=qkb[:, :, 0:32], in0=rt[0][:], in1=rt[1][:], op=ALU.subtract),
                                 reads=[rt[0].res, rt[1].res], writes=[qkb.res])
                            P.op("pool", lambda e, sinb=sinb, x1=x1: e.tensor_tensor(out=rt[2][:], in0=x1, in1=sinb, op=ALU.mult),
                                 reads=[qkf.res, rp.res], writes=[rt[2].res])
                            P.op("pool", lambda e, cosb=cosb, x2=x2: e.tensor_tensor(out=rt[3][:], in0=x2, in1=cosb, op=ALU.mult),
                                 reads=[qkf.res, rp.res], writes=[rt[3].res])
                            P.op("pool", lambda e, qkb=qkb: e.tensor_tensor(out=qkb[:, :, 32:64], in0=rt[2][:], in1=rt[3][:], op=ALU.add),
                                 reads=[rt[2].res, rt[3].res], writes=[qkb.res])
                        else:
                            P.op("dve", lambda e, qkb=qkb: e.tensor_copy(out=qkb[:], in_=qkf[:]),
                                 reads=[qkf.res], writes=[qkb.res])
                        for g in range(2):
                            P.dma("act" if g == 0 else "sp",
                                  qs[t * 128:(t + 1) * 128, :].rearrange("p (j g d) -> p g j d", j=4, g=2)[:, g, :, :],
                                  qkb[:, 4 * g:4 * g + 4, :], reads=[qkb.res], writes=[P.R("qs", t)])
                        psK = psum[4 + (t % 2)]
                        P.op("pe", lambda e, psK=psK, qkb=qkb: e.transpose(
                            out=psK[:].bitcast(BF16)[:, 0:128], in_=qkb[:, 8:10, :].rearrange("p g d -> p (g d)"),
                            identity=ident_b[:]), reads=[qkb.res, ident_b.res], writes=[psK.res])
                        P.op("act", lambda e, psK=psK, t=t: e.copy(
                            out=KT[:, t * 128:(t + 1) * 128], in_=psK[:].bitcast(BF16)[:, 0:128]),
                            reads=[psK.res], writes=[KT.res])
                        if tin == 3 or t == NT - 1:
                            N = (tin + 1) * 128
                            g0 = grp * 512
                            for c in range(15):
                                ps = psum[6 + (c % 2)]
                                for kc in range(8):
                                    P.op("pe", lambda e, kc=kc, c=c, ps=ps, hTg=hTg, N=N: e.matmul(
                                        ps[:, 0:N], win_b[:, kc, ATT_IN + c * 128:ATT_IN + (c + 1) * 128], hTg[:, kc, 0:N],
                                        start=(kc == 0), stop=(kc == 7)), reads=[hTg.res, win_b.res], writes=[ps.res])
                                pst = pst3[pst_rr % 2]
                                pst_rr += 1
                                P.op("act" if c % 2 == 0 else "dve",
                                     (lambda e, ps=ps, pst=pst, N=N: e.copy(out=pst[:, 0:N], in_=ps[:, 0:N])) if c % 2 == 0 else
                                     (lambda e, ps=ps, pst=pst, N=N: e.tensor_copy(out=pst[:, 0:N], in_=ps[:, 0:N])),
                                     reads=[ps.res], writes=[pst.res])
                                P.dma("sp", pT[c * 128:(c + 1) * 128, g0:g0 + N], pst[:, 0:N],
                                      reads=[pst.res], writes=[P.R("pT", grp)])
                P.barrier()
                if stop_after == "phaseB":
                    dK = nc.dram_tensor("dbg_KT", [128, T], BF16, kind="ExternalOutput").ap()
                    dV = nc.dram_tensor("dbg_V", [128, NT, 2, 65], BF16, kind="ExternalOutput").ap()
                    P.dma("sp", dK, KT[:], reads=[KT.res])
                    P.dma("sp", dV, Vaug[:], reads=[Vaug.res])
                    P.finish()
                    return

                with contextlib.ExitStack() as stC:
                    wo_h = sb("wo_h", [64, 8, D], BF16, st=stC)
                    woh_v = w_out[li][0:512, :].rearrange("(h d) n -> d h n", d=64)
                    wstg2 = [sb("wstg2_%d" % i, [64, 8, 224], st=stC) for i in range(2)]
                    for pi, c0 in enumerate(range(0, D, 224)):
                        c1 = min(D, c0 + 224)
                        wsg = wstg2[pi % 2]
                        P.dma("sp" if pi % 2 == 0 else "act", wsg[:, :, 0:c1 - c0], woh_v[:, :, c0:c1], writes=[wsg.res])
                        P.op("pool", lambda e, wsg=wsg, c0=c0, c1=c1: e.tensor_copy(out=wo_h[:, :, c0:c1], in_=wsg[:, :, 0:c1 - c0]),
                             reads=[wsg.res], writes=[wo_h.res])
                    qt2 = [sb("qt%d" % i, [128, 512], BF16, st=stC) for i in range(2)]
                    qT2 = [sb("qT%d" % i, [128, 4, 128], BF16, st=stC) for i in range(2)]
                    Eb = [sb("Eb%d" % i, [128, 512], BF16, st=stC) for i in range(4)]
                    e_rr = 0
                    rcp = sb("rcp", [65, 512], st=stC)
                    nm2 = [sb("nm%d" % i, [64, 512], st=stC) for i in range(2)]
                    attT2 = [sb("attT%d" % i, [64, 8, 128], BF16, st=stC) for i in range(2)]
                    xa2 = [sb("xa%d" % i, [128, D], st=stC) for i in range(2)]
                    tmp = sb("tmpy", [128, D], st=stC)
                    s_rr = [0, 0]
                    for t in range(NT):
                        which = 0 if t < NLT else 1
                        M = mods[which]
                        qt, qT, attT, xa = qt2[t % 2], qT2[t % 2], attT2[t % 2], xa2[t % 2]
                        if t == 0:
                            P.dma("sp", qt[:], qs[0:128, :], reads=[P.R("qs", 0)], writes=[qt.res])
                        if t + 1 < NT:
                            P.dma("sp", qt2[(t + 1) % 2][:], qs[(t + 1) * 128:(t + 2) * 128, :], reads=[P.R("qs", t + 1)],
                                  writes=[qt2[(t + 1) % 2].res])
                        psQ = psum[7]
                        for jh in range(4):
                            P.op("pe", lambda e, jh=jh, qt=qt: e.transpose(
                                out=psQ[:].bitcast(BF16)[:, jh * 128:(jh + 1) * 128], in_=qt[:, jh * 128:(jh + 1) * 128],
                                identity=ident_b[:]), reads=[qt.res, ident_b.res], writes=[psQ.res])
                        P.op("dve", lambda e, qT=qT: e.tensor_copy(
                            out=qT[:], in_=psQ[:].bitcast(BF16)[:, 0:512].rearrange("p (j n) -> p j n", j=4)),
                            reads=[psQ.res], writes=[qT.res])
                        chunks = list(range(NT)) if t < NLT else [32, 33]

                        def score(sc_, g, qT=qT):
                            psS = psum[2 * g + s_rr[g] % 2]
                            s_rr[g] += 1
                            sl = slice(g * 64, (g + 1) * 64)
                            P.op("pe", lambda e, psS=psS, sl=sl, sc_=sc_, qT=qT: e.matmul(
                                psS[:], KT[sl, sc_ * 128:(sc_ + 1) * 128],
                                qT[sl, :, :].rearrange("p h n -> p (h n)"), start=True, stop=True),
                                reads=[KT.res, qT.res], writes=[psS.res])
                            return psS
                        pend = [[score(chunks[0], g)] for g in range(2)]
                        for ci, sc_ in enumerate(chunks):
                            cur = [pend[g].pop(0) for g in range(2)]
                            if ci + 1 < len(chunks):
                                for g in range(2):
                                    pend[g].append(score(chunks[ci + 1], g))
                            for g in range(2):
                                E = Eb[e_rr % 4]
                                e_rr += 1
                                psS, psO = cur[g], psum[4 + g]
                                P.op("act", lambda e, E=E, psS=psS: e.activation(out=E[:], in_=psS[:], func=AF.Exp, scale=0.125),
                                     reads=[psS.res], writes=[E.res])
                                P.op("pe", lambda e, E=E, psO=psO, sc_=sc_, g=g, ci=ci, n=len(chunks): e.matmul(
                                    psO[0:65, :], Vaug[:, sc_, g, :], E[:], start=(ci == 0), stop=(ci == n - 1)),
                                    reads=[E.res, Vaug.res], writes=[psO.res])
                        if last and t >= NLT:
                            continue
                        for g in range(2):
                            psO, nm = psum[4 + g], nm2[g]
                            P.op("dve", lambda e, psO=psO: e.reciprocal(out=rcp[64:65, :], in_=psO[64:65, :]),
                                 reads=[psO.res], writes=[rcp.res])
                            P.op("dve", lambda e, psO=psO, nm=nm: e.tensor_copy(out=nm[:], in_=psO[0:64, :]), reads=[psO.res], writes=[nm.res])
                            psBc = psum[6]
                            P.op("pe", lambda e, psBc=psBc: e.matmul(psBc[0:64, :], ones_f[64:65, 0:64], rcp[64:65, :], start=True, stop=True),
                                 reads=[ones_f.res, rcp.res], writes=[psBc.res])
                            P.op("dve", lambda e, psBc=psBc, nm=nm, g=g, attT=attT: e.tensor_tensor(
                                out=attT[:, 4 * g:4 * g + 4, :].rearrange("p h n -> p (h n)"), in0=nm[:], in1=psBc[0:64, :], op=ALU.mult),
                                reads=[nm.res, psBc.res], writes=[attT.res])
                        if last and t >= NLT:
                            continue
                        P.dma("act", xa[:], xs[t * 128:(t + 1) * 128, :], reads=[P.R("xs", t)], writes=[xa.res])
                        for half in range(2):
                            psY = psum[6 + half]
                            for h in range(8):
                                P.op("pe", lambda e, h=h, half=half, psY=psY, attT=attT: e.matmul(
                                    psY[:], attT[:, h, :], wo_h[:, h, half * 512:(half + 1) * 512],
                                    start=(h == 0), stop=(h == 7)), reads=[attT.res, wo_h.res], writes=[psY.res])
                            P.op("dve", lambda e, half=half, psY=psY, M=M: e.tensor_tensor(
                                out=tmp[:, half * 512:(half + 1) * 512], in0=psY[:], in1=M[2][:, half * 512:(half + 1) * 512],
                                op=ALU.mult), reads=[psY.res, M[2].res], writes=[tmp.res])
                        P.op("dve", lambda e, xa=xa: e.tensor_tensor(out=xa[:], in0=xa[:], in1=tmp[:], op=ALU.add),
                             reads=[xa.res, tmp.res], writes=[xa.res])
                        P.dma("sp", xs[t * 128:(t + 1) * 128, :], xa[:], reads=[xa.res], writes=[P.R("xs", t)])
            P.barrier()
            if stop_after == "attn":
                P.finish()
                return

            NB = 256
            blocks = [(i * NB, i == 0, i == L // NB - 1) for i in range(L // NB)] + [(L, True, True)]
            GC = sb("GC", [128, 2, 4, NCH], st=stL)
            with contextlib.ExitStack() as stR:
                rwv_t = sb("rwv_t", [128, 43], st=stR)
                P.dma("sp", rwv_t[:], rwv[li], writes=[rwv_t.res])
                omka = sb("omka", [128, 8], st=stR)
                P.op("dve", lambda e: e.tensor_scalar(out=omka[:, 0:4], in0=rwv_t[:, 4:8], scalar1=-1.0, scalar2=1.0,
                                                      op0=ALU.mult, op1=ALU.add), reads=[rwv_t.res], writes=[omka.res])
                P.op("dve", lambda e: e.tensor_scalar(out=omka[:, 4:8], in0=rwv_t[:, 4:8], scalar1=-2.0, scalar2=2.0,
                                                      op0=ALU.mult, op1=ALU.add), reads=[rwv_t.res], writes=[omka.res])
                c12 = sb("c12", [128, 1], st=stR)
                P.op("pool", lambda e: e.memset(c12[:], 1e-12), writes=[c12.res])
                c128 = sb("c128", [128, 386], st=stR)
                P.dma("sp", c128[:], cst128[:, :], writes=[c128.res])
                c64 = sb("c64", [64, 3584], BF16, st=stR)
                P.dma("act", c64[:], cst64[:, :], writes=[c64.res])
                BDo, SEL, RST = c128[:, 0:128], c128[:, 128:130], c128[:, 130:386]
                selrk = sb("selrk", [128, 4, 2], st=stR)
                P.op("dve", lambda e: e.tensor_tensor(
                    out=selrk[:], in0=SEL.unsqueeze(1).to_broadcast([128, 4, 2]),
                    in1=rwv_t[:, 8:12].unsqueeze(2).to_broadcast([128, 4, 2]), op=ALU.mult),
                    reads=[c128.res, rwv_t.res], writes=[selrk.res])
                lw3 = []
                wstg = sb("wstg", [128, 512], st=stR)
                for nm, src in (("w2b", rw_w2), ("a2b", rw_a2), ("g2b", rw_g2)):
                    wt = sb(nm, [128, 512], BF16, st=stR)
                    P.dma("sp", wstg[:], src[li], writes=[wstg.res])
                    P.op("pool", lambda e, wt=wt: e.tensor_copy(out=wt[:], in_=wstg[:]), reads=[wstg.res], writes=[wt.res])
                    lw3.append(wt)
                w2b, a2b, g2b = lw3
                PH = sb("PH", [128, 15, NB + 2], st=stR)
                S = sb("S", [128, 15, NB], st=stR)
                KKN = sb("KKN", [128, 4, NB], st=stR)
                LW = sb("LW", [128, 4, NB], st=stR)
                A2 = [sb("A%d" % d, [128, 4, NB], st=stR) for d in range(2)]
                KD = sb("KD", [128, 4, NB], st=stR)
                BDt = sb("BDt", [128, 4, NB], st=stR)
                CU = sb("CU", [128, 4, NB], st=stR)
                EXC = sb("EXC", [128, 4, NB], st=stR)
                EP, EN, EX = (sb(n, [128, 4, NB], st=stR) for n in ("EP", "EN", "EX"))
                TW = sb("TW", [128, NB], BF16, st=stR)
                ADb = sb("ADb", [128, NB], BF16, st=stR)
                SG = sb("SG", [128, NB], BF16, st=stR)
                vb = sb("vb", [128, 4, NB], BF16, st=stR)
                gst = [sb("gst%d" % i, [128, 512], st=stR) for i in range(1)]
                FMs = sb("FMs", [128, 4, 4, 4, 64], BF16, st=stR)
                TMs = sb("TMs", [64, 4, 2, 512], BF16, st=stR)
                Vs = sb("Vs", [64, 4, 512], BF16, st=stR)
                BQm = [sb("BQm%d" % n, [64, 8, 2, 64], BF16, st=stR) for n in range(4)]
                KQm = [sb("KQm%d" % n, [64, 8, 2, 64], BF16, st=stR) for n in range(4)]
                Pm = [[sb("Pm%d_%d" % (n, i), [64, 8, 64], BF16, st=stR) for i in range(2)] for n in range(4)]
                PTm = [[sb("PTm%d_%d" % (n, i), [64, 8, 64], BF16, st=stR) for i in range(2)] for n in range(4)]
                WTm = [[sb("WTm%d_%d" % (n, i), [64, 8, 64], BF16, st=stR) for i in range(2)] for n in range(4)]
                bst_ = sb("bst_", [128, 16], st=stR)
                ev = [0]

                def evac(fn_act, fn_dve, reads, writes):
                    if ev[0] % 2 == 0:
                        P.op("act", fn_act, reads=reads, writes=writes)
                    else:
                        P.op("dve", fn_dve, reads=reads, writes=writes)
                    ev[0] += 1

                mskB = [c64[:, d * 512:(d + 1) * 512] for d in range(2)]
                mskK = [c64[:, 1024 + d * 512:1024 + (d + 1) * 512] for d in range(2)]
                mskN = [c64[:, 2048 + d * 512:2048 + (d + 1) * 512] for d in range(2)]
                id8 = c64[:, 3072:3584]
                pTv = pT.rearrange("(c p) t -> p c t", p=128)
                import os as _os
                CUT = int(_os.environ.get("R1A_CUT", "0"))
                cut_hit = [False]

                def cut(k):
                    if CUT == k:
                        cut_hit[0] = True
                    return cut_hit[0]
                for bi, (g0, s0, s1) in enumerate(blocks):
                    if cut_hit[0]:
                        break
                    ch0 = g0 // CH
                    grp_lo, grp_hi = max(g0 - 1, 0) // 512, min(g0 + NB, T - 1) // 512
                    rd = [P.R("pT", g) for g in range(grp_lo, grp_hi + 1)]
                    lo = g0 - (0 if s0 else 1)
                    hi = g0 + NB + (0 if s1 else 1)
                    off = 1 if s0 else 0
                    if s0:
                        P.op("pool", lambda e: e.memset(PH[:, :, 0:1], 0.0), writes=[PH.res])
                    if s1:
                        P.op("pool", lambda e: e.memset(PH[:, :, NB + 1:NB + 2], 0.0), writes=[PH.res])
                    for c3 in range(5):
                        P.dma("sp" if c3 % 2 == 0 else "act", PH[:, 3 * c3:3 * c3 + 3, off:off + hi - lo], pTv[:, 3 * c3:3 * c3 + 3, lo:hi],
                              reads=rd, writes=[PH.res])
                    pc_ = PH[:, :, 1:NB + 1]
                    P.op("pool", lambda e: e.tensor_tensor(out=S[:], in0=PH[:, :, 0:NB], in1=PH[:, :, 2:NB + 2], op=ALU.add),
                         reads=[PH.res], writes=[S.res])
                    P.op("dve", lambda e, pc_=pc_: e.scalar_tensor_tensor(out=S[:], in0=S[:], scalar=0.5, in1=pc_,
                                                                         op0=ALU.mult, op1=ALU.subtract),
                         reads=[S.res, PH.res], writes=[S.res])
                    P.op("pool", lambda e: e.tensor_tensor(out=S[:], in0=S[:],
                                                           in1=rwv_t[:, 28:43].unsqueeze(2).to_broadcast([128, 15, NB]), op=ALU.mult),
                         reads=[S.res, rwv_t.res], writes=[S.res])
                    P.op("dve", lambda e, pc_=pc_: e.tensor_tensor(out=S[:], in0=S[:], in1=pc_, op=ALU.add),
                         reads=[S.res, PH.res], writes=[S.res])
                    r_, k_, v_ = S[:, 0:4, :], S[:, 4:8, :], S[:, 8:12, :]
                    if cut(1):
                        break
                    for c in range(4):
                        P.op("dve", lambda e, c=c: e.tensor_scalar(out=KKN[:, c, :], in0=S[:, 4 + c, :], scalar1=rwv_t[:, c:c + 1],
                                                                   scalar2=None, op0=ALU.mult),
                             reads=[S.res, rwv_t.res], writes=[KKN.res])
                    P.op("act", lambda e: e.activation(out=EX[:], in_=KKN[:], func=AF.Square), reads=[KKN.res], writes=[EX.res])
                    for hf_ in range(2):
                        ps = next_ps()
                        for cc in range(2):
                            c = 2 * hf_ + cc
                            P.op("pe", lambda e, ps=ps, c=c, cc=cc: e.matmul(ps[:, cc * NB:(cc + 1) * NB], BDo, EX[:, c, :],
                                                                            start=True, stop=True),
                                 reads=[c128.res, EX.res], writes=[ps.res])
                        P.op("act", lambda e, ps=ps, hf_=hf_: e.activation(
                            out=EP[:, 2 * hf_:2 * hf_ + 2, :], in_=ps[:].rearrange("p (c n) -> p c n", c=2), func=AF.Sqrt,
                            bias=c12[:, 0:1], scale=1.0), reads=[ps.res, c12.res], writes=[EP.res])
                    P.op("dve", lambda e: e.reciprocal(out=EP[:], in_=EP[:]), reads=[EP.res], writes=[EP.res])
                    P.op("dve", lambda e: e.tensor_tensor(out=KKN[:], in0=KKN[:], in1=EP[:], op=ALU.mult),
                         reads=[KKN.res, EP.res], writes=[KKN.res])
                    if cut(2):
                        break
                    P.op("act", lambda e: e.activation(out=TW[:], in_=S[:, 12, :], func=AF.Tanh), reads=[S.res], writes=[TW.res])
                    P.op("act", lambda e: e.copy(out=ADb[:], in_=S[:, 13, :]), reads=[S.res], writes=[ADb.res])
                    P.op("act", lambda e: e.activation(out=SG[:], in_=S[:, 14, :], func=AF.Sigmoid), reads=[S.res], writes=[SG.res])
                    P.op("act", lambda e, v_=v_: e.copy(out=vb[:], in_=v_), reads=[S.res], writes=[vb.res])
                    for tt in range(NB // 128):
                        ps = next_ps()
                        P.op("pe", lambda e, ps=ps, tt=tt: e.matmul(ps[:], SG[:, tt * 128:(tt + 1) * 128], g2b[:], start=True, stop=True),
                             reads=[SG.res, g2b.res], writes=[ps.res])
                        gs = gst[0]
                        evac(lambda e, ps=ps, gs=gs: e.copy(out=gs[:], in_=ps[:]),
                             lambda e, ps=ps, gs=gs: e.tensor_copy(out=gs[:], in_=ps[:]), [ps.res], [gs.res])
                        P.dma("pool", gtm[g0 + tt * 128:g0 + (tt + 1) * 128, :], gs[:], reads=[gs.res],
                              writes=[P.R("gtm", (g0 + tt * 128) // 128)])
                    if cut(3):
                        break
                    for d in range(2):
                        A = A2[d]
                        for c in range(4):
                            ps = next_ps()
                            P.op("pe", lambda e, ps=ps, c=c, d=d: e.matmul(
                                ps[:, 0:NB], w2b[d * 64:(d + 1) * 64, c * 128:(c + 1) * 128], TW[d * 64:(d + 1) * 64, :],
                                start=True, stop=True), reads=[w2b.res, TW.res], writes=[ps.res])
                            P.op("pe", lambda e, ps=ps, c=c, d=d: e.matmul(
                                ps[:, NB:2 * NB], a2b[d * 64:(d + 1) * 64, c * 128:(c + 1) * 128], ADb[d * 64:(d + 1) * 64, :],
                                start=True, stop=True), reads=[a2b.res, ADb.res], writes=[ps.res])
                            P.op("act", lambda e, ps=ps, c=c, d=d: e.activation(
                                out=LW[:, c, :], in_=ps[:, 0:NB], func=AF.Sigmoid, bias=rwv_t[:, 12 + 4 * d + c:13 + 4 * d + c], scale=1.0),
                                reads=[ps.res, rwv_t.res], writes=[LW.res])
                            P.op("act", lambda e, ps=ps, c=c, d=d, A=A: e.activation(
                                out=A[:, c, :], in_=ps[:, NB:2 * NB], func=AF.Sigmoid, bias=rwv_t[:, 20 + 4 * d + c:21 + 4 * d + c], scale=1.0),
                                reads=[ps.res, rwv_t.res], writes=[A.res])
                            P.op("dve", lambda e, c=c, A=A: e.tensor_scalar(
                                out=KD[:, c, :], in0=A[:, c, :], scalar1=rwv_t[:, 4 + c:5 + c], scalar2=omka[:, c:c + 1],
                                op0=ALU.mult, op1=ALU.add), reads=[A.res, rwv_t.res, omka.res], writes=[KD.res])
                            P.op("dve", lambda e, c=c: e.tensor_tensor_scan(
                                out=CU[:, c, :], data0=RST, data1=LW[:, c, :], initial=0.0, op0=ALU.mult, op1=ALU.add),
                                reads=[c128.res, LW.res], writes=[CU.res])
                        P.op("pool", lambda e, k_=k_: e.tensor_tensor(out=KD[:], in0=KD[:], in1=k_, op=ALU.mult),
                             reads=[KD.res, S.res], writes=[KD.res])
                        P.op("pool", lambda e, A=A: e.tensor_tensor(out=BDt[:], in0=KKN[:], in1=A[:], op=ALU.mult),
                             reads=[KKN.res, A.res], writes=[BDt.res])
                        cu4 = CU[:].rearrange("p c (n t) -> p c n t", t=CH)
                        if cut(4):
                            break
                        if d == 0:
                            P.op("pool", lambda e: e.tensor_tensor(out=EXC[:], in0=CU[:], in1=LW[:], op=ALU.subtract),
                                 reads=[CU.res, LW.res], writes=[EXC.res])
                            INC = CU
                        else:
                            P.op("pool", lambda e, cu4=cu4: e.tensor_tensor(
                                out=EXC[:].rearrange("p c (n t) -> p c n t", t=CH),
                                in0=cu4[:, :, :, CH - 1:CH].to_broadcast([128, 4, NB // CH, CH]), in1=cu4, op=ALU.subtract),
                                reads=[CU.res], writes=[EXC.res])
                            INC = None
                        P.op("act", lambda e, cu4=cu4, d=d, ch0=ch0: e.activation(
                            out=GC[:, d, :, ch0:ch0 + NB // CH], in_=cu4[:, :, :, CH - 1], func=AF.Exp, scale=-DECAY_C),
                            reads=[CU.res], writes=[GC.res])
                        if INC is None:
                            P.op("pool", lambda e: e.tensor_tensor(out=CU[:], in0=EXC[:], in1=LW[:], op=ALU.add),
                                 reads=[EXC.res, LW.res], writes=[CU.res])
                            INC = CU
                        P.op("act", lambda e, INC=INC: e.activation(out=EP[:], in_=INC[:], func=AF.Exp, scale=-DECAY_C),
                             reads=[INC.res], writes=[EP.res])
                        P.op("act", lambda e, INC=INC: e.activation(out=EN[:], in_=INC[:], func=AF.Exp, scale=DECAY_C),
                             reads=[INC.res], writes=[EN.res])
                        P.op("act", lambda e: e.activation(out=EX[:], in_=EXC[:], func=AF.Exp, scale=-DECAY_C),
                             reads=[EXC.res], writes=[EX.res])

                        def fm_out(arr):
                            return FMs[:, :, arr, :, :].rearrange("p n c t -> p c n t")

                        def v4(tl):
                            return tl[:].rearrange("p c (n t) -> p c n t", t=CH)
                        P.op("pool", lambda e: e.tensor_tensor(out=fm_out(0), in0=v4(KKN), in1=v4(EX), op=ALU.mult),
                             reads=[KKN.res, EX.res], writes=[FMs.res])
                        P.op("dve", lambda e, r_=r_: e.tensor_tensor(out=fm_out(1), in0=r_.rearrange("p c (n t) -> p c n t", t=CH),
                                                                     in1=v4(EP), op=ALU.mult),
                             reads=[S.res, EP.res], writes=[FMs.res])
                        P.op("pool", lambda e: e.tensor_tensor(out=fm_out(2), in0=v4(KD), in1=v4(EN), op=ALU.mult),
                             reads=[KD.res, EN.res], writes=[FMs.res])
                        P.op("dve", lambda e: e.tensor_tensor(out=fm_out(3), in0=v4(BDt), in1=v4(EN), op=ALU.mult),
                             reads=[BDt.res, EN.res], writes=[FMs.res])
                        for hh in range(2):
                            P.dma("sp" if hh == 0 else "act", FM[d][ch0:ch0 + 4][:, :, hh, :].rearrange("n k f -> k n f"),
                                  FMs[hh * 64:(hh + 1) * 64, :, 0:2, :, :].rearrange("p n a c t -> p n (a c t)"),
                                  reads=[FMs.res], writes=[P.R("FM", d, bi)])
                        if cut(5):
                            break
                        for n in range(4):
                            for a in range(2):
                                ps = next_ps()
                                for c in range(4):
                                    P.op("pe", lambda e, ps=ps, n=n, a=a, c=c: e.transpose(
                                        out=ps[:].bitcast(BF16)[0:64, c * 128:(c + 1) * 128], in_=FMs[:, n, 2 + a, c, :],
                                        identity=ident_b[:]), reads=[FMs.res, ident_b.res], writes=[ps.res])
                                sgn = 1.0 if a == 0 else -1.0
                                evac(lambda e, ps=ps, n=n, a=a, sgn=sgn: e.mul(out=TMs[:, n, a, :], in_=ps[:].bitcast(BF16)[0:64, 0:512], mul=sgn),
                                     lambda e, ps=ps, n=n, a=a, sgn=sgn: e.tensor_scalar(out=TMs[:, n, a, :], in0=ps[:].bitcast(BF16)[0:64, 0:512],
                                                                                           scalar1=sgn, scalar2=None, op0=ALU.mult),
                                     [ps.res], [TMs.res])
                            if d == 0:
                                ps = next_ps()
                                for c in range(4):
                                    P.op("pe", lambda e, ps=ps, n=n, c=c: e.transpose(
                                        out=ps[:].bitcast(BF16)[0:64, c * 128:(c + 1) * 128], in_=vb[:, c, n * CH:(n + 1) * CH],
                                        identity=ident_b[:]), reads=[vb.res, ident_b.res], writes=[ps.res])
                                evac(lambda e, ps=ps, n=n: e.copy(out=Vs[:, n, :], in_=ps[:].bitcast(BF16)[0:64, 0:512]),
                                     lambda e, ps=ps, n=n: e.tensor_copy(out=Vs[:, n, :], in_=ps[:].bitcast(BF16)[0:64, 0:512]),
                                     [ps.res], [Vs.res])
                        if _os.environ.get("NO_TMDMA") is None:
                            P.dma("act", TM[d][ch0:ch0 + 4].rearrange("n p f -> p n f"), TMs[:].rearrange("p n a f -> p n (a f)"),
                                  reads=[TMs.res], writes=[P.R("TM", d, bi)])
                            if d == 0:
                                P.dma("sp", VM[ch0:ch0 + 4].rearrange("n p f -> p n f"), Vs[:], reads=[Vs.res], writes=[P.R("VM", bi)])
                        if cut(6):
                            break
                        for n in range(4):
                            pbs = [(next_ps(), next_ps(), next_ps()) for hh in range(2)]
                            for c in range(4):
                                for hh in range(2):
                                    sl = slice(hh * 64, (hh + 1) * 64)
                                    psb, psk, psn = pbs[hh]
                                    P.op("pe", lambda e, psb=psb, n=n, c=c, sl=sl: e.matmul(
                                        psb[0:64, c * 128:(c + 1) * 128], FMs[sl, n, 3, c, :], FMs[sl, n, 0:2, c, :],
                                        start=True, stop=True), reads=[FMs.res], writes=[psb.res])
                                for hh in range(2):
                                    sl = slice(hh * 64, (hh + 1) * 64)
                                    psb, psk, psn = pbs[hh]
                                    P.op("pe", lambda e, psk=psk, n=n, c=c, sl=sl: e.matmul(
                                        psk[0:64, c * 128:(c + 1) * 128], FMs[sl, n, 2, c, :], FMs[sl, n, 0:2, c, :],
                                        start=True, stop=True), reads=[FMs.res], writes=[psk.res])
                                for hh in range(2):
                                    sl = slice(hh * 64, (hh + 1) * 64)
                                    psb, psk, psn = pbs[hh]
                                    P.op("pe", lambda e, psn=psn, n=n, c=c, sl=sl: e.matmul(
                                        psn[0:64, c * 64:(c + 1) * 64], FMs[sl, n, 0, c, :], FMs[sl, n, 3, c, :],
                                        start=True, stop=True), reads=[FMs.res], writes=[psn.res])
                            for hh in range(2):
                                psb, psk, psn = pbs[hh]
                                bq = BQm[n][:].rearrange("p (c hh) a t -> p c hh a t", hh=2)[:, :, hh, :, :]
                                kq = KQm[n][:].rearrange("p (c hh) a t -> p c hh a t", hh=2)[:, :, hh, :, :]
                                pm = Pm[n][0][:].rearrange("p (c hh) t -> p c hh t", hh=2)[:, :, hh, :]
                                P.op("dve", lambda e, psb=psb, bq=bq, d=d: e.tensor_tensor(
                                    out=bq, in0=psb[0:64, :].rearrange("p (c a t) -> p c a t", c=4, a=2),
                                    in1=mskB[d].rearrange("p (c a t) -> p c a t", c=4, a=2), op=ALU.mult),
                                    reads=[psb.res, c64.res], writes=[BQm[n].res])
                                P.op("dve", lambda e, psk=psk, kq=kq, d=d: e.tensor_tensor(
                                    out=kq, in0=psk[0:64, :].rearrange("p (c a t) -> p c a t", c=4, a=2),
                                    in1=mskK[d].rearrange("p (c a t) -> p c a t", c=4, a=2), op=ALU.mult),
                                    reads=[psk.res, c64.res], writes=[KQm[n].res])
                                P.op("dve", lambda e, psn=psn, pm=pm, d=d: e.tensor_tensor(
                                    out=pm, in0=psn[0:64, 0:256].rearrange("p (c t) -> p c t", c=4),
                                    in1=mskN[d][:, 0:256].rearrange("p (c t) -> p c t", c=4), op=ALU.mult),
                                    reads=[psn.res, c64.res], writes=[Pm[n][0].res])
                            P.op("dve", lambda e, n=n: e.tensor_tensor(
                                out=WTm[n][0][:], in0=id8.rearrange("p (h t) -> p h t", h=8), in1=BQm[n][:, :, 0, :], op=ALU.subtract),
                                reads=[c64.res, BQm[n].res], writes=[WTm[n][0].res])
                        if cut(7):
                            break
                        for j in range(1, 6):
                            cur, prv = j % 2, (j - 1) % 2
                            for n in range(4):
                                Pp = Pm[n][prv]
                                if j == 1:
                                    def PTp(h, n=n):
                                        return BQm[n][:, h, 0, :]
                                    ptres = BQm[n].res
                                else:
                                    def PTp(h, n=n, prv=prv):
                                        return PTm[n][prv][:, h, :]
                                    ptres = PTm[n][prv].res
                                ps = next_ps()
                                for h in range(8):
                                    P.op("pe", lambda e, ps=ps, h=h, Pp=Pp, PTp=PTp: e.matmul(
                                        ps[0:64, h * 64:(h + 1) * 64], PTp(h), Pp[:, h, :], start=True, stop=True),
                                        reads=[Pp.res, ptres], writes=[ps.res])
                                Pn = Pm[n][cur]
                                evac(lambda e, ps=ps, Pn=Pn: e.copy(out=Pn[:].rearrange("p h t -> p (h t)"), in_=ps[0:64, :]),
                                     lambda e, ps=ps, Pn=Pn: e.tensor_copy(out=Pn[:].rearrange("p h t -> p (h t)"), in_=ps[0:64, :]),
                                     [ps.res], [Pn.res])
                                if j < 5:
                                    ps2 = next_ps()
                                    for h in range(8):
                                        P.op("pe", lambda e, ps2=ps2, h=h, Pp=Pp, PTp=PTp: e.matmul(
                                            ps2[0:64, h * 64:(h + 1) * 64], Pp[:, h, :], PTp(h), start=True, stop=True),
                                            reads=[Pp.res, ptres], writes=[ps2.res])
                                    PTn = PTm[n][cur]
                                    evac(lambda e, ps2=ps2, PTn=PTn: e.copy(out=PTn[:].rearrange("p h t -> p (h t)"), in_=ps2[0:64, :]),
                                         lambda e, ps2=ps2, PTn=PTn: e.tensor_copy(out=PTn[:].rearrange("p h t -> p (h t)"), in_=ps2[0:64, :]),
                                         [ps2.res], [PTn.res])
                            for n in range(4):
                                Pn, Wp, Wn = Pm[n][cur], WTm[n][prv], WTm[n][cur]
                                ps = next_ps()
                                for h in range(8):
                                    P.op("pe", lambda e, ps=ps, h=h, Pn=Pn, Wp=Wp: e.matmul(
                                        ps[0:64, h * 64:(h + 1) * 64], Pn[:, h, :], Wp[:, h, :], start=True, stop=True),
                                        reads=[Pn.res, Wp.res], writes=[ps.res])
                                P.op("dve", lambda e, ps=ps, Wp=Wp, Wn=Wn: e.tensor_tensor(
                                    out=Wn[:].rearrange("p h t -> p (h t)"), in0=ps[0:64, :], in1=Wp[:].rearrange("p h t -> p (h t)"),
                                    op=ALU.add), reads=[ps.res, Wp.res], writes=[Wn.res])
                        for n in range(4):
                            Wf = WTm[n][5 % 2]
                            res_ = P.R("MAT", d, ch0 + n)
                            P.dma("sp", MAT[d][ch0 + n][:, 0:512], Wf[:].rearrange("p h t -> p (h t)"), reads=[Wf.res], writes=[res_])
                            P.dma("act", MAT[d][ch0 + n][:, 512:1536], KQm[n][:].rearrange("p h a t -> p (h a t)"),
                                  reads=[KQm[n].res], writes=[res_])
                            P.dma("sp", MAT[d][ch0 + n][:, 1536:2560], BQm[n][:].rearrange("p h a t -> p (h a t)"),
                                  reads=[BQm[n].res], writes=[res_])
                        if cut(8):
                            break
                    if cut_hit[0]:
                        break
                    P.op("pool", lambda e: e.tensor_tensor(out=EXC[:], in0=A2[0][:], in1=A2[1][:], op=ALU.add),
                         reads=[A2[0].res, A2[1].res], writes=[EXC.res])
                    for c in range(4):
                        P.op("dve", lambda e, c=c: e.tensor_scalar(
                            out=EXC[:, c, :], in0=EXC[:, c, :], scalar1=rwv_t[:, 4 + c:5 + c], scalar2=omka[:, 4 + c:5 + c],
                            op0=ALU.mult, op1=ALU.add), reads=[EXC.res, rwv_t.res, omka.res], writes=[EXC.res])
                    P.op("pool", lambda e, k_=k_: e.tensor_tensor(out=EXC[:], in0=EXC[:], in1=k_, op=ALU.mult),
                         reads=[EXC.res, S.res], writes=[EXC.res])
                    P.op("dve", lambda e, r_=r_: e.tensor_tensor(out=EXC[:], in0=EXC[:], in1=r_, op=ALU.mult),
                         reads=[EXC.res, S.res], writes=[EXC.res])
                    ps = next_ps()
                    for tt in range(NB // 128):
                        for c in range(4):
                            P.op("pe", lambda e, ps=ps, tt=tt, c=c: e.matmul(
                                ps[:, tt * 8 + 2 * c:tt * 8 + 2 * c + 2], EXC[:, c, tt * 128:(tt + 1) * 128], selrk[:, c, :],
                                start=True, stop=True), reads=[EXC.res, selrk.res], writes=[ps.res])
                    P.op("dve", lambda e, ps=ps: e.tensor_copy(out=bst_[:], in_=ps[:, 0:16]), reads=[ps.res], writes=[bst_.res])
                    for tt in range(NB // 128):
                        P.dma("sp", bon[g0 + tt * 128:g0 + (tt + 1) * 128, :], bst_[:, tt * 8:(tt + 1) * 8], reads=[bst_.res],
                              writes=[P.R("bon", (g0 + tt * 128) // 128)])
                    if cut(9):
                        break
            P.barrier()
            if stop_after == "r1a":
                dG = nc.dram_tensor("dbg_GC", [128, 2, 4, NCH], F32, kind="ExternalOutput").ap()
                P.dma("sp", dG, GC[:], reads=[GC.res])
                P.finish()
                return

            GCd = dscr("GCd%d" % li, [128, 2, 4, NCH])
            P.dma("sp", GCd, GC[:], reads=[GC.res], writes=[P.R("GCd")])
            with contextlib.ExitStack() as stS:
                GC2 = sb("GC2", [64, 2, 4, 2, NCH], st=stS)
                for hh in range(2):
                    P.dma("sp" if hh == 0 else "act", GC2[:, :, :, hh, :], GCd[hh * 64:(hh + 1) * 64], reads=[P.R("GCd")], writes=[GC2.res])
                Zf = [sb("Zf%d" % d, [64, 8, 64], st=stS) for d in range(2)]
                Zb = [sb("Zb%d" % d, [64, 8, 64], BF16, st=stS) for d in range(2)]
                ztmp = sb("ztmp", [64, 8, 64], st=stS)
                for d in range(2):
                    P.op("pool", lambda e, d=d: e.memset(Zf[d][:], 0.0), writes=[Zf[d].res])
                    P.op("pool", lambda e, d=d: e.memset(Zb[d][:], 0.0), writes=[Zb[d].res])
                FMc = [[sb("FMc%d_%d" % (d, i), [64, 2, 512], BF16, st=stS) for i in range(2)] for d in range(2)]
                TMc = [[sb("TMc%d_%d" % (d, i), [64, 1024], BF16, st=stS) for i in range(2)] for d in range(2)]
                Vc = [[sb("Vc%d_%d" % (d, i), [64, 512], BF16, st=stS) for i in range(2)] for d in range(2)]
                MATc = [[sb("MATc%d_%d" % (d, i), [64, 2560], BF16, st=stS) for i in range(2)] for d in range(2)]
                Xs = [sb("Xs%d" % d, [64, 512], BF16, st=stS) for d in range(2)]
                Us = [sb("Us%d" % d, [64, 512], BF16, st=stS) for d in range(2)]
                Os = [[sb("Os%d_%d" % (d, i), [64, 512], st=stS) for i in range(2)] for d in range(2)]
                order = [list(range(64, 68)) + list(range(0, 64)), list(range(67, 63, -1)) + list(range(63, -1, -1))]
                def scan_load(step_, d_):
                    ch_ = order[d_][step_]
                    i_ = step_ % 2
                    b_ = ch_ // 4
                    P.dma("sp", FMc[d_][i_][:], FM[d_][ch_], reads=[P.R("FM", d_, b_)], writes=[FMc[d_][i_].res])
                    P.dma("act", TMc[d_][i_][:], TM[d_][ch_], reads=[P.R("TM", d_, b_)], writes=[TMc[d_][i_].res])
                    P.dma("act", Vc[d_][i_][:], VM[ch_], reads=[P.R("VM", b_)], writes=[Vc[d_][i_].res])
                    P.dma("sp", MATc[d_][i_][:], MAT[d_][ch_], reads=[P.R("MAT", d_, ch_)], writes=[MATc[d_][i_].res])
                scan_load(0, 0)
                scan_load(0, 1)
                for step in range(NCH):
                    for d in range(2):
                        ch = order[d][step]
                        i2 = step % 2
                        bi = ch // 4
                        fm, tm, vc, mat, osb = FMc[d][i2], TMc[d][i2], Vc[d][i2], MATc[d][i2], Os[d][i2]
                        if step + 1 < NCH:
                            scan_load(step + 1, d)
                        psX, psU, psO, psZ = psum[4 * d], psum[4 * d + 1], psum[4 * d + 2], psum[4 * d + 3]
                        zb, zf, xs_, us_ = Zb[d], Zf[d], Xs[d], Us[d]
                        for h in range(8):
                            c, hh = h // 2, h % 2
                            P.op("pe", lambda e, h=h, c=c, hh=hh, fm=fm, zb=zb, psX=psX: e.matmul(
                                psX[0:64, h * 64:(h + 1) * 64], fm[:, hh, c * 64:(c + 1) * 64], zb[:, h, :],
                                start=(h == 0), stop=False), reads=[fm.res, zb.res], writes=[psX.res])
                            P.op("pe", lambda e, h=h, mat=mat, vc=vc, psX=psX: e.matmul(
                                psX[0:64, h * 64:(h + 1) * 64], mat[:, 512 + h * 128:512 + h * 128 + 64], vc[:, h * 64:(h + 1) * 64],
                                start=False, stop=(h == 7)), reads=[mat.res, vc.res], writes=[psX.res])
                        P.op("act", lambda e, xs_=xs_, psX=psX: e.copy(out=xs_[:], in_=psX[0:64, :]), reads=[psX.res], writes=[xs_.res])
                        for h in range(8):
                            P.op("pe", lambda e, h=h, mat=mat, xs_=xs_, psU=psU: e.matmul(
                                psU[0:64, h * 64:(h + 1) * 64], mat[:, h * 64:(h + 1) * 64], xs_[:, h * 64:(h + 1) * 64],
                                start=(h == 0), stop=(h == 7)), reads=[mat.res, xs_.res], writes=[psU.res])
                        P.op("dve", lambda e, us_=us_, psU=psU: e.tensor_copy(out=us_[:], in_=psU[0:64, :]), reads=[psU.res], writes=[us_.res])
                        for h in range(8):
                            c, hh = h // 2, h % 2
                            P.op("pe", lambda e, h=h, c=c, hh=hh, fm=fm, zb=zb, psO=psO: e.matmul(
                                psO[0:64, h * 64:(h + 1) * 64], fm[:, hh, 256 + c * 64:256 + (c + 1) * 64], zb[:, h, :],
                                start=(h == 0), stop=False), reads=[fm.res, zb.res], writes=[psO.res])
                            P.op("pe", lambda e, h=h, mat=mat, vc=vc, psO=psO: e.matmul(
                                psO[0:64, h * 64:(h + 1) * 64], mat[:, 512 + h * 128 + 64:512 + h * 128 + 128], vc[:, h * 64:(h + 1) * 64],
                                start=False, stop=False), reads=[mat.res, vc.res], writes=[psO.res])
                            P.op("pe", lambda e, h=h, mat=mat, us_=us_, psO=psO: e.matmul(
                                psO[0:64, h * 64:(h + 1) * 64], mat[:, 1536 + h * 128 + 64:1536 + h * 128 + 128], us_[:, h * 64:(h + 1) * 64],
                                start=False, stop=(h == 7)), reads=[mat.res, us_.res], writes=[psO.res])
                        P.op("act", lambda e, osb=osb, psO=psO: e.copy(out=osb[:], in_=psO[0:64, :]), reads=[psO.res], writes=[osb.res])
                        P.dma("act", Od[d][ch * CH:(ch + 1) * CH, :], osb[:], reads=[osb.res], writes=[P.R("Od", d, ch)])
                        for h in range(8):
                            P.op("pe", lambda e, h=h, tm=tm, vc=vc, psZ=psZ: e.matmul(
                                psZ[0:64, h * 64:(h + 1) * 64], tm[:, h * 64:(h + 1) * 64], vc[:, h * 64:(h + 1) * 64],
                                start=(h == 0), stop=False), reads=[tm.res, vc.res], writes=[psZ.res])
                            P.op("pe", lambda e, h=h, tm=tm, us_=us_, psZ=psZ: e.matmul(
                                psZ[0:64, h * 64:(h + 1) * 64], tm[:, 512 + h * 64:512 + (h + 1) * 64], us_[:, h * 64:(h + 1) * 64],
                                start=False, stop=(h == 7)), reads=[tm.res, us_.res], writes=[psZ.res])
                        P.op("dve", lambda e, psZ=psZ, zf=zf: e.tensor_tensor(
                            out=ztmp[:], in0=psZ[0:64, :].rearrange("p (h v) -> p h v", h=8), in1=zf[:], op=ALU.add),
                            reads=[psZ.res, zf.res], writes=[ztmp.res])
                        gcv = GC2[:, d, :, :, ch:ch + 1].rearrange("p c hh o -> p (c hh) o").to_broadcast([64, 8, 64])
                        P.op("dve", lambda e, zf=zf, gcv=gcv: e.tensor_tensor(out=zf[:], in0=ztmp[:], in1=gcv, op=ALU.mult),
                             reads=[ztmp.res, GC2.res], writes=[zf.res])
                        P.op("act", lambda e, zb=zb, zf=zf: e.copy(out=zb[:], in_=zf[:]), reads=[zf.res], writes=[zb.res])
            P.barrier()
            if stop_after == "scan":
                P.finish()
                return

            with contextlib.ExitStack() as stO:
                load_cast = make_loader(stO)
                wo_b = sb("wo_b2", [128, 8, D], BF16, st=stO)
                load_cast(wo_b, lambda c0, c1: w_out[li].rearrange("(kc p) n -> p kc n", p=128)[:, :, c0:c1], D, 224)
                gnw = sb("gnw", [128, 2, 512], st=stO)
                P.dma("sp", gnw[:].rearrange("p a f -> p (a f)"), gn_wb[li].rearrange("a f -> (a f)").partition_broadcast(128),
                      writes=[gnw.res])
                gne = sb("gne", [128, 1], st=stO)
                P.op("pool", lambda e: e.memset(gne[:], GN_EPS), writes=[gne.res])
                o0 = [sb("o0_%d" % i, [128, 8, 64], st=stO) for i in range(2)]
                o1 = [sb("o1_%d" % i, [128, 8, 64], st=stO) for i in range(2)]
                vt2 = [sb("vt%d" % i, [128, 8, 64], BF16, st=stO) for i in range(2)]
                gt2 = [sb("gt%d" % i, [128, 512], st=stO) for i in range(2)]
                bt2 = [sb("bt%d" % i, [128, 8], st=stO) for i in range(2)]
                xr2 = [sb("xr%d" % i, [128, D], st=stO) for i in range(2)]
                sqr = sb("sqr", [128, 8, 64], st=stO)
                tb = sb("tb", [128, 8, 64], st=stO)
                stt = sb("stt", [128, 32], st=stO)
                rwb2 = [sb("rwb%d" % i, [128, 512], BF16, st=stO) for i in range(2)]
                rwT2 = [sb("rwT%d" % i, [128, 4, 128], BF16, st=stO) for i in range(2)]
                tmpy = sb("tmpy2", [128, D], st=stO)
                VMf = VM.rearrange("n p f -> (n p) f")
                for t in range(NLT if last else NT):
                    which = 0 if t < NLT else 1
                    M = mods[which]
                    a0, a1, vt, gt, bt, xr, rwb, rwT = o0[t % 2], o1[t % 2], vt2[t % 2], gt2[t % 2], bt2[t % 2], xr2[t % 2], rwb2[t % 2], rwT2[t % 2]
                    rows = slice(t * 128, (t + 1) * 128)

                    def ro_load(t_):
                        r_ = slice(t_ * 128, (t_ + 1) * 128)
                        i_ = t_ % 2
                        P.dma("sp", o0[i_][:].rearrange("p h v -> p (h v)"), Od[0][r_, :], reads=[P.R("Od", 0, 2 * t_), P.R("Od", 0, 2 * t_ + 1)], writes=[o0[i_].res])
                        P.dma("act", o1[i_][:].rearrange("p h v -> p (h v)"), Od[1][r_, :], reads=[P.R("Od", 1, 2 * t_), P.R("Od", 1, 2 * t_ + 1)], writes=[o1[i_].res])
                        P.dma("sp", vt2[i_][:].rearrange("p h v -> p (h v)"), VMf[r_, :], reads=[P.R("VM", (2 * t_) // 4)], writes=[vt2[i_].res])
                        P.dma("act", gt2[i_][:], gtm[r_, :], reads=[P.R("gtm", t_)], writes=[gt2[i_].res])
                        P.dma("sp", bt2[i_][:], bon[r_, :], reads=[P.R("bon", t_)], writes=[bt2[i_].res])
                        P.dma("act", xr2[i_][:], xs[r_, :], reads=[P.R("xs", t_)], writes=[xr2[i_].res])
                    if t == 0:
                        ro_load(0)
                    if t + 1 < (NLT if last else NT):
                        ro_load(t + 1)
                    P.op("pool", lambda e, a0=a0, a1=a1: e.tensor_tensor(out=a0[:], in0=a0[:], in1=a1[:], op=ALU.add),
                         reads=[a0.res, a1.res], writes=[a0.res])
                    P.op("dve", lambda e, a0=a0: e.tensor_reduce(out=stt[:, 0:8], in_=a0[:], axis=AX.X, op=ALU.add),
                         reads=[a0.res], writes=[stt.res])
                    P.op("dve", lambda e: e.tensor_scalar(out=stt[:, 8:16], in0=stt[:, 0:8], scalar1=1.0 / HD, scalar2=None, op0=ALU.mult),
                         reads=[stt.res], writes=[stt.res])
                    P.op("dve", lambda e, a0=a0: e.tensor_tensor(out=a0[:], in0=a0[:], in1=stt[:, 8:16].unsqueeze(2).to_broadcast([128, 8, 64]),
                                                                  op=ALU.subtract), reads=[a0.res, stt.res], writes=[a0.res])
                    P.op("act", lambda e, a0=a0: e.activation(out=sqr[:], in_=a0[:], func=AF.Square), reads=[a0.res], writes=[sqr.res])
                    P.op("dve", lambda e: e.tensor_reduce(out=stt[:, 16:24], in_=sqr[:], axis=AX.X, op=ALU.add),
                         reads=[sqr.res], writes=[stt.res])
                    P.op("act", lambda e: e.activation(out=stt[:, 24:32], in_=stt[:, 16:24], func=AF.Sqrt, bias=gne[:, 0:1], scale=1.0 / HD),
                         reads=[stt.res, gne.res], writes=[stt.res])
                    P.op("dve", lambda e: e.reciprocal(out=stt[:, 24:32], in_=stt[:, 24:32]), reads=[stt.res], writes=[stt.res])
                    P.op("dve", lambda e, a0=a0: e.tensor_tensor(out=a0[:], in0=a0[:], in1=stt[:, 24:32].unsqueeze(2).to_broadcast([128, 8, 64]),
                                                                  op=ALU.mult), reads=[a0.res, stt.res], writes=[a0.res])
                    a0f = a0[:].rearrange("p h v -> p (h v)")
                    P.op("pool", lambda e, a0=a0, a0f=a0f: e.tensor_tensor(out=a0f, in0=a0f, in1=gnw[:, 0, :], op=ALU.mult),
                         reads=[a0.res, gnw.res], writes=[a0.res])
                    P.op("pool", lambda e, a0=a0, a0f=a0f: e.tensor_tensor(out=a0f, in0=a0f, in1=gnw[:, 1, :], op=ALU.add),
                         reads=[a0.res, gnw.res], writes=[a0.res])
                    P.op("dve", lambda e, vt=vt, bt=bt: e.tensor_tensor(out=tb[:], in0=vt[:], in1=bt[:].unsqueeze(2).to_broadcast([128, 8, 64]),
                                                                        op=ALU.mult), reads=[vt.res, bt.res], writes=[tb.res])
                    P.op("pool", lambda e, a0=a0: e.tensor_tensor(out=a0[:], in0=a0[:], in1=tb[:], op=ALU.add),
                         reads=[a0.res, tb.res], writes=[a0.res])
                    P.op("dve", lambda e, a0f=a0f, a0=a0, gt=gt, rwb=rwb: e.tensor_tensor(out=rwb[:], in0=a0f, in1=gt[:], op=ALU.mult),
                         reads=[a0.res, gt.res], writes=[rwb.res])
                    psT = psum[0 + (t % 2)]
                    for c in range(4):
                        P.op("pe", lambda e, c=c, rwb=rwb, psT=psT: e.transpose(
                            out=psT[:].bitcast(BF16)[:, c * 128:(c + 1) * 128], in_=rwb[:, c * 128:(c + 1) * 128], identity=ident_b[:]),
                            reads=[rwb.res, ident_b.res], writes=[psT.res])
                    P.op("act", lambda e, rwT=rwT, psT=psT: e.copy(
                        out=rwT[:], in_=psT[:].bitcast(BF16)[:, 0:512].rearrange("p (c n) -> p c n", c=4)),
                        reads=[psT.res], writes=[rwT.res])
                    for half in range(2):
                        psY = psum[2 + 2 * (t % 2) + half]
                        for c in range(4):
                            P.op("pe", lambda e, c=c, half=half, psY=psY, rwT=rwT: e.matmul(
                                psY[:], rwT[:, c, :], wo_b[:, 4 + c, half * 512:(half + 1) * 512],
                                start=(c == 0), stop=(c == 3)), reads=[rwT.res, wo_b.res], writes=[psY.res])
                        P.op("dve", lambda e, half=half, psY=psY, M=M: e.tensor_tensor(
                            out=tmpy[:, half * 512:(half + 1) * 512], in0=psY[:], in1=M[2][:, half * 512:(half + 1) * 512],
                            op=ALU.mult), reads=[psY.res, M[2].res], writes=[tmpy.res])
                    P.op("pool", lambda e, xr=xr: e.tensor_tensor(out=xr[:], in0=xr[:], in1=tmpy[:], op=ALU.add),
                         reads=[xr.res, tmpy.res], writes=[xr.res])
                    P.dma("sp", xs[rows, :], xr[:], reads=[xr.res], writes=[P.R("xs", t)])
            P.barrier()
            if stop_after == "rw":
                P.finish()
                return

            is_moe = li % 2 == 1
            jj = li // 2
            n_tiles = NLT if last else NT
            G_all = sb("G_all", [128, NT, 8], st=stL)
            with contextlib.ExitStack() as stN:
                xt2 = [sb("nxt%d" % i, [128, D], st=stN) for i in range(2)]
                sq = sb("nsq", [128, D], st=stN)
                hf2 = sb("nhf2", [128, D], st=stN)
                hb2 = [sb("nhb%d" % i, [128, D], BF16, st=stN) for i in range(2)]
                hTg2 = [sb("nhTg%d" % i, [128, 8, 512], BF16, st=stN) for i in range(2)]
                st10 = sb("nst", [128, 48], st=stN)
                if is_moe:
                    wr = sb("wr", [128, 8, NE], st=stN)
                    P.dma("sp", wr[:], moe_router[jj].rearrange("(kc p) e -> p kc e", p=128), writes=[wr.res])
                    hTf = sb("hTf", [128, 8, 128], st=stN)
                for t in range(n_tiles):
                    which = 0 if t < NLT else 1
                    M = mods[which]
                    grp, tin = t // 4, t % 4
                    xt, hb, hTg = xt2[t % 2], hb2[t % 2], hTg2[grp % 2]
                    if t == 0:
                        P.dma("sp", xt[:], xs[0:128, :], reads=[P.R("xs", 0)], writes=[xt.res])
                    if t + 1 < n_tiles:
                        P.dma("sp", xt2[(t + 1) % 2][:], xs[(t + 1) * 128:(t + 2) * 128, :], reads=[P.R("xs", t + 1)],
                              writes=[xt2[(t + 1) % 2].res])
                    P.op("act", lambda e, xt=xt: e.activation(out=sq[:], in_=xt[:], func=AF.Square), reads=[xt.res], writes=[sq.res])
                    P.op("dve", lambda e: e.tensor_reduce(out=st10[:, 0:1], in_=sq[:], axis=AX.X, op=ALU.add),
                         reads=[sq.res], writes=[st10.res])
                    P.op("act", lambda e: e.activation(out=st10[:, 1:2], in_=st10[:, 0:1], func=AF.Sqrt, bias=eps_t[:, 0:1], scale=1.0 / D),
                         reads=[st10.res, eps_t.res], writes=[st10.res])
                    P.op("dve", lambda e: e.reciprocal(out=st10[:, 1:2], in_=st10[:, 1:2]), reads=[st10.res], writes=[st10.res])
                    P.op("dve", lambda e, xt=xt, M=M: e.scalar_tensor_tensor(
                        out=sq[:], in0=xt[:], scalar=st10[:, 1:2], in1=M[4][:], op0=ALU.mult, op1=ALU.mult),
                        reads=[xt.res, st10.res, M[4].res], writes=[sq.res])
                    if is_moe:
                        P.op("dve", lambda e, M=M: e.tensor_tensor(out=hf2[:], in0=sq[:], in1=M[3][:], op=ALU.add),
                             reads=[sq.res, M[3].res], writes=[hf2.res])
                        P.op("act", lambda e, hb=hb: e.copy(out=hb[:], in_=hf2[:]), reads=[hf2.res], writes=[hb.res])
                    else:
                        P.op("dve", lambda e, hb=hb, M=M: e.tensor_tensor(out=hb[:], in0=sq[:], in1=M[3][:], op=ALU.add),
                             reads=[sq.res, M[3].res], writes=[hb.res])
                    psT = psum[t % 2]
                    for kc in range(8):
                        P.op("pe", lambda e, kc=kc, psT=psT, hb=hb: e.transpose(
                            out=psT[:].bitcast(BF16)[:, kc * 128:(kc + 1) * 128], in_=hb[:, kc * 128:(kc + 1) * 128],
                            identity=ident_b[:]), reads=[hb.res, ident_b.res], writes=[psT.res])
                    P.op("act", lambda e, psT=psT, hTg=hTg, tin=tin: e.copy(
                        out=hTg[:, :, tin * 128:(tin + 1) * 128], in_=psT[:].bitcast(BF16).rearrange("p (k n) -> p k n", k=8)),
                        reads=[psT.res], writes=[hTg.res])
                    if is_moe:
                        for hf_ in range(2):
                            psF = psum[2 + hf_]
                            for k4 in range(4):
                                kc = 4 * hf_ + k4
                                P.op("pe", lambda e, psF=psF, k4=k4, kc=kc: e.transpose(
                                    out=psF[:, k4 * 128:(k4 + 1) * 128], in_=hf2[:, kc * 128:(kc + 1) * 128], identity=ident_f[:]),
                                    reads=[hf2.res, ident_f.res], writes=[psF.res])
                            P.op("dve" if hf_ == 0 else "act",
                                 (lambda e, psF=psF, hf_=hf_: e.tensor_copy(out=hTf[:, 4 * hf_:4 * hf_ + 4, :],
                                                                          in_=psF[:].rearrange("p (k n) -> p k n", k=4))) if hf_ == 0 else
                                 (lambda e, psF=psF, hf_=hf_: e.copy(out=hTf[:, 4 * hf_:4 * hf_ + 4, :],
                                                                    in_=psF[:].rearrange("p (k n) -> p k n", k=4))),
                                 reads=[psF.res], writes=[hTf.res])
                        psL = psum[4 + (t % 2)]
                        for kc in range(8):
                            P.op("pe", lambda e, kc=kc, psL=psL: e.matmul(psL[:, 0:NE], hTf[:, kc, :], wr[:, kc, :],
                                                                         start=(kc == 0), stop=(kc == 7)),
                                 reads=[hTf.res, wr.res], writes=[psL.res])
                        lg, m8, ng, ex, gm = st10[:, 8:16], st10[:, 16:24], st10[:, 24:25], st10[:, 32:40], st10[:, 40:48]
                        P.op("dve", lambda e, psL=psL, lg=lg: e.tensor_copy(out=lg, in_=psL[:, 0:NE]), reads=[psL.res], writes=[st10.res])
                        P.op("dve", lambda e, lg=lg, m8=m8: e.max(out=m8, in_=lg), reads=[st10.res], writes=[st10.res])
                        P.op("dve", lambda e, ng=ng, m8=m8: e.tensor_scalar(out=ng, in0=m8[:, 0:1], scalar1=-1.0, scalar2=None, op0=ALU.mult),
                             reads=[st10.res], writes=[st10.res])
                        P.op("act", lambda e, ex=ex, lg=lg, ng=ng: e.activation(out=ex, in_=lg, func=AF.Exp, bias=ng, scale=1.0),
                             reads=[st10.res], writes=[st10.res])
                        P.op("dve", lambda e, gm=gm, lg=lg, m8=m8, ex=ex: e.scalar_tensor_tensor(
                            out=gm, in0=lg, scalar=m8[:, 1:2], in1=ex, op0=ALU.is_ge, op1=ALU.mult),
                            reads=[st10.res], writes=[st10.res])
                        P.op("dve", lambda e, gm=gm: e.tensor_reduce(out=st10[:, 25:26], in_=gm, axis=AX.X, op=ALU.add),
                             reads=[st10.res], writes=[st10.res])
                        P.op("dve", lambda e: e.reciprocal(out=st10[:, 25:26], in_=st10[:, 25:26]), reads=[st10.res], writes=[st10.res])
                        P.op("dve", lambda e, gm=gm, t=t: e.tensor_scalar(out=G_all[:, t, :], in0=gm, scalar1=st10[:, 25:26], scalar2=None,
                                                                           op0=ALU.mult), reads=[st10.res], writes=[G_all.res])
                    if tin == 3 or t == n_tiles - 1:
                        N = (tin + 1) * 128
                        P.dma("act", H2[grp][:, :, 0:N], hTg[:, :, 0:N], reads=[hTg.res], writes=[P.R("H2", grp)])
            P.barrier()
            if stop_after == "norm2":
                dG = nc.dram_tensor("dbg_G", [128, NT, 8], F32, kind="ExternalOutput").ap()
                P.dma("sp", dG, G_all[:], reads=[G_all.res])
                P.finish()
                return

            groups = [(g, g * 512, 512, 0) for g in range(8)] + ([] if last else [(8, L, LC, 1)])
            if is_moe:
                experts = [(moe_wg[jj][e], moe_wu[jj][e], moe_wd[jj][e], DFE // 128, e) for e in range(NE)]
                split = [7, 7, 7, 7]
            else:
                experts = [(ffn_wg[jj], ffn_wu[jj], ffn_wd[jj], DFF // 128, None)]
                split = [8, 8, 6]
            subs = []
            for (wgs, wus, wds, nfc, e) in experts:
                f0 = 0
                for nf in split:
                    subs.append((wgs, wus, wds, f0, nf, e))
                    f0 += nf
                assert f0 == nfc
            with contextlib.ExitStack() as stF:
                load_cast = make_loader(stF)
                WG = [sb("WG%d" % i, [128, 8, 1024], BF16, st=stF) for i in range(2)]
                WU = [sb("WU%d" % i, [128, 8, 1024], BF16, st=stF) for i in range(2)]
                WD = [sb("WD%d" % i, [128, 8, 1024], BF16, st=stF) for i in range(2)]
                h2g = [sb("h2g%d" % i, [128, 8, 512], BF16, st=stF) for i in range(2)]
                aT = sb("aT", [128, 8, 512], BF16, st=stF)
                sgt = [sb("sgt%d" % i, [128, 512], st=stF) for i in range(2)]
                xf2 = [sb("xf%d" % i, [128, D], st=stF) for i in range(2)]
                dl2 = [sb("dl%d" % i, [128, D], st=stF) for i in range(2)]

                def load_tasks(si):
                    wgs, wus, wds, f0, nf, e = subs[si]
                    b = si % 2
                    tasks = []
                    for (dst, src) in ((WG[b], wgs), (WU[b], wus)):
                        v = src.rearrange("(kc p) n -> p kc n", p=128)
                        for c0 in range(0, nf * 128, 224):
                            c1 = min(nf * 128, c0 + 224)

                            tasks.append(lambda dst=dst, v=v, c0=c0, c1=c1, f0=f0: load_cast(
                                dst, lambda a, b_, v=v, f0=f0, c0=c0: v[:, :, f0 * 128 + c0 + a:f0 * 128 + c0 + b_], c1 - c0, 224, nk=8, col0=c0))
                    vd = wds[f0 * 128:(f0 + nf) * 128, :].rearrange("(f p) n -> p f n", p=128)
                    for c0 in range(0, D, 224):
                        c1 = min(D, c0 + 224)
                        tasks.append(lambda vd=vd, c0=c0, c1=c1, nf=nf, b=b: load_cast(
                            WD[b], lambda a, b_, vd=vd, c0=c0: vd[:, :, c0 + a:c0 + b_], c1 - c0, 224, nk=nf, col0=c0))
                    return tasks

                for tsk in load_tasks(0):
                    tsk()
                xrr = 0
                for si, (wgs, wus, wds, f0, nf, e) in enumerate(subs):
                    b = si % 2
                    nxt = load_tasks(si + 1) if si + 1 < len(subs) else []
                    per_g = (len(nxt) + len(groups) - 1) // len(groups)
                    tiles_seq = [(t0 // 128 + tt) for (g, t0, N, which) in groups for tt in range(N // 128)]

                    def load_hg(gi_):
                        g_, t0_, N_, w_ = groups[gi_]
                        P.dma("sp", h2g[gi_ % 2][:, :, 0:N_], H2[g_][:, :, 0:N_], reads=[P.R("H2", g_)], writes=[h2g[gi_ % 2].res])

                    def load_xf(k_):
                        t_ = tiles_seq[k_]
                        P.dma("act", xf2[k_ % 2][:], xs[t_ * 128:(t_ + 1) * 128, :], reads=[P.R("xs", t_)], writes=[xf2[k_ % 2].res])
                    load_hg(0)
                    load_xf(0)
                    xk = 0
                    for gi, (g, t0, N, which) in enumerate(groups):
                        M = mods[which]
                        hg = h2g[gi % 2]
                        if gi + 1 < len(groups):
                            load_hg(gi + 1)
                        for f in range(nf):
                            psG, psU = psum[(2 * f) % 4], psum[(2 * f + 1) % 4]
                            for kc in range(8):
                                P.op("pe", lambda e_, kc=kc, f=f, psG=psG, hg=hg, N=N, b=b: e_.matmul(
                                    psG[:, 0:N], WG[b][:, kc, f * 128:(f + 1) * 128], hg[:, kc, 0:N], start=(kc == 0), stop=(kc == 7)),
                                    reads=[WG[b].res, hg.res], writes=[psG.res])
                            for kc in range(8):
                                P.op("pe", lambda e_, kc=kc, f=f, psU=psU, hg=hg, N=N, b=b: e_.matmul(
                                    psU[:, 0:N], WU[b][:, kc, f * 128:(f + 1) * 128], hg[:, kc, 0:N], start=(kc == 0), stop=(kc == 7)),
                                    reads=[WU[b].res, hg.res], writes=[psU.res])
                            sg_ = sgt[f % 2]
                            P.op("act", lambda e_, sg_=sg_, psG=psG, N=N: e_.activation(out=sg_[:, 0:N], in_=psG[:, 0:N], func=AF.Silu),
                                 reads=[psG.res], writes=[sg_.res])
                            P.op("dve", lambda e_, sg_=sg_, psU=psU, f=f, N=N: e_.tensor_tensor(
                                out=aT[:, f, 0:N], in0=sg_[:, 0:N], in1=psU[:, 0:N], op=ALU.mult),
                                reads=[sg_.res, psU.res], writes=[aT.res])
                        for tt in range(N // 128):
                            t = t0 // 128 + tt
                            xf, dl = xf2[xk % 2], dl2[xk % 2]
                            rows = slice(t * 128, (t + 1) * 128)
                            for half in range(2):
                                psY = psum[4 + (2 * tt + half) % 4]
                                for f in range(nf):
                                    P.op("pe", lambda e_, f=f, tt=tt, half=half, psY=psY, b=b, nf=nf: e_.matmul(
                                        psY[:], aT[:, f, tt * 128:(tt + 1) * 128], WD[b][:, f, half * 512:(half + 1) * 512],
                                        start=(f == 0), stop=(f == nf - 1)), reads=[aT.res, WD[b].res], writes=[psY.res])
                                hs = slice(half * 512, (half + 1) * 512)
                                if e is None:
                                    P.op("dve", lambda e_, psY=psY, dl=dl, hs=hs, M=M: e_.tensor_tensor(
                                        out=dl[:, hs], in0=psY[:], in1=M[5][:, hs], op=ALU.mult),
                                        reads=[psY.res, M[5].res], writes=[dl.res])
                                else:
                                    P.op("dve", lambda e_, psY=psY, dl=dl, hs=hs, M=M, t=t, e=e: e_.scalar_tensor_tensor(
                                        out=dl[:, hs], in0=psY[:], scalar=G_all[:, t, e:e + 1], in1=M[5][:, hs],
                                        op0=ALU.mult, op1=ALU.mult), reads=[psY.res, M[5].res, G_all.res], writes=[dl.res])
                            P.op("dve", lambda e_, xf=xf, dl=dl: e_.tensor_tensor(out=xf[:], in0=xf[:], in1=dl[:], op=ALU.add),
                                 reads=[xf.res, dl.res], writes=[xf.res])
                            P.dma("sp", xs[rows, :], xf[:], reads=[xf.res], writes=[P.R("xs", t)])
                            xk += 1
                            if xk < len(tiles_seq):
                                load_xf(xk)
                        for tsk in nxt[gi * per_g:(gi + 1) * per_g]:
                            tsk()
            P.barrier()
            if stop_after == "ffn":
                P.finish()
                return
    for i in range(8):
        P.dma("sp" if i % 2 == 0 else "act", y_out[i * 512:(i + 1) * 512, :], xs[i * 512:(i + 1) * 512, :],
              reads=[P.R("xs", 4 * i + j) for j in range(4)], writes=[P.R("y", i)])
    P.finish()


def _prep_inputs(inputs):
    f = lambda a: np.ascontiguousarray(np.asarray(a, dtype=np.float32))
    com = {}
    for k in ("ada_w", "ada_b", "norm1_g", "norm2_g", "w_in", "w_out", "ffn_wg", "ffn_wu", "ffn_wd", "moe_router", "moe_wg", "moe_wu", "moe_wd"):
        com[k] = f(inputs[k])
    com["qk_gain"] = f(np.concatenate([inputs["q_gain"], inputs["k_gain"]], axis=1))
    rows = L // 64
    row = np.repeat(np.arange(rows, dtype=np.float32), 64)
    col = np.tile(np.arange(64, dtype=np.float32), rows)
    inv = (10000.0 ** (-np.arange(0, 32, 2, dtype=np.float32) / 32)).astype(np.float32)
    ang = np.concatenate([row[:, None] * inv, col[:, None] * inv], axis=-1).astype(np.float32)
    com["rope"] = f(np.concatenate([np.cos(ang), np.sin(ang)], axis=-1))
    com["ident"] = np.eye(128, dtype=np.float32)
    col = lambda v, n: np.asarray(v, np.float32).reshape(n, 128).T
    rwv = []
    for i in range(DEPTH):
        parts = [col(inputs["rw_kk"][i], 4), col(inputs["rw_ka"][i], 4), col(inputs["rw_rk"][i], 4),
                 col(inputs["rw_w0"][i][0], 4), col(inputs["rw_w0"][i][1], 4),
                 col(inputs["rw_a0"][i][0], 4), col(inputs["rw_a0"][i][1], 4), col(inputs["shift_mu"][i], 15)]
        rwv.append(np.concatenate(parts, axis=1))
    com["rwv"] = f(np.stack(rwv))
    com["rw_w2"] = f(np.asarray(inputs["rw_w2"]).reshape(DEPTH, 128, 512))
    com["rw_a2"] = f(np.asarray(inputs["rw_a2"]).reshape(DEPTH, 128, 512))
    com["rw_g2"] = f(inputs["rw_g2"])
    com["gn_wb"] = f(np.stack([inputs["rw_gn_w"], inputs["rw_gn_b"]], axis=1))
    c128 = np.zeros((128, 386), np.float32)
    c128[:64, :64] = 1.0
    c128[64:, 64:128] = 1.0
    c128[:64, 128] = 1.0
    c128[64:, 129] = 1.0
    c128[:, 130:386] = 1.0
    c128[:, 130:386:CH] = 0.0
    com["cst128"] = c128
    si, ti = np.meshgrid(np.arange(CH), np.arange(CH), indexing="ij")
    strict = [(si < ti), (si > ti)]
    incl = [(si <= ti), (si >= ti)]
    c64 = np.zeros((64, 7, 512), np.float32)
    for d in range(2):
        mb = np.stack([strict[d].astype(np.float32), -incl[d].astype(np.float32)], axis=1)
        mk = np.stack([strict[d].astype(np.float32), incl[d].astype(np.float32)], axis=1)
        c64[:, d] = np.tile(mb.reshape(64, 1, 128), (1, 4, 1)).reshape(64, 512)
        c64[:, 2 + d] = np.tile(mk.reshape(64, 1, 128), (1, 4, 1)).reshape(64, 512)
        mn = strict[1 - d].astype(np.float32)
        c64[:, 4 + d] = np.tile(mn.reshape(64, 1, 64), (1, 8, 1)).reshape(64, 512)
    c64[:, 6] = np.tile(np.eye(64, dtype=np.float32).reshape(64, 1, 64), (1, 8, 1)).reshape(64, 512)
    com["cst64"] = c64.reshape(64, 3584).astype(ml_dtypes.bfloat16)
    return com


def kernel(**inputs):
    com = _prep_inputs(inputs)
    x = np.asarray(inputs["x"], dtype=np.float32)
    ctx = np.asarray(inputs["ctx"], dtype=np.float32)
    c = np.asarray(inputs["c"], dtype=np.float32)
    c_ctx = np.asarray(inputs["c_ctx"], dtype=np.float32)
    nc = bass.Bass("TRN2", target_bir_lowering=False)
    build(nc)
    in_maps = []
    for b in range(8):
        m = dict(com)
        m["x"] = np.ascontiguousarray(x[b])
        m["ctx"] = np.ascontiguousarray(ctx[b])
        m["cvec"] = np.ascontiguousarray(
            np.concatenate([c[b].reshape(8, 128).T, c_ctx.reshape(8, 128).T], axis=1))
        in_maps.append(m)
    res = run_bass_kernel_spmd(nc, in_maps, core_ids=list(range(8)))
    return np.stack([r["y"] for r in res.results], axis=0)
```
